# Optimizing a Trainium2 kernel written in Bass

```python
import math
import jax
import jax.numpy as jnp
from jax import lax
import numpy as np

D_MODEL = 1024
BATCH = 4
SEQ = 4096
DEPTH = 1
DEC_BATCH = 128
DEC_SEQ = 4
PAST_LEN = 2048
PAGE_SIZE = 128

N_HEADS = 4
HEAD_DIM = 64
QK_DIM = 2 * HEAD_DIM
V_DIM = 2 * HEAD_DIM
ATT_WIDTH = N_HEADS * V_DIM
ATTN_SCALE = HEAD_DIM ** -0.5
Q_BLOCK = 128
NEG_INF = -1e30
NUM_BUCKETS = 32
MAX_EXACT = NUM_BUCKETS // 2
MAX_DISTANCE = 128
CONV_DIM = D_MODEL // 2
CONV_WIDTH = 31
Q_COLS = N_HEADS * QK_DIM
K_COLS = N_HEADS * QK_DIM
V_COLS = N_HEADS * V_DIM
GLU_COLS = 2 * CONV_DIM
GATE_COLS = 2 * D_MODEL
IN_COLS = Q_COLS + K_COLS + V_COLS + GLU_COLS + GATE_COLS
SPLITS = [Q_COLS, Q_COLS + K_COLS, Q_COLS + K_COLS + V_COLS, Q_COLS + K_COLS + V_COLS + GLU_COLS]
N_EXPERTS = 32
TOP_K = 4
D_FF = D_MODEL
SWIGLU_LIMIT = 7.0
SWIGLU_ALPHA = 1.702
MOE_BLOCK = 256
DEEPNORM_ALPHA = (2 * DEPTH) ** 0.25
DEEPNORM_BETA = (8 * DEPTH) ** -0.25
LN_EPS = 1e-5

kernel_name = 'hybrid_diffattn_conformer_moe_step'


def layer_norm(x, g, b):
    xf = x.astype(jnp.float32)
    mu = jnp.mean(xf, -1, keepdims=True)
    var = jnp.mean(jnp.square(xf - mu), -1, keepdims=True)
    y = (xf - mu) * lax.rsqrt(var + LN_EPS) * g.astype(jnp.float32) + b.astype(jnp.float32)
    return y.astype(x.dtype)


def lambda_init(layer):
    return 0.8 - 0.6 * math.exp(-0.3 * layer)


def t5_bucket(dist):
    n = jnp.maximum(dist, 0)
    nf = jnp.maximum(n, 1).astype(jnp.float32)
    large = MAX_EXACT + (jnp.log(nf / MAX_EXACT) / math.log(MAX_DISTANCE / MAX_EXACT)
                         * (NUM_BUCKETS - MAX_EXACT)).astype(jnp.int32)
    large = jnp.minimum(large, NUM_BUCKETS - 1)
    return jnp.where(n < MAX_EXACT, n, large)


def diff_attn_block(q, k, v, q_pos, k_pos, rel_bias, lam):
    s = jnp.einsum('bqhcd,bkhcd->bchqk', q, k).astype(jnp.float32) * ATTN_SCALE
    bias = rel_bias[t5_bucket(q_pos[:, None] - k_pos[None, :])].astype(jnp.float32)
    s = s + jnp.transpose(bias, (2, 0, 1))
    s = jnp.where(k_pos[None, :] <= q_pos[:, None], s, NEG_INF)
    p = jax.nn.softmax(s, axis=-1)
    w = p[:, 0] - lam * p[:, 1]
    return jnp.einsum('bhqk,bkhv->bqhv', w.astype(v.dtype), v)


def diff_attention(q, k, v, q_pos, k_pos, rel_bias, lam):
    B, T = q.shape[0], q.shape[1]
    if T > Q_BLOCK and T % Q_BLOCK == 0:
        nb = T // Q_BLOCK
        qb = jnp.swapaxes(q.reshape(B, nb, Q_BLOCK, N_HEADS, 2, HEAD_DIM), 0, 1)
        pb = q_pos.reshape(nb, Q_BLOCK)
        ob = lax.map(lambda a: diff_attn_block(a[0], k, v, a[1], k_pos, rel_bias, lam), (qb, pb))
        return jnp.swapaxes(ob, 0, 1).reshape(B, T, N_HEADS, V_DIM)
    return diff_attn_block(q, k, v, q_pos, k_pos, rel_bias, lam)


def conformer_conv(c, conv_prev, conv_w, conv_b, ln_g, ln_b, w_proj, b_proj):
    u = c[..., :CONV_DIM] * jax.nn.sigmoid(c[..., CONV_DIM:])
    buf = jnp.concatenate([conv_prev.astype(u.dtype), u], axis=1)
    y = lax.conv_general_dilated(buf, conv_w[:, None, :].astype(u.dtype), window_strides=(1,),
                                 padding='VALID', dimension_numbers=('NWC', 'WIO', 'NWC'),
                                 feature_group_count=CONV_DIM) + conv_b
    y = jax.nn.silu(layer_norm(y, ln_g, ln_b))
    return y @ w_proj + b_proj, buf[:, buf.shape[1] - (CONV_WIDTH - 1):]


def token_mixers(h, k_past, v_past, conv_prev, p, lam_init):
    B, T, _ = h.shape
    P = k_past.shape[1]
    z = h @ p['w_in'] + p['b_in']
    q, k_new, v_new, c, gates = jnp.split(z, SPLITS, axis=-1)
    q = q.reshape(B, T, N_HEADS, 2, HEAD_DIM)
    k_new = k_new.reshape(B, T, N_HEADS, QK_DIM)
    v_new = v_new.reshape(B, T, N_HEADS, V_DIM)
    k_all = jnp.concatenate([k_past.astype(h.dtype), k_new], axis=1).reshape(B, P + T, N_HEADS, 2, HEAD_DIM)
    v_all = jnp.concatenate([v_past.astype(h.dtype), v_new], axis=1)
    f32 = jnp.float32
    lam = (jnp.exp(jnp.sum(p['lambda_q1'].astype(f32) * p['lambda_k1'].astype(f32)))
           - jnp.exp(jnp.sum(p['lambda_q2'].astype(f32) * p['lambda_k2'].astype(f32))) + lam_init)
    q_pos = P + jnp.arange(T, dtype=jnp.int32)
    k_pos = jnp.arange(P + T, dtype=jnp.int32)
    o = diff_attention(q, k_all, v_all, q_pos, k_pos, p['rel_bias'], lam).astype(f32)
    o = o * lax.rsqrt(jnp.mean(jnp.square(o), -1, keepdims=True) + LN_EPS) * p['subln_g'].astype(f32)
    o = (o * (1.0 - lam_init)).astype(h.dtype).reshape(B, T, ATT_WIDTH)
    a = o @ p['w_attn_proj']
    b, conv_new = conformer_conv(c, conv_prev, p['conv_w'], p['conv_b'], p['conv_ln_g'], p['conv_ln_b'],
                                 p['w_conv_proj'], p['b_conv_proj'])
    g_a, g_b = jnp.split(jax.nn.sigmoid(gates), 2, axis=-1)
    mix = (g_a * a + g_b * b) @ p['w_out']
    return mix, k_new, v_new, conv_new


def moe(x, router_w, router_b, w1, b1, w2, b2):
    n, d = x.shape
    logits = (x @ router_w + router_b).astype(jnp.float32)
    top_v, top_i = lax.top_k(logits, TOP_K)
    top_w = jax.nn.softmax(top_v, axis=-1)
    gates = jnp.einsum('tk,tke->te', top_w, jax.nn.one_hot(top_i, N_EXPERTS, dtype=jnp.float32)).astype(x.dtype)
    blk = min(MOE_BLOCK, n)
    nb = -(-n // blk)
    pad = nb * blk - n
    xp = jnp.pad(x, ((0, pad), (0, 0))).reshape(nb, blk, d)
    gp = jnp.pad(gates, ((0, pad), (0, 0))).reshape(nb, blk, N_EXPERTS)

    def expert_block(args):
        xb, gb = args
        hh = jnp.einsum('td,edf->tef', xb, w1) + b1
        g = jnp.minimum(hh[..., :D_FF], SWIGLU_LIMIT)
        u = jnp.clip(hh[..., D_FF:], -SWIGLU_LIMIT, SWIGLU_LIMIT)
        act = (u + 1.0) * g * jax.nn.sigmoid(SWIGLU_ALPHA * g)
        return jnp.einsum('tef,efd->td', act * gb[..., None], w2) + gb @ b2

    y = lax.map(expert_block, (xp, gp))
    return y.reshape(nb * blk, d)[:n]


def decoder_layer(h, k_past, v_past, conv_prev, p, lam_init):
    mix, k_new, v_new, conv_new = token_mixers(h, k_past, v_past, conv_prev, p, lam_init)
    h1 = layer_norm(DEEPNORM_ALPHA * h + mix, p['ln1_g'], p['ln1_b'])
    B, T, D = h1.shape
    ff = moe(h1.reshape(B * T, D), p['router_w'], p['router_b'], p['expert_w1'], p['expert_b1'],
             p['expert_w2'], p['expert_b2']).reshape(B, T, D)
    h2 = layer_norm(DEEPNORM_ALPHA * h1 + ff, p['ln2_g'], p['ln2_b'])
    return h2, k_new, v_new, conv_new


def setup_inputs(seed: int = 0) -> dict:
    key = jax.random.key(seed)
    ks = jax.random.split(key, 36)
    f32 = jnp.float32
    nrm = lambda k, s: jax.random.normal(k, s, f32)
    n_pages = PAST_LEN // PAGE_SIZE
    n_used = DEC_BATCH * n_pages
    n_pool = n_used + max(1, n_used // 4)
    page_table = jax.random.permutation(ks[0], n_pool)[:n_used].reshape(DEC_BATCH, n_pages).astype(jnp.int32)
    w_in = nrm(ks[1], (DEPTH, D_MODEL, IN_COLS)) * D_MODEL ** -0.5
    w_in = w_in.at[:, :, SPLITS[1]:SPLITS[2]].multiply(DEEPNORM_BETA)
    return {
        'x_prompt': nrm(ks[2], (BATCH, SEQ, D_MODEL)),
        'x_sample': nrm(ks[3], (DEC_BATCH, DEC_SEQ, D_MODEL)),
        'cache_k': nrm(ks[4], (DEPTH, n_pool, PAGE_SIZE, N_HEADS, QK_DIM)),
        'cache_v': nrm(ks[5], (DEPTH, n_pool, PAGE_SIZE, N_HEADS, V_DIM)) * DEEPNORM_BETA,
        'page_table': page_table,
        'state_conv': nrm(ks[6], (DEPTH, DEC_BATCH, CONV_WIDTH - 1, CONV_DIM)) * 0.5,
        'w_in': w_in,
        'b_in': nrm(ks[7], (DEPTH, IN_COLS)) * 0.02,
        'lambda_q1': nrm(ks[8], (DEPTH, HEAD_DIM)) * 0.1,
        'lambda_k1': nrm(ks[9], (DEPTH, HEAD_DIM)) * 0.1,
        'lambda_q2': nrm(ks[10], (DEPTH, HEAD_DIM)) * 0.1,
        'lambda_k2': nrm(ks[11], (DEPTH, HEAD_DIM)) * 0.1,
        'subln_g': 1.0 + 0.01 * nrm(ks[12], (DEPTH, V_DIM)),
        'rel_bias': nrm(ks[13], (NUM_BUCKETS, N_HEADS)) * 0.5,
        'w_attn_proj': nrm(ks[14], (DEPTH, ATT_WIDTH, D_MODEL)) * ATT_WIDTH ** -0.5 * DEEPNORM_BETA,
        'conv_w': nrm(ks[15], (DEPTH, CONV_WIDTH, CONV_DIM)) * CONV_WIDTH ** -0.5,
        'conv_b': nrm(ks[16], (DEPTH, CONV_DIM)) * 0.02,
        'conv_ln_g': 1.0 + 0.01 * nrm(ks[17], (DEPTH, CONV_DIM)),
        'conv_ln_b': nrm(ks[18], (DEPTH, CONV_DIM)) * 0.01,
        'w_conv_proj': nrm(ks[19], (DEPTH, CONV_DIM, D_MODEL)) * CONV_DIM ** -0.5 * DEEPNORM_BETA,
        'b_conv_proj': nrm(ks[20], (DEPTH, D_MODEL)) * 0.02,
        'w_out': nrm(ks[21], (DEPTH, D_MODEL, D_MODEL)) * D_MODEL ** -0.5 * DEEPNORM_BETA,
        'ln1_g': 1.0 + 0.01 * nrm(ks[22], (DEPTH, D_MODEL)),
        'ln1_b': nrm(ks[23], (DEPTH, D_MODEL)) * 0.01,
        'router_w': nrm(ks[24], (DEPTH, D_MODEL, N_EXPERTS)) * D_MODEL ** -0.5,
        'router_b': nrm(ks[25], (DEPTH, N_EXPERTS)) * 0.01,
        'expert_w1': nrm(ks[26], (DEPTH, N_EXPERTS, D_MODEL, 2 * D_FF)) * D_MODEL ** -0.5,
        'expert_b1': nrm(ks[27], (DEPTH, N_EXPERTS, 2 * D_FF)) * 0.01,
        'expert_w2': nrm(ks[28], (DEPTH, N_EXPERTS, D_FF, D_MODEL)) * D_FF ** -0.5 * DEEPNORM_BETA,
        'expert_b2': nrm(ks[29], (DEPTH, N_EXPERTS, D_MODEL)) * 0.01,
        'ln2_g': 1.0 + 0.01 * nrm(ks[30], (DEPTH, D_MODEL)),
        'ln2_b': nrm(ks[31], (DEPTH, D_MODEL)) * 0.01,
    }


def reference(x_prompt, x_sample, cache_k, cache_v, page_table, state_conv, w_in, b_in, lambda_q1, lambda_k1,
              lambda_q2, lambda_k2, subln_g, rel_bias, w_attn_proj, conv_w, conv_b, conv_ln_g, conv_ln_b,
              w_conv_proj, b_conv_proj, w_out, ln1_g, ln1_b, router_w, router_b, expert_w1, expert_b1,
              expert_w2, expert_b2, ln2_g, ln2_b):
    n_pages = page_table.shape[1]
    past = n_pages * PAGE_SIZE
    hp, hs = x_prompt, x_sample
    bp, bs = hp.shape[0], hs.shape[0]
    kp_l, vp_l, cp_l, ks_l, vs_l, cs_l = [], [], [], [], [], []
    for l in range(DEPTH):
        p = dict(w_in=w_in[l], b_in=b_in[l], lambda_q1=lambda_q1[l], lambda_k1=lambda_k1[l],
                 lambda_q2=lambda_q2[l], lambda_k2=lambda_k2[l], subln_g=subln_g[l], rel_bias=rel_bias,
                 w_attn_proj=w_attn_proj[l], conv_w=conv_w[l], conv_b=conv_b[l], conv_ln_g=conv_ln_g[l],
                 conv_ln_b=conv_ln_b[l], w_conv_proj=w_conv_proj[l], b_conv_proj=b_conv_proj[l], w_out=w_out[l],
                 ln1_g=ln1_g[l], ln1_b=ln1_b[l], router_w=router_w[l], router_b=router_b[l],
                 expert_w1=expert_w1[l], expert_b1=expert_b1[l], expert_w2=expert_w2[l], expert_b2=expert_b2[l],
                 ln2_g=ln2_g[l], ln2_b=ln2_b[l])
        lam0 = lambda_init(l)
        hp, kp, vp, cp = decoder_layer(hp, jnp.zeros((bp, 0, N_HEADS, QK_DIM), hp.dtype),
                                       jnp.zeros((bp, 0, N_HEADS, V_DIM), hp.dtype),
                                       jnp.zeros((bp, CONV_WIDTH - 1, CONV_DIM), hp.dtype), p, lam0)
        k_past = cache_k[l][page_table].reshape(bs, past, N_HEADS, QK_DIM)
        v_past = cache_v[l][page_table].reshape(bs, past, N_HEADS, V_DIM)
        hs, ksn, vsn, csn = decoder_layer(hs, k_past, v_past, state_conv[l], p, lam0)
        kp_l.append(kp); vp_l.append(vp); cp_l.append(cp)
        ks_l.append(ksn); vs_l.append(vsn); cs_l.append(csn)
    return (hp, hs, jnp.stack(kp_l), jnp.stack(vp_l), jnp.stack(cp_l), jnp.stack(ks_l), jnp.stack(vs_l), jnp.stack(cs_l))
```

```python
import math
import os
import numpy as np
from contextlib import ExitStack
import concourse.bass as bass
import concourse.mybir as mybir
from concourse.bass_utils import run_bass_kernel_spmd

F32 = mybir.dt.float32
BF16 = mybir.dt.bfloat16
I32 = mybir.dt.int32
AF = mybir.ActivationFunctionType
ALU = mybir.AluOpType
AX = mybir.AxisListType

NCORES = 8
D = 1024
L = 4096
NBLK = 32
NQ = 16
TOK = 2048
SS = 16
ST = 64
NTILE = 17
QOFF, KOFF, VOFF, AOFF, GOFF, GAOFF = 0, 512, 1024, 1536, 2048, 2560
ALPHA = float(2.0 ** 0.25)
LAM0 = 0.8 - 0.6 * math.exp(0.0)
EPS = 1e-5
NEG = -30000.0
NPOOL = int(os.environ.get('KPOOL', '2560'))
KSTOP = int(os.environ.get('KSTOP', '99'))
HALVES = (list(range(0, 9)), list(range(9, 17)))


class Sched:
    def __init__(self, nc, es, n_dma_sems=32):
        self.nc = nc
        self.eng = {"pe": nc.tensor, "act": nc.scalar, "dve": nc.vector, "pool": nc.gpsimd, "sp": nc.sync}
        self.sem = {k: es.enter_context(nc.semaphore("s_" + k)) for k in self.eng}
        self.cnt = {k: 0 for k in self.eng}
        self.dsem = [es.enter_context(nc.semaphore("d%d" % i)) for i in range(n_dma_sems)]
        self.dcnt = [0] * n_dma_sems
        self.dnext = 0
        self.known = {k: {} for k in self.eng}
        self.lastw = {}
        self.readers = {}

    def _wait(self, e, tok):
        kind, key, val = tok
        if kind == "e" and key == e and (e == "pe" or val > self.cnt[e]):
            return
        if self.known[e].get((kind, key), 0) >= val:
            return
        self.known[e][(kind, key)] = val
        s = self.sem[key] if kind == "e" else self.dsem[key]
        self.eng[e].wait_ge(s, val)

    def _deps(self, e, reads, writes):
        toks = []
        for r in reads:
            if r in self.lastw:
                toks.append(self.lastw[r])
        for w in writes:
            if w in self.lastw:
                toks.append(self.lastw[w])
            toks.extend(self.readers.get(w, []))
        for t in toks:
            self._wait(e, t)

    def _record(self, tok, reads, writes):
        for r in reads:
            self.readers.setdefault(r, []).append(tok)
        for w in writes:
            self.lastw[w] = tok
            self.readers[w] = []

    def op(self, e, fn, reads=(), writes=(), inc=True):
        self._deps(e, reads, writes)
        ins = fn()
        tok = ("e", e, self.cnt[e] + 1)
        if inc:
            self.cnt[e] += 1
            ins.then_inc(self.sem[e], 1)
        self._record(tok, reads, writes)
        return ins

    def dma(self, q, fn, reads=(), writes=()):
        i = self.dnext
        self.dnext = (self.dnext + 1) % len(self.dsem)
        if self.dcnt[i] > 0:
            self._wait(q, ("d", i, self.dcnt[i]))
        self._deps(q, reads, writes)
        ins = fn()
        self.dcnt[i] += 16
        ins.then_inc(self.dsem[i], 16)
        self._record(("d", i, self.dcnt[i]), reads, writes)
        return ins

    def barrier(self):
        for e in self.eng:
            for f in self.eng:
                if f != e and self.cnt[f] > 0:
                    self._wait(e, ("e", f, self.cnt[f]))
            for i, c in enumerate(self.dcnt):
                if c > 0:
                    self._wait(e, ("d", i, c))
        self.lastw.clear()
        self.readers.clear()

    def finish(self, e="sp"):
        for f in self.eng:
            if f != e and self.cnt[f] > 0:
                self._wait(e, ("e", f, self.cnt[f]))
        for i, c in enumerate(self.dcnt):
            if c > 0:
                self._wait(e, ("d", i, c))


class _Stop(Exception):
    pass


def build_program():
    nc = bass.Bass("TRN2", target_bir_lowering=False)
    try:
        _build_body(nc)
    except _Stop:
        pass
    return nc


def _build_body(nc):
    din = lambda n, s, dt=F32: nc.dram_tensor(n, s, dt, kind="ExternalInput").ap()
    dout = lambda n, s: nc.dram_tensor(n, s, F32, kind="ExternalOutput").ap()
    dscr = lambda n, s, dt=F32: nc.dram_tensor(n, s, dt, kind="Internal").ap()
    xloc = din("xloc", [L, D]); xs = din("xs", [ST, D])
    ck = din("ck", [NPOOL * 128, 512]); cv = din("cv", [NPOOL * 128, 512])
    ptab = din("ptab", [SS * 16], I32); sconv = din("sconv", [SS, 30, 512])
    iot = din("iot", [128, 1]); vmk_d = din("vmk", [128, 1]); hm_d = din("hm", [128, 1])
    braw_d = din("braw", [128, 5 * 4 * 128]); bmask_d = din("bmask", [128, 5 * 4 * 128])
    w_in = din("w_in", [D, 4608]); b_in = din("b_in", [4608])
    lq1 = din("lq1", [64]); lk1 = din("lk1", [64]); lq2 = din("lq2", [64]); lk2 = din("lk2", [64])
    subg = din("subg", [128]); rb31_d = din("rb31", [4])
    wap_d = din("wap", [512, D]); convw = din("convw", [31, 512]); convb = din("convb", [512])
    clng = din("clng", [512]); clnb = din("clnb", [512]); wcp_d = din("wcp", [512, D]); bcp_d = din("bcp", [D])
    wout_d = din("wout", [D, D]); ln1g = din("ln1g", [D]); ln1b = din("ln1b", [D])
    rw_d = din("rw", [D, 32]); rbias = din("rbias", [32])
    w1_d = din("w1", [32, D, 2048]); b1_d = din("b1", [32, 2048]); w2_d = din("w2", [32, D, D]); b2_d = din("b2", [32, D])
    ln2g = din("ln2g", [D]); ln2b = din("ln2b", [D])
    y_p = dout("y_p", [TOK, D]); y_s = dout("y_s", [ST, D])
    nk_p = dout("nk_p", [TOK, 512]); nv_p = dout("nv_p", [TOK, 512]); nc_p = dout("nc_p", [30, 512])
    nk_s = dout("nk_s", [ST, 512]); nv_s = dout("nv_s", [ST, 512]); nc_s = dout("nc_s", [SS, 30, 512])
    yacc_scr = dscr("yacc_scr", [NTILE, 128, D]); h1T_scr = dscr("h1T_scr", [128, 8, NTILE * 128], BF16)
    gates_scr = dscr("gates_scr", [NTILE, 128, 32])
    w_in_v = w_in.rearrange("(kc p) n -> p kc n", p=128)

    with ExitStack() as es0:
        _nm = {"i": 0}

        def _sbuf(stack, n, s, dt):
            _nm["i"] += 1
            return stack.enter_context(nc.sbuf_tensor("sb%d_%s" % (_nm["i"], n), s, dt))
        sb0 = lambda n, s, dt=F32: _sbuf(es0, n, s, dt)
        pb = [es0.enter_context(nc.psum_tensor("pb%d" % i, [128, 512], F32)) for i in range(8)]
        S = Sched(nc, es0)
        st = {"bank": 0, "acc": 0, "u": 0}

        def bank():
            i = st["bank"]
            st["bank"] = (i + 1) % 6
            return pb[i], "pb%d" % i

        def accbank():
            i = 6 + st["acc"]
            st["acc"] = (st["acc"] + 1) % 2
            return pb[i], "pb%d" % i

        def uniq(p):
            st["u"] += 1
            return "%s_%d" % (p, st["u"])

        V = lambda fn, **kw: S.op("dve", fn, **kw)
        A = lambda fn, **kw: S.op("act", fn, **kw)
        G = lambda fn, **kw: S.op("pool", fn, **kw)
        P = lambda fn, **kw: S.op("pe", fn, **kw)

        def stop_here(k):
            if KSTOP == k:
                S.finish("sp")
                raise _Stop()
        ld = lambda fn, **kw: S.dma("sp", fn, **kw)
        ldc = lambda fn, **kw: S.dma("pool", fn, **kw)

        def mm(out, okey, pairs, reads):
            n = len(pairs)
            for i, (l, r) in enumerate(pairs):
                P(lambda: nc.tensor.matmul(out, lhsT=l, rhs=r, start=(i == 0), stop=(i == n - 1)),
                  reads=reads, writes=[okey], inc=(i == n - 1))

        ident = sb0("ident", [128, 128]); identb = sb0("identb", [128, 128], BF16)
        onesM = sb0("onesM", [128, 128]); epsc = sb0("epsc", [128, 1])
        binT = sb0("binT", [128, 36]); bk_bc = sb0("bk_bc", [128, 512]); bv_bc = sb0("bv_bc", [128, 512])
        cwT = sb0("cwT", [128, 4, 32]); pT12 = sb0("pT12", [128, 12])
        lamc = sb0("lamc", [128, 4]); subg_bc = sb0("subg_bc", [128, 128]); rb31 = sb0("rb31", [128, 4])
        vmk = sb0("vmk", [128, 1]); hm = sb0("hm", [128, 1]); iotc = sb0("iotc", [128, 1])
        idx = sb0("idx", [128, SS * 16], I32)
        G(lambda: nc.gpsimd.memset(ident[:], 0.0), writes=["ident"])
        G(lambda: nc.gpsimd.affine_select(out=ident[:], in_=ident[:], pattern=[[-1, 128]], compare_op=ALU.not_equal,
                                          fill=1.0, base=0, channel_multiplier=1), reads=["ident"], writes=["ident"])
        V(lambda: nc.vector.tensor_copy(out=identb[:], in_=ident[:]), reads=["ident"], writes=["identb"])
        V(lambda: nc.vector.memset(onesM[:], 1.0 / 512.0), writes=["onesM"])
        V(lambda: nc.vector.memset(epsc[:], EPS), writes=["epsc"])
        ld(lambda: nc.sync.dma_start(out=bk_bc[:], in_=b_in[KOFF:KOFF + 512].partition_broadcast(128)), writes=["bk_bc"])
        ld(lambda: nc.sync.dma_start(out=bv_bc[:], in_=b_in[VOFF:VOFF + 512].partition_broadcast(128)), writes=["bv_bc"])
        ld(lambda: nc.sync.dma_start(out=subg_bc[:], in_=subg.partition_broadcast(128)), writes=["subg_bc"])
        ld(lambda: nc.sync.dma_start(out=rb31[:], in_=rb31_d.partition_broadcast(128)), writes=["rb31"])
        ld(lambda: nc.sync.dma_start(out=vmk[:], in_=vmk_d), writes=["vmk"])
        ld(lambda: nc.sync.dma_start(out=hm[:], in_=hm_d), writes=["hm"])
        ld(lambda: nc.sync.dma_start(out=iotc[:], in_=iot), writes=["iotc"])
        V(lambda: nc.vector.tensor_scalar(out=subg_bc[:], in0=subg_bc[:], scalar1=1.0 - LAM0, scalar2=None, op0=ALU.mult),
          reads=["subg_bc"], writes=["subg_bc"])
        with ExitStack() as est:
            sbt = lambda n, s, dt=F32: _sbuf(est, n, s, dt)
            brow = sbt("brow", [36, 128]); crow = sbt("crow", [31, 512]); prow = sbt("prow", [12, 128])
            lqt = sbt("lqt", [128, 4, 64]); ptb = sbt("ptb", [128, SS * 16], I32); ptf = sbt("ptf", [128, SS * 16])
            ld(lambda: nc.sync.dma_start(out=brow[:], in_=b_in.rearrange("(c p) -> c p", p=128)), writes=["brow"])
            ld(lambda: nc.sync.dma_start(out=crow[:], in_=convw), writes=["crow"])
            for i, src in enumerate((convb, clng, clnb)):
                ld(lambda: nc.sync.dma_start(out=prow[4 * i:4 * i + 4, :], in_=src.rearrange("(c p) -> c p", p=128)), writes=["prow%d" % i])
            for i, src in enumerate((lq1, lk1, lq2, lk2)):
                ld(lambda: nc.sync.dma_start(out=lqt[:, i, :], in_=src.partition_broadcast(128)), writes=["lqt%d" % i])
            ld(lambda: nc.sync.dma_start(out=ptb[:], in_=ptab.partition_broadcast(128)), writes=["ptb"])
            b, bkey = bank()
            P(lambda: nc.tensor.transpose(out=b[:, 0:36], in_=brow[0:36, :], identity=ident[0:36, 0:36]), reads=["brow", "ident"], writes=[bkey])
            V(lambda: nc.vector.tensor_copy(out=binT[:], in_=b[:, 0:36]), reads=[bkey], writes=["binT"])
            b, bkey = bank()
            for cc in range(4):
                P(lambda: nc.tensor.transpose(out=b[:, cc * 32:cc * 32 + 31], in_=crow[0:31, cc * 128:(cc + 1) * 128],
                                              identity=ident[0:31, 0:31]), reads=["crow", "ident"], writes=[bkey], inc=(cc == 3))
            V(lambda: nc.vector.memset(cwT[:], 0.0), writes=["cwT"])
            V(lambda: nc.vector.tensor_copy(out=cwT[:, :, 0:31], in_=b[:, 0:128].rearrange("p (c w) -> p c w", w=32)[:, :, 0:31]),
              reads=[bkey], writes=["cwT"])
            b, bkey = bank()
            P(lambda: nc.tensor.transpose(out=b[:, 0:12], in_=prow[0:12, :], identity=ident[0:12, 0:12]),
              reads=["prow0", "prow1", "prow2", "ident"], writes=[bkey])
            V(lambda: nc.vector.tensor_copy(out=pT12[:], in_=b[:, 0:12]), reads=[bkey], writes=["pT12"])
            for i in range(2):
                V(lambda: nc.vector.tensor_tensor(out=lqt[:, 2 * i, :], in0=lqt[:, 2 * i, :], in1=lqt[:, 2 * i + 1, :], op=ALU.mult),
                  reads=["lqt%d" % (2 * i), "lqt%d" % (2 * i + 1)], writes=["lqt%d" % (2 * i)])
                V(lambda: nc.vector.reduce_sum(out=lamc[:, i:i + 1], in_=lqt[:, 2 * i, :], axis=AX.X), reads=["lqt%d" % (2 * i)], writes=["lam%d" % i])
                A(lambda: nc.scalar.activation(out=lamc[:, i:i + 1], in_=lamc[:, i:i + 1], func=AF.Exp), reads=["lam%d" % i], writes=["lam%d" % i])
            V(lambda: nc.vector.tensor_tensor(out=lamc[:, 2:3], in0=lamc[:, 0:1], in1=lamc[:, 1:2], op=ALU.subtract), reads=["lam0", "lam1"], writes=["lam2"])
            V(lambda: nc.vector.tensor_scalar(out=lamc[:, 3:4], in0=lamc[:, 2:3], scalar1=LAM0, scalar2=-1.0, op0=ALU.add, op1=ALU.mult),
              reads=["lam2"], writes=["nlam"])
            V(lambda: nc.vector.tensor_copy(out=ptf[:], in_=ptb[:]), reads=["ptb"], writes=["ptf"])
            V(lambda: nc.vector.tensor_scalar(out=ptf[:], in0=ptf[:], scalar1=128.0, scalar2=iotc[:, 0:1], op0=ALU.mult, op1=ALU.add),
              reads=["ptf", "iotc"], writes=["ptf"])
            V(lambda: nc.vector.tensor_copy(out=idx[:], in_=ptf[:]), reads=["ptf"], writes=["idx"])
            S.barrier()
        if KSTOP == 0:
            S.finish("sp")
            raise _Stop()
        cbT = pT12[:, 0:4]; lgT = pT12[:, 4:8]; lbT = pT12[:, 8:12]
        nlam = lamc[:, 3:4]

        ld(lambda: nc.sync.dma_start(out=nc_s[:, 0:26, :], in_=sconv[:, 4:30, :]), writes=["nc_s_a"])

        def ln_rstd(var_ap, out_ap, tmp_ap, keys_r, key_w):
            A(lambda: nc.scalar.activation(out=tmp_ap, in_=var_ap, func=AF.Sqrt, bias=epsc[0:var_ap.shape[0], 0:1], scale=1.0),
              reads=keys_r + ["epsc"], writes=[key_w + "_sd"])
            V(lambda: nc.vector.reciprocal(out=out_ap, in_=tmp_ap), reads=[key_w + "_sd"], writes=[key_w])

        def transpose_rows(src_tile, skey, rows, dst, dkeyp, dt_engine_pair=("act", "dve")):
            keys = []
            skeys = list(skey) if isinstance(skey, (list, tuple)) else [skey]
            for half in range(2):
                b, bkey = bank()
                for q in range(4):
                    kc = half * 4 + q
                    P(lambda: nc.tensor.transpose(out=b[:, q * 128:q * 128 + rows], in_=src_tile[0:rows, kc * 128:(kc + 1) * 128],
                                                  identity=ident[0:rows, 0:rows]), reads=skeys + ["ident"], writes=[bkey], inc=(q == 3))
                sv = b[:, :].rearrange("p (q r) -> p q r", r=128)[:, :, 0:rows]
                src_ap, src_key = sv, bkey
                for di, (d_, dk) in enumerate(zip(dst, dkeyp)):
                    dv = d_[:, half * 4:half * 4 + 4, 0:rows]
                    k = dk + "h%d" % half
                    if di > 0:
                        G(lambda: nc.gpsimd.tensor_copy(out=dv, in_=src_ap), reads=[src_key], writes=[k])
                    elif half == 0:
                        A(lambda: nc.scalar.copy(out=dv, in_=src_ap), reads=[src_key], writes=[k])
                    else:
                        V(lambda: nc.vector.tensor_copy(out=dv, in_=src_ap), reads=[src_key], writes=[k])
                    keys.append(k)
                    if di == 0:
                        src_ap, src_key = dv, k
            return keys

        with ExitStack() as esP:
            sbP = lambda n, s, dt=F32: _sbuf(esP, n, s, dt)
            sT = sbP("sT", [128, 4, TOK + ST], BF16)
            oT = sbP("oT", [128, 4, TOK + ST], BF16)
            bthi = sbP("bthi", [128, 5, 4, 128], BF16); btlo = sbP("btlo", [128, 5, 4, 128], BF16)
            with ExitStack() as est:
                sbt = lambda n, s, dt=F32: _sbuf(est, n, s, dt)
                braw = sbt("braw", [128, 5, 4, 128]); bmsk = sbt("bmsk", [128, 5, 4, 128]); bt32 = sbt("bt32", [128, 5, 4, 128])
                ld(lambda: nc.sync.dma_start(out=braw[:].rearrange("p a b c -> p (a b c)"), in_=braw_d), writes=["braw"])
                ld(lambda: nc.sync.dma_start(out=bmsk[:].rearrange("p a b c -> p (a b c)"), in_=bmask_d), writes=["bmsk"])
                for h in range(4):
                    V(lambda: nc.vector.tensor_scalar(out=bt32[:, :, h, :], in0=braw[:, :, h, :], scalar1=rb31[:, h:h + 1], scalar2=8.0,
                                                      op0=ALU.subtract, op1=ALU.mult), reads=["braw", "rb31"], writes=["bt32"])
                V(lambda: nc.vector.tensor_tensor(out=bt32[:], in0=bt32[:], in1=bmsk[:], op=ALU.add), reads=["bt32", "bmsk"], writes=["bt32"])
                V(lambda: nc.vector.tensor_copy(out=bthi[:], in_=bt32[:]), reads=["bt32"], writes=["bthi"])
                V(lambda: nc.vector.tensor_copy(out=braw[:], in_=bthi[:]), reads=["bthi"], writes=["braw"])
                V(lambda: nc.vector.tensor_tensor(out=bt32[:], in0=bt32[:], in1=braw[:], op=ALU.subtract), reads=["bt32", "braw"], writes=["bt32"])
                V(lambda: nc.vector.tensor_copy(out=btlo[:], in_=bt32[:]), reads=["bt32"], writes=["btlo"])
                S.barrier()

            def subln_store(o_ap, okey, rows, dst_ap, dkey, scr):
                sq, ss, sd, rs = scr
                V(lambda: nc.vector.tensor_tensor(out=sq[0:rows, :], in0=o_ap, in1=o_ap, op=ALU.mult), reads=[okey], writes=["sl_sq"])
                V(lambda: nc.vector.reduce_sum(out=ss[0:rows, :], in_=sq[0:rows, :], axis=AX.X), reads=["sl_sq"], writes=["sl_ss"])
                A(lambda: nc.scalar.activation(out=sd[0:rows, :], in_=ss[0:rows, :], func=AF.Sqrt, bias=epsc[0:rows, 0:1], scale=1.0 / 128.0),
                  reads=["sl_ss", "epsc"], writes=["sl_sd"])
                V(lambda: nc.vector.reciprocal(out=rs[0:rows, :], in_=sd[0:rows, :]), reads=["sl_sd"], writes=["sl_rs"])
                V(lambda: nc.vector.scalar_tensor_tensor(out=dst_ap, in0=o_ap, scalar=rs[0:rows, 0:1], in1=subg_bc[0:rows, :],
                                                         op0=ALU.mult, op1=ALU.mult), reads=[okey, "sl_rs", "subg_bc"], writes=[dkey])

            def combine(acc, akey, rows, osb, okey, scr2):
                r0, r1 = scr2
                V(lambda: nc.vector.reciprocal(out=r0[0:rows, :], in_=acc[0:rows, 0, 128:129]), reads=[akey], writes=["cb_r0"])
                V(lambda: nc.vector.reciprocal(out=r1[0:rows, :], in_=acc[0:rows, 1, 128:129]), reads=[akey], writes=["cb_r1"])
                V(lambda: nc.vector.tensor_tensor(out=r1[0:rows, :], in0=r1[0:rows, :], in1=nlam[0:rows, :], op=ALU.mult), reads=["cb_r1", "nlam"], writes=["cb_r1"])
                V(lambda: nc.vector.tensor_scalar(out=osb, in0=acc[0:rows, 0, 0:128], scalar1=r0[0:rows, 0:1], scalar2=None, op0=ALU.mult),
                  reads=[akey, "cb_r0"], writes=[okey])
                V(lambda: nc.vector.scalar_tensor_tensor(out=osb, in0=acc[0:rows, 1, 0:128], scalar=r1[0:rows, 0:1], in1=osb, op0=ALU.mult, op1=ALU.add),
                  reads=[akey, "cb_r1", okey], writes=[okey])

            with ExitStack() as esKV:
                sbK = lambda n, s, dt=F32: _sbuf(esKV, n, s, dt)
                KT = sbK("KT", [128, 4, L], BF16)
                VA = sbK("VA", [128, NBLK, 4, 130], BF16)
                V(lambda: nc.vector.memset(VA[:, :, :, 128:129], 1.0), writes=["VAones"])
                V(lambda: nc.vector.memset(VA[:, :, :, 129:130], 0.0), writes=["VAz"])
                with ExitStack() as es1:
                    sb1 = lambda n, s, dt=F32: _sbuf(es1, n, s, dt)
                    wk = sb1("wk", [128, 8, 512], BF16); wv = sb1("wv", [128, 8, 512], BF16); wc = sb1("wc", [128, 8, 1024], BF16)
                    xb = [sb1("xb%d" % i, [128, D]) for i in range(2)]
                    xT1 = [sb1("xT1_%d" % i, [128, 8, 512], BF16) for i in range(1)]
                    ub = sb1("ub", [128, 4, 608])
                    sig = sb1("sig", [128, 512])
                    tk = [sb1("tk%d" % i, [128, 512]) for i in range(4)]
                    ycv = sb1("ycv", [128, 4, 256]); ysq = sb1("ysq", [128, 4, 256])
                    mnb = sb1("mnb", [128, 256]); m2 = sb1("m2", [128, 256]); var = sb1("var", [128, 256]); rstd = sb1("rstd", [128, 256])
                    tt = sb1("tt", [128, 256]); nct = sb1("nct", [30, 512]); sdt = sb1("sdt", [128, 256])
                    ldc(lambda: nc.gpsimd.dma_start(out=wk[:], in_=w_in_v[:, :, KOFF:KOFF + 512]), writes=["wk"])
                    ldc(lambda: nc.gpsimd.dma_start(out=wv[:], in_=w_in_v[:, :, VOFF:VOFF + 512]), writes=["wv"])
                    ldc(lambda: nc.gpsimd.dma_start(out=wc[:], in_=w_in_v[:, :, AOFF:AOFF + 1024]), writes=["wc"])
                    V(lambda: nc.vector.memset(ub[:], 0.0), writes=["ub"])
                    tki = 0
                    for c in range(8):
                        xTc, xk = xT1[0], "xT1_0"
                        xkeys = []
                        for b_ in range(4):
                            blk = 4 * c + b_
                            xbi, xbk = xb[blk % 2], "xb%d" % (blk % 2)
                            ld(lambda: nc.sync.dma_start(out=xbi[:], in_=xloc[blk * 128:(blk + 1) * 128, :]), writes=[xbk])
                            for half in range(2):
                                bq, bqk = bank()
                                for q in range(4):
                                    kc = half * 4 + q
                                    P(lambda: nc.tensor.transpose(out=bq[:, q * 128:(q + 1) * 128], in_=xbi[:, kc * 128:(kc + 1) * 128], identity=ident[:]),
                                      reads=[xbk, "ident"], writes=[bqk], inc=(q == 3))
                                dv = xTc[:, half * 4:half * 4 + 4, b_ * 128:(b_ + 1) * 128]
                                sv = bq[:, :].rearrange("p (q r) -> p q r", r=128)
                                k = xk + "_%d_%d" % (b_, half)
                                if half == 0:
                                    A(lambda: nc.scalar.copy(out=dv, in_=sv), reads=[bqk], writes=[k])
                                else:
                                    V(lambda: nc.vector.tensor_copy(out=dv, in_=sv), reads=[bqk], writes=[k])
                                xkeys.append(k)
                        for h in range(4):
                            bq, bqk = bank()
                            mm(bq[:, :], bqk, [(wk[:, kc, h * 128:(h + 1) * 128], xTc[:, kc, :]) for kc in range(8)], xkeys + ["wk"])
                            A(lambda: nc.scalar.activation(out=KT[:, h, c * 512:(c + 1) * 512], in_=bq[:, :], func=AF.Identity,
                                                           bias=binT[:, 4 + h:5 + h], scale=1.0), reads=[bqk, "binT"], writes=[uniq("KT")])
                        for b_ in range(4):
                            blk = 4 * c + b_
                            xr = [xk + "_%d_0" % b_, xk + "_%d_1" % b_]
                            for which in range(2):
                                w_, wkey, bias_, dst = ((wk, "wk", bk_bc, nk_p), (wv, "wv", bv_bc, nv_p))[which]
                                bq, bqk = bank()
                                mm(bq[:, :], bqk, [(xTc[:, kc, b_ * 128:(b_ + 1) * 128], w_[:, kc, :]) for kc in range(8)], xr + [wkey])
                                t_, tkey = tk[tki % 4], "tk%d" % (tki % 4)
                                tki += 1
                                V(lambda: nc.vector.tensor_tensor(out=t_[:], in0=bq[:, :], in1=bias_[:], op=ALU.add), reads=[bqk, "bk_bc", "bv_bc"], writes=[tkey])
                                ld(lambda: nc.sync.dma_start(out=dst[blk * 64:(blk + 1) * 64, :], in_=t_[64:128, :]), reads=[tkey], writes=[uniq("okv")])
                                if which == 1:
                                    vak = "VA%d" % blk
                                    A(lambda: nc.scalar.copy(out=VA[:, blk, :, 0:128], in_=t_[:].rearrange("p (h v) -> p h v", v=128)),
                                      reads=[tkey, "VAones", "VAz"], writes=[vak])
                                    if blk == 0:
                                        V(lambda: nc.vector.tensor_scalar(out=VA[:, 0, :, :], in0=VA[:, 0, :, :], scalar1=vmk[:, 0:1], scalar2=None, op0=ALU.mult),
                                          reads=[vak, "vmk", "VAones", "VAz"], writes=[vak])
                        if c > 0:
                            A(lambda: nc.scalar.copy(out=ub[:, :, 0:32], in_=ub[:, :, 512:544]), reads=["ub"], writes=["ub"])
                        for cc in range(4):
                            ba_, bak = bank()
                            mm(ba_[:, :], bak, [(wc[:, kc, cc * 128:(cc + 1) * 128], xTc[:, kc, :]) for kc in range(8)], xkeys + ["wc"])
                            bg_, bgk = bank()
                            mm(bg_[:, :], bgk, [(wc[:, kc, 512 + cc * 128:512 + (cc + 1) * 128], xTc[:, kc, :]) for kc in range(8)], xkeys + ["wc"])
                            A(lambda: nc.scalar.activation(out=sig[:], in_=bg_[:, :], func=AF.Sigmoid, bias=binT[:, 16 + cc:17 + cc], scale=1.0),
                              reads=[bgk, "binT"], writes=["sig"])
                            V(lambda: nc.vector.scalar_tensor_tensor(out=ub[:, cc, 32:544], in0=ba_[:, :], scalar=binT[:, 12 + cc:13 + cc], in1=sig[:],
                                                                     op0=ALU.add, op1=ALU.mult), reads=[bak, "sig", "binT"], writes=["ub"])
                        if c == 0:
                            V(lambda: nc.vector.tensor_scalar(out=ub[:, :, 32:96], in0=ub[:, :, 32:96], scalar1=hm[:, 0:1], scalar2=None, op0=ALU.mult),
                              reads=["ub", "hm"], writes=["ub"])
                        for cc in range(4):
                            v66 = ub[:, cc, 66:578].rearrange("p (b x) -> p b x", x=128)
                            yv = ycv[:, cc, :].rearrange("p (b t) -> p b t", t=64)
                            V(lambda: nc.vector.tensor_scalar(out=yv, in0=v66[:, :, 0:64], scalar1=cwT[:, cc, 0:1], scalar2=cbT[:, cc:cc + 1],
                                                              op0=ALU.mult, op1=ALU.add), reads=["ub", "cwT", "pT12"], writes=["ycv%d" % cc])
                            for w in range(1, 31):
                                V(lambda: nc.vector.scalar_tensor_tensor(out=yv, in0=v66[:, :, w:w + 64], scalar=cwT[:, cc, w:w + 1], in1=yv,
                                                                         op0=ALU.mult, op1=ALU.add), reads=["ub", "ycv%d" % cc], writes=["ycv%d" % cc])
                        ykeys = ["ycv%d" % cc for cc in range(4)]
                        G(lambda: nc.gpsimd.tensor_tensor(out=ysq[:], in0=ycv[:], in1=ycv[:], op=ALU.mult), reads=ykeys, writes=["ysq"])
                        bm, bmk = bank()
                        mm(bm[:, 0:256], bmk, [(onesM[:], ycv[:, cc, :]) for cc in range(4)], ykeys + ["onesM"])
                        bs, bsk = bank()
                        mm(bs[:, 0:256], bsk, [(onesM[:], ysq[:, cc, :]) for cc in range(4)], ["ysq", "onesM"])
                        A(lambda: nc.scalar.copy(out=mnb[:], in_=bm[:, 0:256]), reads=[bmk], writes=["mnb"])
                        G(lambda: nc.gpsimd.tensor_tensor(out=m2[:], in0=mnb[:], in1=mnb[:], op=ALU.mult), reads=["mnb"], writes=["m2"])
                        V(lambda: nc.vector.tensor_tensor(out=var[:], in0=bs[:, 0:256], in1=m2[:], op=ALU.subtract), reads=[bsk, "m2"], writes=["var"])
                        ln_rstd(var[:], rstd[:], sdt[:], ["var"], "rstd")
                        for cc in range(4):
                            V(lambda: nc.vector.tensor_tensor(out=tt[:], in0=ycv[:, cc, :], in1=mnb[:], op=ALU.subtract), reads=["ycv%d" % cc, "mnb"], writes=["tt"])
                            G(lambda: nc.gpsimd.tensor_tensor(out=tt[:], in0=tt[:], in1=rstd[:], op=ALU.mult), reads=["tt", "rstd"], writes=["tt"])
                            A(lambda: nc.scalar.activation(out=sT[:, cc, c * 256:(c + 1) * 256], in_=tt[:], func=AF.Silu, bias=lbT[:, cc:cc + 1], scale=lgT[:, cc:cc + 1]),
                              reads=["tt", "pT12"], writes=[uniq("sT")])
                        if c == 7:
                            bq, bqk = bank()
                            for cc in range(4):
                                P(lambda: nc.tensor.transpose(out=bq[0:30, cc * 128:(cc + 1) * 128], in_=ub[:, cc, 514:544], identity=ident[:]),
                                  reads=["ub", "ident"], writes=[bqk], inc=(cc == 3))
                            V(lambda: nc.vector.tensor_copy(out=nct[:], in_=bq[0:30, :]), reads=[bqk], writes=["nct"])
                            ld(lambda: nc.sync.dma_start(out=nc_p[:, :], in_=nct[:]), reads=["nct"], writes=["nc_p"])
                    S.barrier()
                if KSTOP == 1:
                    S.finish("sp")
                    raise _Stop()
                with ExitStack() as es2:
                    sb2 = lambda n, s, dt=F32: _sbuf(es2, n, s, dt)
                    wq = sb2("wq", [128, 8, 512], BF16)
                    xo = [sb2("xo%d" % i, [128, D]) for i in range(2)]
                    xTo = [sb2("xTo%d" % i, [128, 8, 128], BF16) for i in range(2)]
                    qT = [sb2("qT%d" % i, [128, 4, 128], BF16) for i in range(2)]
                    PT = [sb2("PT%d" % i, [128, 4, 128], BF16) for i in range(3)]
                    otok = [sb2("otok%d" % i, [128, 4, 128]) for i in range(2)]
                    osb = sb2("osb", [128, 128])
                    scr = (sb2("sl_sq", [128, 128]), sb2("sl_ss", [128, 1]), sb2("sl_sd", [128, 1]), sb2("sl_rs", [128, 1]))
                    scr2 = (sb2("cb_r0", [128, 1]), sb2("cb_r1", [128, 1]))
                    ldc(lambda: nc.gpsimd.dma_start(out=wq[:], in_=w_in_v[:, :, QOFF:QOFF + 512]), writes=["wq"])
                    pti = 0
                    for I in range(NQ):
                        xoi, xok = xo[I % 2], "xo%d" % (I % 2)
                        for hh in range(2):
                            r0 = (2 * I + hh) * 128 + 64
                            ld(lambda: nc.sync.dma_start(out=xoi[hh * 64:(hh + 1) * 64, :], in_=xloc[r0:r0 + 64, :]), writes=[xok])
                        xTi, xTk = xTo[I % 2], "xTo%d" % (I % 2)
                        xkeys = transpose_rows(xoi, xok, 128, [xTi], [xTk])
                        qTi, qk = qT[I % 2], "qT%d" % (I % 2)
                        bq, bqk = bank()
                        for h in range(4):
                            for kc in range(8):
                                P(lambda: nc.tensor.matmul(bq[:, h * 128:(h + 1) * 128], lhsT=wq[:, kc, h * 128:(h + 1) * 128], rhs=xTi[:, kc, :],
                                                           start=(kc == 0), stop=(kc == 7)), reads=xkeys + ["wq"], writes=[bqk], inc=(kc == 7 and h == 3))
                        for h in range(4):
                            A(lambda: nc.scalar.activation(out=qTi[:, h, :], in_=bq[:, h * 128:(h + 1) * 128], func=AF.Identity, bias=binT[:, h:h + 1], scale=1.0),
                              reads=[bqk, "binT"], writes=[qk])
                        oti, otk = otok[I % 2], "otok%d" % (I % 2)
                        nkb = 2 * I + 2
                        for h in range(4):
                            ab, abk = accbank()
                            acc = ab[:, 0:260].rearrange("p (m v) -> p m v", v=130)
                            for m in range(2):
                                ps_ = slice(m * 64, (m + 1) * 64)
                                for g0 in range(0, nkb, 4):
                                    grp = list(range(g0, min(g0 + 4, nkb)))
                                    sbk_, sbkk = bank()
                                    for j, kb in enumerate(grp):
                                        r = kb - (2 * I - 1)
                                        special = 0 <= r <= 2
                                        osl = sbk_[:, j * 128:(j + 1) * 128]
                                        last = (j == len(grp) - 1)
                                        P(lambda: nc.tensor.matmul(osl, lhsT=KT[ps_, h, kb * 128:(kb + 1) * 128], rhs=qTi[ps_, h, :], start=True, stop=not special),
                                          reads=[qk], writes=[sbkk], inc=(last and not special))
                                        if special:
                                            P(lambda: nc.tensor.matmul(osl, lhsT=identb[:], rhs=bthi[:, r, h, :], start=False, stop=False),
                                              reads=["identb"], writes=[sbkk], inc=False)
                                            P(lambda: nc.tensor.matmul(osl, lhsT=identb[:], rhs=btlo[:, r, h, :], start=False, stop=True),
                                              reads=[], writes=[sbkk], inc=last)
                                    n = len(grp)
                                    pt_, ptk = PT[pti % 3], "PT%d" % (pti % 3)
                                    pti += 1
                                    A(lambda: nc.scalar.activation(out=pt_[:, 0:n, :], in_=sbk_[:, 0:n * 128].rearrange("p (j q) -> p j q", q=128),
                                                                   func=AF.Exp, scale=0.125), reads=[sbkk], writes=[ptk])
                                    for j, kb in enumerate(grp):
                                        P(lambda: nc.tensor.matmul(acc[:, m, :], lhsT=pt_[:, j, :], rhs=VA[:, kb, h, :], start=(kb == 0), stop=(kb == nkb - 1)),
                                          reads=[ptk], writes=[abk], inc=(j == n - 1))
                            combine(acc, abk, 128, osb[:], "osb", scr2)
                            subln_store(osb[:], "osb", 128, oti[:, h, :], otk, scr)
                        bq, bqk = bank()
                        for h in range(4):
                            P(lambda: nc.tensor.transpose(out=bq[:, h * 128:(h + 1) * 128], in_=oti[:, h, :], identity=ident[:]),
                              reads=[otk, "ident"], writes=[bqk], inc=(h == 3))
                        A(lambda: nc.scalar.copy(out=oT[:, :, I * 128:(I + 1) * 128], in_=bq[:, :].rearrange("p (h q) -> p h q", q=128)),
                          reads=[bqk], writes=[uniq("oT")])
                    S.barrier()
                if KSTOP == 2:
                    S.finish("sp")
                    raise _Stop()
            with ExitStack() as esS:
                sbS = lambda n, s, dt=F32: _sbuf(esS, n, s, dt)
                wq = sbS("wq", [128, 8, 512], BF16); wk = sbS("wk", [128, 8, 512], BF16)
                wv = sbS("wv", [128, 8, 512], BF16); wc = sbS("wc", [128, 8, 1024], BF16)
                xss = sbS("xss", [128, D]); xTs = sbS("xTs", [128, 8, 128], BF16)
                qTs = sbS("qTs", [128, 4, ST], BF16); kTs = sbS("kTs", [128, 4, ST], BF16)
                tks = sbS("tks", [128, 512]); vnew = sbS("vnew", [4, SS, 4, 130], BF16)
                uT = sbS("uT", [128, 4, ST]); sig = sbS("sig", [128, ST]); utok = sbS("utok", [ST, 512])
                cbuf = sbS("cbuf", [128, 4, SS, 34]); scv = [sbS("scv%d" % i, [120, 512]) for i in range(2)]
                ycs = sbS("ycs", [128, 4, ST]); ysqs = sbS("ysqs", [128, 4, ST])
                mnb = sbS("mnb", [128, ST]); m2 = sbS("m2", [128, ST]); var = sbS("var", [128, ST]); rstd = sbS("rstd", [128, ST]); tt = sbS("tt", [128, ST]); sdt = sbS("sdt", [128, ST])
                kpg = [sbS("kpg%d" % i, [128, 512]) for i in range(4)]
                vpg = [sbS("vpg%d" % i, [128, 512], BF16) for i in range(4)]
                KTs = sbS("KTs", [128, 4, 16, 128], BF16); VAs = sbS("VAs", [128, 16, 4, 130], BF16)
                PTs = [sbS("PTs%d" % i, [128, 16, 4], BF16) for i in range(2)]
                PTn = [sbS("PTn%d" % i, [4, 4], BF16) for i in range(2)]
                os_ = sbS("os_", [4, 128]); otoks = sbS("otoks", [4, 4, 128])
                scr = (sbS("sl_sq", [128, 128]), sbS("sl_ss", [128, 1]), sbS("sl_sd", [128, 1]), sbS("sl_rs", [128, 1]))
                scr2 = (sbS("cb_r0", [128, 1]), sbS("cb_r1", [128, 1]))
                for wt, off, n_, nm in ((wq, QOFF, 512, "wq"), (wk, KOFF, 512, "wk"), (wv, VOFF, 512, "wv"), (wc, AOFF, 1024, "wc")):
                    ldc(lambda: nc.gpsimd.dma_start(out=wt[:], in_=w_in_v[:, :, off:off + n_]), writes=[nm])
                V(lambda: nc.vector.memset(VAs[:, :, :, 128:129], 1.0), writes=["VAs1"])
                V(lambda: nc.vector.memset(VAs[:, :, :, 129:130], 0.0), writes=["VAs0"])
                V(lambda: nc.vector.memset(vnew[:, :, :, 128:129], 1.0), writes=["vn1"])
                V(lambda: nc.vector.memset(vnew[:, :, :, 129:130], 0.0), writes=["vn0"])
                ld(lambda: nc.sync.dma_start(out=xss[0:ST, :], in_=xs[:, :]), writes=["xss"])
                xkeys = transpose_rows(xss, "xss", ST, [xTs], ["xTs"])
                xc = xTs
                for (w_, wkey, dstT, dk, c0) in ((wq, "wq", qTs, "qTs", 0), (wk, "wk", kTs, "kTs", 4)):
                    bq, bqk = bank()
                    for h in range(4):
                        for kc in range(8):
                            P(lambda: nc.tensor.matmul(bq[:, h * ST:(h + 1) * ST], lhsT=w_[:, kc, h * 128:(h + 1) * 128], rhs=xc[:, kc, 0:ST],
                                                       start=(kc == 0), stop=(kc == 7)), reads=xkeys + [wkey], writes=[bqk], inc=(kc == 7 and h == 3))
                    for h in range(4):
                        A(lambda: nc.scalar.activation(out=dstT[:, h, :], in_=bq[:, h * ST:(h + 1) * ST], func=AF.Identity, bias=binT[:, c0 + h:c0 + h + 1], scale=1.0),
                          reads=[bqk, "binT"], writes=[dk])
                for (w_, wkey, bias_, dst) in ((wk, "wk", bk_bc, nk_s), (wv, "wv", bv_bc, nv_s)):
                    bq, bqk = bank()
                    mm(bq[0:ST, :], bqk, [(xc[:, kc, 0:ST], w_[:, kc, :]) for kc in range(8)], xkeys + [wkey])
                    V(lambda: nc.vector.tensor_tensor(out=tks[0:ST, :], in0=bq[0:ST, :], in1=bias_[0:ST, :], op=ALU.add), reads=[bqk, "bk_bc", "bv_bc"], writes=["tks"])
                    ld(lambda: nc.sync.dma_start(out=dst[:, :], in_=tks[0:ST, :]), reads=["tks"], writes=[uniq("oks")])
                for s in range(SS):
                    bq, bqk = bank()
                    mm(bq[0:4, :], bqk, [(xc[:, kc, 4 * s:4 * s + 4], wv[:, kc, :]) for kc in range(8)], xkeys + ["wv"])
                    V(lambda: nc.vector.tensor_tensor(out=vnew[0:4, s, :, 0:128], in0=bq[0:4, :].rearrange("p (h v) -> p h v", v=128),
                                                      in1=bv_bc[0:4, :].rearrange("p (h v) -> p h v", v=128), op=ALU.add),
                      reads=[bqk, "bv_bc", "vn1", "vn0"], writes=["vnew"])
                for cc in range(4):
                    ba_, bak = bank()
                    mm(ba_[:, 0:ST], bak, [(wc[:, kc, cc * 128:(cc + 1) * 128], xc[:, kc, 0:ST]) for kc in range(8)], xkeys + ["wc"])
                    bg_, bgk = bank()
                    mm(bg_[:, 0:ST], bgk, [(wc[:, kc, 512 + cc * 128:512 + (cc + 1) * 128], xc[:, kc, 0:ST]) for kc in range(8)], xkeys + ["wc"])
                    A(lambda: nc.scalar.activation(out=sig[:], in_=bg_[:, 0:ST], func=AF.Sigmoid, bias=binT[:, 16 + cc:17 + cc], scale=1.0), reads=[bgk, "binT"], writes=["sig"])
                    V(lambda: nc.vector.scalar_tensor_tensor(out=uT[:, cc, :], in0=ba_[:, 0:ST], scalar=binT[:, 12 + cc:13 + cc], in1=sig[:], op0=ALU.add, op1=ALU.mult),
                      reads=[bak, "sig", "binT"], writes=["uT"])
                bq, bqk = bank()
                for cc in range(4):
                    P(lambda: nc.tensor.transpose(out=bq[0:ST, cc * 128:(cc + 1) * 128], in_=uT[:, cc, :], identity=ident[:]), reads=["uT", "ident"], writes=[bqk], inc=(cc == 3))
                V(lambda: nc.vector.tensor_copy(out=utok[:], in_=bq[0:ST, :]), reads=[bqk], writes=["utok"])
                for s in range(SS):
                    ld(lambda: nc.sync.dma_start(out=nc_s[s, 26:30, :], in_=utok[4 * s:4 * s + 4, :]), reads=["utok"], writes=["nc_s_b%d" % s])
                for g in range(4):
                    sc, sck = scv[g % 2], "scv%d" % (g % 2)
                    ld(lambda: nc.sync.dma_start(out=sc[:], in_=sconv[4 * g:4 * g + 4].rearrange("s r c -> (s r) c")), writes=[sck])
                    for cc in range(4):
                        bq, bqk = bank()
                        P(lambda: nc.tensor.transpose(out=bq[:, 0:120], in_=sc[0:120, cc * 128:(cc + 1) * 128], identity=ident[0:120, 0:120]), reads=[sck, "ident"], writes=[bqk])
                        A(lambda: nc.scalar.copy(out=cbuf[:, cc, 4 * g:4 * g + 4, 0:30], in_=bq[:, 0:120].rearrange("p (s r) -> p s r", r=30)), reads=[bqk], writes=[uniq("cbuf")])
                S.barrier()
                V(lambda: nc.vector.tensor_copy(out=cbuf[:, :, :, 30:34], in_=uT[:].rearrange("p c (s t) -> p c s t", t=4)), reads=[], writes=["cbuf"])
                for cc in range(4):
                    yv = ycs[:, cc, :].rearrange("p (s t) -> p s t", t=4)
                    V(lambda: nc.vector.tensor_scalar(out=yv, in0=cbuf[:, cc, :, 0:4], scalar1=cwT[:, cc, 0:1], scalar2=cbT[:, cc:cc + 1], op0=ALU.mult, op1=ALU.add),
                      reads=["cbuf"], writes=["ycs%d" % cc])
                    for w in range(1, 31):
                        V(lambda: nc.vector.scalar_tensor_tensor(out=yv, in0=cbuf[:, cc, :, w:w + 4], scalar=cwT[:, cc, w:w + 1], in1=yv, op0=ALU.mult, op1=ALU.add),
                          reads=["cbuf", "ycs%d" % cc], writes=["ycs%d" % cc])
                ykeys = ["ycs%d" % cc for cc in range(4)]
                G(lambda: nc.gpsimd.tensor_tensor(out=ysqs[:], in0=ycs[:], in1=ycs[:], op=ALU.mult), reads=ykeys, writes=["ysqs"])
                bm, bmk = bank()
                mm(bm[:, 0:ST], bmk, [(onesM[:], ycs[:, cc, :]) for cc in range(4)], ykeys)
                bs, bsk = bank()
                mm(bs[:, 0:ST], bsk, [(onesM[:], ysqs[:, cc, :]) for cc in range(4)], ["ysqs"])
                A(lambda: nc.scalar.copy(out=mnb[:], in_=bm[:, 0:ST]), reads=[bmk], writes=["mnb"])
                G(lambda: nc.gpsimd.tensor_tensor(out=m2[:], in0=mnb[:], in1=mnb[:], op=ALU.mult), reads=["mnb"], writes=["m2"])
                V(lambda: nc.vector.tensor_tensor(out=var[:], in0=bs[:, 0:ST], in1=m2[:], op=ALU.subtract), reads=[bsk, "m2"], writes=["var"])
                ln_rstd(var[:], rstd[:], sdt[:], ["var"], "rstd")
                for cc in range(4):
                    V(lambda: nc.vector.tensor_tensor(out=tt[:], in0=ycs[:, cc, :], in1=mnb[:], op=ALU.subtract), reads=["ycs%d" % cc, "mnb"], writes=["tt"])
                    G(lambda: nc.gpsimd.tensor_tensor(out=tt[:], in0=tt[:], in1=rstd[:], op=ALU.mult), reads=["tt", "rstd"], writes=["tt"])
                    A(lambda: nc.scalar.activation(out=sT[:, cc, TOK:TOK + ST], in_=tt[:], func=AF.Silu, bias=lbT[:, cc:cc + 1], scale=lgT[:, cc:cc + 1]),
                      reads=["tt"], writes=[uniq("sTs")])
                kpi = 0
                for s in range(SS):
                    for pg in range(16):
                        col = s * 16 + pg
                        kp, kpk = kpg[kpi % 4], "kpg%d" % (kpi % 4)
                        kpi += 1
                        ldc(lambda: nc.gpsimd.indirect_dma_start(out=kp[:, :], out_offset=None, in_=ck, in_offset=bass.IndirectOffsetOnAxis(ap=idx[:, col:col + 1], axis=0)),
                            reads=["idx"], writes=[kpk])
                        vp, vpk = vpg[(kpi - 1) % 4], "vpg%d" % ((kpi - 1) % 4)
                        ldc(lambda: nc.gpsimd.indirect_dma_start(out=vp[:, :], out_offset=None, in_=cv, in_offset=bass.IndirectOffsetOnAxis(ap=idx[:, col:col + 1], axis=0)),
                            reads=["idx"], writes=[vpk])
                        G(lambda: nc.gpsimd.tensor_copy(out=VAs[:, pg, :, 0:128], in_=vp[:, :].rearrange("p (h v) -> p h v", v=128)),
                          reads=[vpk, "VAs1", "VAs0"], writes=["VAs_%d" % pg])
                        bq, bqk = bank()
                        for h in range(4):
                            P(lambda: nc.tensor.transpose(out=bq[:, h * 128:(h + 1) * 128], in_=kp[:, h * 128:(h + 1) * 128], identity=ident[:]),
                              reads=[kpk, "ident"], writes=[bqk], inc=(h == 3))
                        cpk = "KTs_%d" % pg
                        if pg % 2 == 0:
                            A(lambda: nc.scalar.copy(out=KTs[:, :, pg, :], in_=bq[:, :].rearrange("p (h k) -> p h k", k=128)), reads=[bqk], writes=[cpk])
                        else:
                            V(lambda: nc.vector.tensor_copy(out=KTs[:, :, pg, :], in_=bq[:, :].rearrange("p (h k) -> p h k", k=128)), reads=[bqk], writes=[cpk])
                    ktkeys = ["KTs_%d" % pg for pg in range(16)]
                    vakeys = ["VAs_%d" % pg for pg in range(16)]
                    qs = slice(4 * s, 4 * s + 4)
                    for h in range(4):
                        ab, abk = accbank()
                        acc = ab[:, 0:260].rearrange("p (m v) -> p m v", v=130)
                        for m in range(2):
                            ps_ = slice(m * 64, (m + 1) * 64)
                            sbk_, sbkk = bank()
                            for pg in range(16):
                                osl = sbk_[:, pg * 4:(pg + 1) * 4]
                                sp_ = (pg == 15)
                                P(lambda: nc.tensor.matmul(osl, lhsT=KTs[ps_, h, pg, :], rhs=qTs[ps_, h, qs], start=True, stop=not sp_),
                                  reads=ktkeys + ["qTs"], writes=[sbkk], inc=False)
                                if sp_:
                                    P(lambda: nc.tensor.matmul(osl, lhsT=identb[:], rhs=bthi[:, 3, h, 0:4], start=False, stop=False), reads=[], writes=[sbkk], inc=False)
                                    P(lambda: nc.tensor.matmul(osl, lhsT=identb[:], rhs=btlo[:, 3, h, 0:4], start=False, stop=True), reads=[], writes=[sbkk], inc=False)
                            osn = sbk_[0:4, 64:68]
                            P(lambda: nc.tensor.matmul(osn, lhsT=kTs[ps_, h, qs], rhs=qTs[ps_, h, qs], start=True, stop=False), reads=["kTs", "qTs"], writes=[sbkk], inc=False)
                            P(lambda: nc.tensor.matmul(osn, lhsT=identb[0:4, 0:4], rhs=bthi[0:4, 4, h, 0:4], start=False, stop=False), reads=[], writes=[sbkk], inc=False)
                            P(lambda: nc.tensor.matmul(osn, lhsT=identb[0:4, 0:4], rhs=btlo[0:4, 4, h, 0:4], start=False, stop=True), reads=[], writes=[sbkk], inc=True)
                            pi = (2 * h + m) % 2
                            pt_, ptk = PTs[pi], "PTs%d" % pi
                            pn_, pnk = PTn[pi], "PTn%d" % pi
                            A(lambda: nc.scalar.activation(out=pt_[:], in_=sbk_[:, 0:64].rearrange("p (g q) -> p g q", q=4), func=AF.Exp, scale=0.125), reads=[sbkk], writes=[ptk])
                            A(lambda: nc.scalar.activation(out=pn_[:], in_=sbk_[0:4, 64:68], func=AF.Exp, scale=0.125), reads=[sbkk], writes=[pnk])
                            for pg in range(16):
                                P(lambda: nc.tensor.matmul(acc[0:4, m, :], lhsT=pt_[:, pg, :], rhs=VAs[:, pg, h, :], start=(pg == 0), stop=False),
                                  reads=[ptk] + vakeys, writes=[abk], inc=False)
                            P(lambda: nc.tensor.matmul(acc[0:4, m, :], lhsT=pn_[:], rhs=vnew[0:4, s, h, :], start=False, stop=True), reads=[pnk, "vnew"], writes=[abk], inc=True)
                        combine(acc, abk, 4, os_[:], "os_", scr2)
                        subln_store(os_[:], "os_", 4, otoks[0:4, h, :], "otoks", scr)
                    bq, bqk = bank()
                    for h in range(4):
                        P(lambda: nc.tensor.transpose(out=bq[:, h * 4:(h + 1) * 4], in_=otoks[0:4, h, :], identity=ident[0:4, 0:4]), reads=["otoks", "ident"], writes=[bqk], inc=(h == 3))
                    V(lambda: nc.vector.tensor_copy(out=oT[:, :, TOK + 4 * s:TOK + 4 * s + 4], in_=bq[:, 0:16].rearrange("p (h q) -> p h q", q=4)), reads=[bqk], writes=[uniq("oTs")])
                S.barrier()
                if KSTOP == 3:
                    S.finish("sp")
                    raise _Stop()
            with ExitStack() as es3:
                sb3 = lambda n, s, dt=F32: _sbuf(es3, n, s, dt)
                wg = sb3("wg", [128, 8, 2048], BF16); wap = sb3("wap", [128, 4, D], BF16); wcp = sb3("wcp", [128, 4, D], BF16)
                wout = sb3("wout", [128, 8, D], BF16)
                bgg = sb3("bgg", [128, 2048]); bcpb = sb3("bcpb", [128, D]); g1b = sb3("g1b", [128, D]); b1b = sb3("b1b", [128, D])
                rw = sb3("rw", [128, 8, 32]); rbb = sb3("rbb", [128, 32]); b2all = sb3("b2all", [32, D])
                xo = [sb3("xo%d" % i, [128, D]) for i in range(2)]
                xTo = sb3("xTo", [128, 8, 128], BF16)
                sgt = sb3("sgt", [128, 2048]); mt = sb3("mt", [128, D]); t2 = sb3("t2", [128, 512]); mT = sb3("mT", [128, 8, 128], BF16)
                pre = sb3("pre", [128, D]); h1 = sb3("h1", [128, D]); ya = sb3("ya", [128, D])
                h1Tf = sb3("h1Tf", [128, 8, 128]); h1Tb = sb3("h1Tb", [128, 8, 128], BF16)
                bn = sb3("bn", [128, 2, 6]); mv = sb3("mv", [128, 2]); sd = sb3("sd", [128, 1]); rs1 = sb3("rs1", [128, 1])
                lg = sb3("lg", [128, 32]); t8 = sb3("t8", [128, 8]); msk = sb3("msk", [128, 32]); nmx = sb3("nmx", [128, 1])
                ex = sb3("ex", [128, 32]); ssum = sb3("ssum", [128, 1]); gt = sb3("gt", [128, 32]); gtT = sb3("gtT", [32, 128])
                ldc(lambda: nc.gpsimd.dma_start(out=wg[:], in_=w_in_v[:, :, GAOFF:GAOFF + 2048]), writes=["wg"])
                ldc(lambda: nc.gpsimd.dma_start(out=wap[:], in_=wap_d.rearrange("(c p) n -> p c n", p=128)), writes=["wap"])
                ldc(lambda: nc.gpsimd.dma_start(out=wcp[:], in_=wcp_d.rearrange("(c p) n -> p c n", p=128)), writes=["wcp"])
                ldc(lambda: nc.gpsimd.dma_start(out=wout[:], in_=wout_d.rearrange("(c p) n -> p c n", p=128)), writes=["wout"])
                ld(lambda: nc.sync.dma_start(out=bgg[:], in_=b_in[GAOFF:GAOFF + 2048].partition_broadcast(128)), writes=["bgg"])
                ld(lambda: nc.sync.dma_start(out=bcpb[:], in_=bcp_d.partition_broadcast(128)), writes=["bcpb"])
                ld(lambda: nc.sync.dma_start(out=g1b[:], in_=ln1g.partition_broadcast(128)), writes=["g1b"])
                ld(lambda: nc.sync.dma_start(out=b1b[:], in_=ln1b.partition_broadcast(128)), writes=["b1b"])
                ld(lambda: nc.sync.dma_start(out=rw[:], in_=rw_d.rearrange("(c p) e -> p c e", p=128)), writes=["rw"])
                ld(lambda: nc.sync.dma_start(out=rbb[:], in_=rbias.partition_broadcast(128)), writes=["rbb"])
                ld(lambda: nc.sync.dma_start(out=b2all[:], in_=b2_d), writes=["b2all"])
                V(lambda: nc.vector.memset(h1Tb[:], 0.0), writes=["h1Tbh0", "h1Tbh1"])
                V(lambda: nc.vector.memset(h1Tf[:], 0.0), writes=["h1Tfh0", "h1Tfh1"])
                for I in range(NTILE):
                    rows = 128 if I < NQ else ST
                    R = slice(0, rows)
                    cols = slice(I * 128, I * 128 + rows)
                    xoi, xok = xo[I % 2], "xo%d" % (I % 2)
                    if I < NQ:
                        for hh in range(2):
                            r0 = (2 * I + hh) * 128 + 64
                            ld(lambda: nc.sync.dma_start(out=xoi[hh * 64:(hh + 1) * 64, :], in_=xloc[r0:r0 + 64, :]), writes=[xok])
                    else:
                        ld(lambda: nc.sync.dma_start(out=xoi[R, :], in_=xs[:, :]), writes=[xok])
                    xkeys = transpose_rows(xoi, xok, rows, [xTo], ["xTo"])
                    for p_ in range(4):
                        bq, bqk = bank()
                        mm(bq[R, :], bqk, [(xTo[:, kc, R], wg[:, kc, p_ * 512:(p_ + 1) * 512]) for kc in range(8)], xkeys + ["wg"])
                        V(lambda: nc.vector.tensor_tensor(out=sgt[R, p_ * 512:(p_ + 1) * 512], in0=bq[R, :], in1=bgg[R, p_ * 512:(p_ + 1) * 512], op=ALU.add),
                          reads=[bqk, "bgg"], writes=["sgt"])
                    A(lambda: nc.scalar.activation(out=sgt[R, :], in_=sgt[R, :], func=AF.Sigmoid), reads=["sgt"], writes=["sgt"])
                    stop_here(41)
                    for half in range(2):
                        hs = slice(half * 512, (half + 1) * 512)
                        bq, bqk = bank()
                        mm(bq[R, :], bqk, [(oT[:, h, cols], wap[:, h, hs]) for h in range(4)], ["wap"])
                        V(lambda: nc.vector.tensor_tensor(out=mt[R, hs], in0=bq[R, :], in1=sgt[R, hs], op=ALU.mult), reads=[bqk, "sgt"], writes=["mt%d" % half])
                        bq, bqk = bank()
                        mm(bq[R, :], bqk, [(sT[:, cc, cols], wcp[:, cc, hs]) for cc in range(4)], ["wcp"])
                        V(lambda: nc.vector.tensor_tensor(out=t2[R, :], in0=bq[R, :], in1=bcpb[R, hs], op=ALU.add), reads=[bqk, "bcpb"], writes=["t2"])
                        G(lambda: nc.gpsimd.tensor_tensor(out=t2[R, :], in0=t2[R, :], in1=sgt[R, 1024 + half * 512:1024 + (half + 1) * 512], op=ALU.mult),
                          reads=["t2", "sgt"], writes=["t2"])
                        G(lambda: nc.gpsimd.tensor_tensor(out=mt[R, hs], in0=mt[R, hs], in1=t2[R, :], op=ALU.add), reads=["t2", "mt%d" % half], writes=["mt%d" % half])
                    stop_here(42)
                    mkeys = transpose_rows(mt, ["mt0", "mt1"], rows, [mT], ["mT"])
                    for half in range(2):
                        hs = slice(half * 512, (half + 1) * 512)
                        bq, bqk = bank()
                        mm(bq[R, :], bqk, [(mT[:, kc, R], wout[:, kc, hs]) for kc in range(8)], mkeys + ["wout"])
                        V(lambda: nc.vector.scalar_tensor_tensor(out=pre[R, hs], in0=xoi[R, hs], scalar=ALPHA, in1=bq[R, :], op0=ALU.mult, op1=ALU.add),
                          reads=[bqk, xok], writes=["pre%d" % half])
                        V(lambda: nc.vector.bn_stats(out=bn[R, half, :], in_=pre[R, hs]), reads=["pre%d" % half], writes=["bn%d" % half])
                    V(lambda: nc.vector.bn_aggr(out=mv[R, :], in_=bn[R, :, :]), reads=["bn0", "bn1"], writes=["mv"])
                    ln_rstd(mv[R, 1:2], rs1[R, :], sd[R, :], ["mv"], "rs1")
                    V(lambda: nc.vector.tensor_scalar(out=h1[R, :], in0=pre[R, :], scalar1=mv[R, 0:1], scalar2=rs1[R, 0:1], op0=ALU.subtract, op1=ALU.mult),
                      reads=["pre0", "pre1", "mv", "rs1"], writes=["h1"])
                    G(lambda: nc.gpsimd.tensor_tensor(out=h1[R, :], in0=h1[R, :], in1=g1b[R, :], op=ALU.mult), reads=["h1", "g1b"], writes=["h1"])
                    G(lambda: nc.gpsimd.tensor_tensor(out=h1[R, :], in0=h1[R, :], in1=b1b[R, :], op=ALU.add), reads=["h1", "b1b"], writes=["h1"])
                    stop_here(43)
                    hkeys = transpose_rows(h1, "h1", rows, [h1Tf, h1Tb], ["h1Tf", "h1Tb"])
                    stop_here(44)
                    ld(lambda: nc.sync.dma_start(out=h1T_scr[:, :, I * 128:(I + 1) * 128], in_=h1Tb[:]), reads=["h1Tbh0", "h1Tbh1"], writes=[uniq("h1Ts")])
                    bq, bqk = bank()
                    mm(bq[R, 0:32], bqk, [(h1Tf[:, kc, R], rw[:, kc, :]) for kc in range(8)], ["h1Tfh0", "h1Tfh1", "rw"])
                    V(lambda: nc.vector.tensor_tensor(out=lg[R, :], in0=bq[R, 0:32], in1=rbb[R, :], op=ALU.add), reads=[bqk, "rbb"], writes=["lg"])
                    V(lambda: nc.vector.max(out=t8[R, :], in_=lg[R, :]), reads=["lg"], writes=["t8"])
                    V(lambda: nc.vector.tensor_scalar(out=msk[R, :], in0=lg[R, :], scalar1=t8[R, 3:4], scalar2=None, op0=ALU.is_ge), reads=["lg", "t8"], writes=["msk"])
                    V(lambda: nc.vector.tensor_scalar(out=nmx[R, :], in0=t8[R, 0:1], scalar1=-1.0, scalar2=None, op0=ALU.mult), reads=["t8"], writes=["nmx"])
                    A(lambda: nc.scalar.activation(out=ex[R, :], in_=lg[R, :], func=AF.Exp, bias=nmx[R, 0:1], scale=1.0), reads=["lg", "nmx"], writes=["ex"])
                    V(lambda: nc.vector.tensor_tensor(out=ex[R, :], in0=ex[R, :], in1=msk[R, :], op=ALU.mult), reads=["ex", "msk"], writes=["ex"])
                    V(lambda: nc.vector.reduce_sum(out=ssum[R, :], in_=ex[R, :], axis=AX.X), reads=["ex"], writes=["ssum"])
                    V(lambda: nc.vector.reciprocal(out=ssum[R, :], in_=ssum[R, :]), reads=["ssum"], writes=["ssum"])
                    V(lambda: nc.vector.tensor_scalar(out=gt[R, :], in0=ex[R, :], scalar1=ssum[R, 0:1], scalar2=None, op0=ALU.mult), reads=["ex", "ssum"], writes=["gt"])
                    stop_here(45)
                    ld(lambda: nc.sync.dma_start(out=gates_scr[I, R, :], in_=gt[R, :]), reads=["gt"], writes=[uniq("gts")])
                    bq, bqk = bank()
                    P(lambda: nc.tensor.transpose(out=bq[0:32, 0:rows], in_=gt[R, :], identity=ident[R, R]), reads=["gt", "ident"], writes=[bqk])
                    A(lambda: nc.scalar.copy(out=gtT[:, R], in_=bq[0:32, 0:rows]), reads=[bqk], writes=["gtT"])
                    for half in range(2):
                        hs = slice(half * 512, (half + 1) * 512)
                        bq, bqk = bank()
                        mm(bq[R, :], bqk, [(gtT[:, R], b2all[:, hs])], ["gtT", "b2all"])
                        V(lambda: nc.vector.scalar_tensor_tensor(out=ya[R, hs], in0=h1[R, hs], scalar=ALPHA, in1=bq[R, :], op0=ALU.mult, op1=ALU.add),
                          reads=[bqk, "h1"], writes=["ya%d" % half])
                    ld(lambda: nc.sync.dma_start(out=yacc_scr[I, R, :], in_=ya[R, :]), reads=["ya0", "ya1"], writes=[uniq("yas")])
                    stop_here(46)
                    if I == 15:
                        stop_here(47)
                S.barrier()
                if KSTOP == 4:
                    S.finish("sp")
                    raise _Stop()
        with ExitStack() as esB:
            sbB = lambda n, s, dt=F32: _sbuf(esB, n, s, dt)
            b1T = sbB("b1T", [128, 16, 32])
            with ExitStack() as est:
                b1rows = _sbuf(est, "b1rows", [32, 2048], F32)
                ld(lambda: nc.sync.dma_start(out=b1rows[:], in_=b1_d), writes=["b1rows"])
                bq, bqk = bank()
                for c_ in range(16):
                    P(lambda: nc.tensor.transpose(out=bq[:, c_ * 32:(c_ + 1) * 32], in_=b1rows[0:32, c_ * 128:(c_ + 1) * 128], identity=ident[0:32, 0:32]),
                      reads=["b1rows", "ident"], writes=[bqk], inc=(c_ == 15))
                V(lambda: nc.vector.tensor_copy(out=b1T[:], in_=bq[:, :].rearrange("p (c e) -> p c e", e=32)), reads=[bqk], writes=["b1T"])
                S.barrier()
            w1t = [sbB("w1t%d" % i, [128, 8, 2048], BF16) for i in range(2)]
            w2t = [sbB("w2t%d" % i, [128, 8, D], BF16) for i in range(2)]
            h1Th = sbB("h1Th", [128, 8, 9 * 128], BF16); yacc = sbB("yacc", [128, 9, D]); gts = sbB("gts", [128, 9, 32])
            g2b = sbB("g2b", [128, D]); b2b = sbB("b2b", [128, D])
            g32 = [sbB("g32_%d" % i, [128, 512]) for i in range(2)]; sg32 = [sbB("sg32_%d" % i, [128, 512]) for i in range(2)]
            u32 = [sbB("u32_%d" % i, [128, 512]) for i in range(2)]
            actT = [sbB("actT%d" % i, [128, 8, 512], BF16) for i in range(2)]
            bn = sbB("bn", [128, 2, 6]); mv = sbB("mv", [128, 2]); sd = sbB("sd", [128, 1]); rs1 = sbB("rs1", [128, 1]); yo = [sbB("yo%d" % i, [128, D]) for i in range(2)]
            ld(lambda: nc.sync.dma_start(out=g2b[:], in_=ln2g.partition_broadcast(128)), writes=["g2b"])
            ld(lambda: nc.sync.dma_start(out=b2b[:], in_=ln2b.partition_broadcast(128)), writes=["b2b"])
            wi = 0
            ai = 0

            def load_expert(e_, slot):
                w1e_, w1k_ = w1t[slot % 2], "w1t%d" % (slot % 2)
                w2e_, w2k_ = w2t[slot % 2], "w2t%d" % (slot % 2)
                for q in range(4):
                    ldc(lambda: nc.gpsimd.dma_start(out=w1e_[:, :, q * 512:(q + 1) * 512], in_=w1_d[e_].rearrange("(c p) f -> p c f", p=128)[:, :, q * 512:(q + 1) * 512]),
                        writes=[w1k_ + "_%d" % q])
                for q in range(2):
                    ldc(lambda: nc.gpsimd.dma_start(out=w2e_[:, :, q * 512:(q + 1) * 512], in_=w2_d[e_].rearrange("(c p) f -> p c f", p=128)[:, :, q * 512:(q + 1) * 512]),
                        writes=[w2k_ + "_%d" % q])

            for hf, tiles in enumerate(HALVES):
                nt = len(tiles)
                ncols = nt * 128
                t0 = tiles[0]
                ld(lambda: nc.sync.dma_start(out=h1Th[:, :, 0:ncols], in_=h1T_scr[:, :, t0 * 128:t0 * 128 + ncols]), writes=["h1Th"])
                ld(lambda: nc.sync.dma_start(out=yacc[:, 0:nt, :], in_=yacc_scr[t0:t0 + nt].rearrange("t p d -> p t d")), writes=["yacc%d" % i for i in range(nt)])
                ld(lambda: nc.sync.dma_start(out=gts[:, 0:nt, :], in_=gates_scr[t0:t0 + nt].rearrange("t p e -> p t e")), writes=["gts"])
                groups = [(g0, min(512, ncols - g0)) for g0 in range(0, ncols, 512)]
                for e in range(32):
                    if wi == 0:
                        load_expert(0, 0)
                    w1e, w1k = w1t[wi % 2], "w1t%d" % (wi % 2)
                    w2e, w2k = w2t[wi % 2], "w2t%d" % (wi % 2)
                    wi += 1
                    if wi < 64:
                        load_expert(wi % 32, wi)
                    for (g0, n) in groups:
                        at, atk = actT[ai % 2], "actT%d" % (ai % 2)
                        ai += 1
                        akeys = []
                        for fc in range(8):
                            bi = (ai * 8 + fc) % 2
                            gg, ggk = g32[bi], "g32_%d" % bi
                            sgg, sgk = sg32[bi], "sg32_%d" % bi
                            uu, uuk = u32[bi], "u32_%d" % bi
                            bgq, bgk = bank()
                            mm(bgq[:, 0:n], bgk, [(w1e[:, kc, fc * 128:(fc + 1) * 128], h1Th[:, kc, g0:g0 + n]) for kc in range(8)], ["h1Th", w1k + "_%d" % (fc // 4)])
                            buq, buk = bank()
                            mm(buq[:, 0:n], buk, [(w1e[:, kc, 1024 + fc * 128:1024 + (fc + 1) * 128], h1Th[:, kc, g0:g0 + n]) for kc in range(8)],
                               ["h1Th", w1k + "_%d" % (2 + fc // 4)])
                            V(lambda: nc.vector.tensor_scalar(out=gg[:, 0:n], in0=bgq[:, 0:n], scalar1=b1T[:, fc, e:e + 1], scalar2=7.0, op0=ALU.add, op1=ALU.min),
                              reads=[bgk, "b1T"], writes=[ggk])
                            A(lambda: nc.scalar.activation(out=sgg[:, 0:n], in_=gg[:, 0:n], func=AF.Sigmoid, scale=1.702), reads=[ggk], writes=[sgk])
                            V(lambda: nc.vector.tensor_scalar(out=uu[:, 0:n], in0=buq[:, 0:n], scalar1=b1T[:, 8 + fc, e:e + 1], scalar2=7.0, op0=ALU.add, op1=ALU.min),
                              reads=[buk, "b1T"], writes=[uuk])
                            G(lambda: nc.gpsimd.tensor_scalar(out=uu[:, 0:n], in0=uu[:, 0:n], scalar1=-7.0, scalar2=1.0, op0=ALU.max, op1=ALU.add), reads=[uuk], writes=[uuk])
                            G(lambda: nc.gpsimd.tensor_tensor(out=uu[:, 0:n], in0=uu[:, 0:n], in1=gg[:, 0:n], op=ALU.mult), reads=[uuk, ggk], writes=[uuk])
                            k = atk + "_%d" % fc
                            G(lambda: nc.gpsimd.tensor_tensor(out=at[:, fc, 0:n], in0=uu[:, 0:n], in1=sgg[:, 0:n], op=ALU.mult), reads=[uuk, sgk], writes=[k])
                            akeys.append(k)
                        for tt_ in range(n // 128):
                            ti = g0 // 128 + tt_
                            for half in range(2):
                                hs = slice(half * 512, (half + 1) * 512)
                                bq, bqk = bank()
                                mm(bq[:, :], bqk, [(at[:, fc, tt_ * 128:(tt_ + 1) * 128], w2e[:, fc, hs]) for fc in range(8)], akeys + [w2k + "_%d" % half])
                                V(lambda: nc.vector.scalar_tensor_tensor(out=yacc[:, ti, hs], in0=bq[:, :], scalar=gts[:, ti, e:e + 1], in1=yacc[:, ti, hs],
                                                                         op0=ALU.mult, op1=ALU.add), reads=[bqk, "gts", "yacc%d" % ti], writes=["yacc%d" % ti])
                for ti, I in enumerate(tiles):
                    rows = 128 if I < NQ else ST
                    R = slice(0, rows)
                    yk = "yacc%d" % ti
                    for half in range(2):
                        V(lambda: nc.vector.bn_stats(out=bn[R, half, :], in_=yacc[R, ti, half * 512:(half + 1) * 512]), reads=[yk], writes=["bn%d" % half])
                    V(lambda: nc.vector.bn_aggr(out=mv[R, :], in_=bn[R, :, :]), reads=["bn0", "bn1"], writes=["mv"])
                    ln_rstd(mv[R, 1:2], rs1[R, :], sd[R, :], ["mv"], "rs1")
                    yoi, yok = yo[ti % 2], "yo%d" % (ti % 2)
                    V(lambda: nc.vector.tensor_scalar(out=yoi[R, :], in0=yacc[R, ti, :], scalar1=mv[R, 0:1], scalar2=rs1[R, 0:1], op0=ALU.subtract, op1=ALU.mult),
                      reads=[yk, "mv", "rs1"], writes=[yok])
                    G(lambda: nc.gpsimd.tensor_tensor(out=yoi[R, :], in0=yoi[R, :], in1=g2b[R, :], op=ALU.mult), reads=[yok, "g2b"], writes=[yok])
                    G(lambda: nc.gpsimd.tensor_tensor(out=yoi[R, :], in0=yoi[R, :], in1=b2b[R, :], op=ALU.add), reads=[yok, "b2b"], writes=[yok])
                    if I < NQ:
                        ld(lambda: nc.sync.dma_start(out=y_p[I * 128:(I + 1) * 128, :], in_=yoi[:, :]), reads=[yok], writes=[uniq("yout")])
                    else:
                        ld(lambda: nc.sync.dma_start(out=y_s[:, :], in_=yoi[R, :]), reads=[yok], writes=[uniq("yout")])
            S.finish("sp")


def _bucket_table():
    n = np.arange(0, 512)
    nf = np.maximum(n, 1).astype(np.float32)
    large = 16 + (np.log(nf / np.float32(16.0)) / np.float32(math.log(128 / 16)) * np.float32(16.0)).astype(np.int32)
    large = np.minimum(large, 31)
    return np.where(n < 16, n, large).astype(np.int64)


def _bias_tables(rel_bias):
    bt = _bucket_table()
    kk = np.arange(128)[:, None]
    cc = np.arange(128)[None, :]
    dist = np.zeros((5, 128, 128), np.int64)
    for r in range(3):
        d = np.where(cc < 64, 128 * (1 - r) + 64 + cc - kk, 128 * (2 - r) + cc - kk)
        dist[r] = d
    dist[3] = 128 + cc - kk
    dist[4] = cc - kk
    valid = dist >= 0
    valid[3][:, 4:] = True
    valid[4][4:, :] = True
    valid[4][:, 4:] = True
    dcl = np.clip(dist, 0, 511)
    braw = rel_bias[bt[dcl]]
    braw = np.where(valid[..., None], braw, 0.0).astype(np.float32)
    braw = np.ascontiguousarray(braw.transpose(1, 0, 3, 2)).reshape(128, 5 * 4 * 128)
    bmask = np.where(valid, 0.0, 8.0 * NEG).astype(np.float32)
    bmask = np.ascontiguousarray(np.broadcast_to(bmask[:, :, None, :], (5, 128, 4, 128)).transpose(1, 0, 2, 3)).reshape(128, 5 * 4 * 128)
    return braw, bmask


def kernel(x_prompt, x_sample, cache_k, cache_v, page_table, state_conv, w_in, b_in, lambda_q1, lambda_k1,
           lambda_q2, lambda_k2, subln_g, rel_bias, w_attn_proj, conv_w, conv_b, conv_ln_g, conv_ln_b,
           w_conv_proj, b_conv_proj, w_out, ln1_g, ln1_b, router_w, router_b, expert_w1, expert_b1,
           expert_w2, expert_b2, ln2_g, ln2_b):
    f = lambda a: np.ascontiguousarray(np.asarray(a, dtype=np.float32))
    x_prompt, x_sample, state_conv = f(x_prompt), f(x_sample), f(state_conv)
    rel_bias = f(rel_bias)
    braw, bmask = _bias_tables(rel_bias)
    shared = {
        "ck": f(cache_k).reshape(NPOOL * 128, 512), "cv": f(cache_v).reshape(NPOOL * 128, 512),
        "iot": np.arange(128, dtype=np.float32).reshape(128, 1), "braw": braw, "bmask": bmask,
        "w_in": f(w_in)[0], "b_in": f(b_in)[0],
        "lq1": f(lambda_q1)[0], "lk1": f(lambda_k1)[0], "lq2": f(lambda_q2)[0], "lk2": f(lambda_k2)[0],
        "subg": f(subln_g)[0], "rb31": np.ascontiguousarray(rel_bias[31]),
        "wap": f(w_attn_proj)[0], "convw": f(conv_w)[0], "convb": f(conv_b)[0], "clng": f(conv_ln_g)[0], "clnb": f(conv_ln_b)[0],
        "wcp": f(w_conv_proj)[0], "bcp": f(b_conv_proj)[0], "wout": f(w_out)[0], "ln1g": f(ln1_g)[0], "ln1b": f(ln1_b)[0],
        "rw": f(router_w)[0], "rbias": f(router_b)[0], "w1": f(expert_w1)[0], "b1": f(expert_b1)[0],
        "w2": f(expert_w2)[0], "b2": f(expert_b2)[0], "ln2g": f(ln2_g)[0], "ln2b": f(ln2_b)[0],
    }
    pt = np.ascontiguousarray(np.asarray(page_table, dtype=np.int32))
    nc = build_program()
    in_maps = []
    for c in range(NCORES):
        b, j = c // 2, c % 2
        if j == 1:
            xl = x_prompt[b]
        else:
            xl = np.concatenate([np.zeros((64, D), np.float32), x_prompt[b, :L - 64]], axis=0)
        one = np.ones((128, 1), np.float32)
        vm = one.copy()
        if j == 0:
            vm[:64] = 0.0
        m = dict(shared)
        m.update({
            "xloc": np.ascontiguousarray(xl),
            "xs": np.ascontiguousarray(x_sample[c * SS:(c + 1) * SS].reshape(ST, D)),
            "ptab": np.ascontiguousarray(pt[c * SS:(c + 1) * SS].reshape(-1)),
            "sconv": np.ascontiguousarray(state_conv[0, c * SS:(c + 1) * SS]),
            "vmk": vm, "hm": (one * float(j)).astype(np.float32),
        })
        in_maps.append(m)
    res = run_bass_kernel_spmd(nc, in_maps, core_ids=list(range(NCORES))).results
    B = x_prompt.shape[0]
    y_p = np.zeros((B, L, D), np.float32)
    nk_p = np.zeros((1, B, L, 4, 128), np.float32)
    nv_p = np.zeros((1, B, L, 4, 128), np.float32)
    nc_p = np.zeros((1, B, 30, 512), np.float32)
    y_s = np.zeros((128, 4, D), np.float32)
    nk_s = np.zeros((1, 128, 4, 4, 128), np.float32)
    nv_s = np.zeros((1, 128, 4, 4, 128), np.float32)
    nc_s = np.zeros((1, 128, 30, 512), np.float32)
    for c in range(NCORES):
        b, j = c // 2, c % 2
        r = res[c]
        rows = (np.arange(NBLK)[:, None] * 128 + 64 * j + np.arange(64)[None, :]).reshape(-1)
        y_p[b, rows] = r["y_p"]
        nk_p[0, b, rows] = r["nk_p"].reshape(TOK, 4, 128)
        nv_p[0, b, rows] = r["nv_p"].reshape(TOK, 4, 128)
        if j == 1:
            nc_p[0, b] = r["nc_p"]
        ss = slice(c * SS, (c + 1) * SS)
        y_s[ss] = r["y_s"].reshape(SS, 4, D)
        nk_s[0, ss] = r["nk_s"].reshape(SS, 4, 4, 128)
        nv_s[0, ss] = r["nv_s"].reshape(SS, 4, 4, 128)
        nc_s[0, ss] = r["nc_s"]
    return (y_p, y_s, nk_p, nv_p, nc_p, nk_s, nv_s, nc_s)
```

```python
import math
import os
import numpy as np
from contextlib import ExitStack
import concourse.bass as bass
import concourse.mybir as mybir
from concourse.bass_utils import run_bass_kernel_spmd

F32 = mybir.dt.float32
BF16 = mybir.dt.bfloat16
I32 = mybir.dt.int32
AF = mybir.ActivationFunctionType
ALU = mybir.AluOpType
AX = mybir.AxisListType

NCORES = 8
D = 1024
L = 4096
NBLK = 32
NQ = 16
TOK = 2048
SS = 16
ST = 64
NTILE = 17
QOFF, KOFF, VOFF, AOFF, GOFF, GAOFF = 0, 512, 1024, 1536, 2048, 2560
ALPHA = float(2.0 ** 0.25)
LAM0 = 0.8 - 0.6 * math.exp(0.0)
EPS = 1e-5
NEG = -30000.0
NPOOL = int(os.environ.get('KPOOL', '2560'))
KSTOP = int(os.environ.get('KSTOP', '99'))
HALVES = (list(range(0, 9)), list(range(9, 17)))


class Sched:
    def __init__(self, nc, es, n_dma_sems=32):
        self.nc = nc
        self.eng = {"pe": nc.tensor, "act": nc.scalar, "dve": nc.vector, "pool": nc.gpsimd, "sp": nc.sync}
        self.sem = {k: es.enter_context(nc.semaphore("s_" + k)) for k in self.eng}
        self.cnt = {k: 0 for k in self.eng}
        self.dsem = [es.enter_context(nc.semaphore("d%d" % i)) for i in range(n_dma_sems)]
        self.dcnt = [0] * n_dma_sems
        self.dnext = 0
        self.known = {k: {} for k in self.eng}
        self.lastw = {}
        self.readers = {}

    def _wait(self, e, tok):
        kind, key, val = tok
        if kind == "e" and key == e and (e == "pe" or val > self.cnt[e]):
            return
        if self.known[e].get((kind, key), 0) >= val:
            return
        self.known[e][(kind, key)] = val
        s = self.sem[key] if kind == "e" else self.dsem[key]
        self.eng[e].wait_ge(s, val)

    def _deps(self, e, reads, writes):
        toks = []
        for r in reads:
            if r in self.lastw:
                toks.append(self.lastw[r])
        for w in writes:
            if w in self.lastw:
                toks.append(self.lastw[w])
            toks.extend(self.readers.get(w, []))
        for t in toks:
            self._wait(e, t)

    def _record(self, tok, reads, writes):
        for r in reads:
            self.readers.setdefault(r, []).append(tok)
        for w in writes:
            self.lastw[w] = tok
            self.readers[w] = []

    def op(self, e, fn, reads=(), writes=(), inc=True):
        self._deps(e, reads, writes)
        ins = fn()
        tok = ("e", e, self.cnt[e] + 1)
        if inc:
            self.cnt[e] += 1
            ins.then_inc(self.sem[e], 1)
        self._record(tok, reads, writes)
        return ins

    MAX_OUTSTANDING = {"pool": 4, "sp": 8}

    def dma(self, q, fn, reads=(), writes=()):
        i = self.dnext
        self.dnext = (self.dnext + 1) % len(self.dsem)
        if self.dcnt[i] > 0:
            self._wait(q, ("d", i, self.dcnt[i]))
        hist = self.__dict__.setdefault("dhist", {}).setdefault(q, [])
        k = self.MAX_OUTSTANDING.get(q, 8)
        if len(hist) >= k:
            self._wait(q, hist[-k])
        self._deps(q, reads, writes)
        ins = fn()
        self.dcnt[i] += 16
        ins.then_inc(self.dsem[i], 16)
        tok = ("d", i, self.dcnt[i])
        hist.append(tok)
        self._record(tok, reads, writes)
        return ins

    def barrier(self):
        for e in self.eng:
            for f in self.eng:
                if f != e and self.cnt[f] > 0:
                    self._wait(e, ("e", f, self.cnt[f]))
            for i, c in enumerate(self.dcnt):
                if c > 0:
                    self._wait(e, ("d", i, c))
        self.lastw.clear()
        self.readers.clear()

    def finish(self, e="sp"):
        for f in self.eng:
            if f != e and self.cnt[f] > 0:
                self._wait(e, ("e", f, self.cnt[f]))
        for i, c in enumerate(self.dcnt):
            if c > 0:
                self._wait(e, ("d", i, c))


class _Stop(Exception):
    pass


def build_program():
    nc = bass.Bass("TRN2", target_bir_lowering=False)
    try:
        _build_body(nc)
    except _Stop:
        pass
    return nc


def _build_body(nc):
    din = lambda n, s, dt=F32: nc.dram_tensor(n, s, dt, kind="ExternalInput").ap()
    dout = lambda n, s: nc.dram_tensor(n, s, F32, kind="ExternalOutput").ap()
    dscr = lambda n, s, dt=F32: nc.dram_tensor(n, s, dt, kind="Internal").ap()
    xloc = din("xloc", [L, D]); xs = din("xs", [ST, D])
    ck = din("ck", [NPOOL * 128, 512]); cv = din("cv", [NPOOL * 128, 512])
    ptab = din("ptab", [SS * 16], I32); sconv = din("sconv", [SS, 30, 512])
    iot = din("iot", [128, 1]); vmk_d = din("vmk", [128, 1]); hm_d = din("hm", [128, 1])
    braw_d = din("braw", [128, 5 * 4 * 128]); bmask_d = din("bmask", [128, 5 * 4 * 128])
    w_in = din("w_in", [D, 4608]); b_in = din("b_in", [4608])
    lq1 = din("lq1", [64]); lk1 = din("lk1", [64]); lq2 = din("lq2", [64]); lk2 = din("lk2", [64])
    subg = din("subg", [128]); rb31_d = din("rb31", [4])
    wap_d = din("wap", [512, D]); convw = din("convw", [31, 512]); convb = din("convb", [512])
    clng = din("clng", [512]); clnb = din("clnb", [512]); wcp_d = din("wcp", [512, D]); bcp_d = din("bcp", [D])
    wout_d = din("wout", [D, D]); ln1g = din("ln1g", [D]); ln1b = din("ln1b", [D])
    rw_d = din("rw", [D, 32]); rbias = din("rbias", [32])
    w1_d = din("w1", [32, D, 2048]); b1_d = din("b1", [32, 2048]); w2_d = din("w2", [32, D, D]); b2_d = din("b2", [32, D])
    ln2g = din("ln2g", [D]); ln2b = din("ln2b", [D])
    y_p = dout("y_p", [TOK, D]); y_s = dout("y_s", [ST, D])
    nk_p = dout("nk_p", [TOK, 512]); nv_p = dout("nv_p", [TOK, 512]); nc_p = dout("nc_p", [30, 512])
    nk_s = dout("nk_s", [ST, 512]); nv_s = dout("nv_s", [ST, 512]); nc_s = dout("nc_s", [SS, 30, 512])
    yacc_scr = dscr("yacc_scr", [NTILE, 128, D]); h1T_scr = dscr("h1T_scr", [128, 8, NTILE * 128], BF16)
    gates_scr = dscr("gates_scr", [NTILE, 128, 32])
    w_in_v = w_in.rearrange("(kc p) n -> p kc n", p=128)

    with ExitStack() as es0:
        _nm = {"i": 0}

        def _sbuf(stack, n, s, dt):
            _nm["i"] += 1
            return stack.enter_context(nc.sbuf_tensor("sb%d_%s" % (_nm["i"], n), s, dt))
        sb0 = lambda n, s, dt=F32: _sbuf(es0, n, s, dt)
        pb = [es0.enter_context(nc.psum_tensor("pb%d" % i, [128, 512], F32)) for i in range(8)]
        S = Sched(nc, es0)
        st = {"bank": 0, "acc": 0, "u": 0}

        def bank():
            i = st["bank"]
            st["bank"] = (i + 1) % 6
            return pb[i], "pb%d" % i

        def accbank():
            i = 6 + st["acc"]
            st["acc"] = (st["acc"] + 1) % 2
            return pb[i], "pb%d" % i

        def uniq(p):
            st["u"] += 1
            return "%s_%d" % (p, st["u"])

        V = lambda fn, **kw: S.op("dve", fn, **kw)
        A = lambda fn, **kw: S.op("act", fn, **kw)
        G = lambda fn, **kw: S.op("pool", fn, **kw)
        P = lambda fn, **kw: S.op("pe", fn, **kw)

        def stop_here(k):
            if KSTOP == k:
                S.finish("sp")
                raise _Stop()
        ld = lambda fn, **kw: S.dma("sp", fn, **kw)
        ldc = lambda fn, **kw: S.dma("pool", fn, **kw)

        def mm(out, okey, pairs, reads):
            n = len(pairs)
            for i, (l, r) in enumerate(pairs):
                P(lambda: nc.tensor.matmul(out, lhsT=l, rhs=r, start=(i == 0), stop=(i == n - 1)),
                  reads=reads, writes=[okey], inc=(i == n - 1))

        ident = sb0("ident", [128, 128]); identb = sb0("identb", [128, 128], BF16)
        onesM = sb0("onesM", [128, 128]); epsc = sb0("epsc", [128, 1])
        binT = sb0("binT", [128, 36]); bk_bc = sb0("bk_bc", [128, 512]); bv_bc = sb0("bv_bc", [128, 512])
        cwT = sb0("cwT", [128, 4, 32]); pT12 = sb0("pT12", [128, 12])
        lamc = sb0("lamc", [128, 4]); subg_bc = sb0("subg_bc", [128, 128]); rb31 = sb0("rb31", [128, 4])
        vmk = sb0("vmk", [128, 1]); hm = sb0("hm", [128, 1]); iotc = sb0("iotc", [128, 1])
        idx = sb0("idx", [128, SS * 16], I32)
        G(lambda: nc.gpsimd.memset(ident[:], 0.0), writes=["ident"])
        G(lambda: nc.gpsimd.affine_select(out=ident[:], in_=ident[:], pattern=[[-1, 128]], compare_op=ALU.not_equal,
                                          fill=1.0, base=0, channel_multiplier=1), reads=["ident"], writes=["ident"])
        V(lambda: nc.vector.tensor_copy(out=identb[:], in_=ident[:]), reads=["ident"], writes=["identb"])
        V(lambda: nc.vector.memset(onesM[:], 1.0 / 512.0), writes=["onesM"])
        V(lambda: nc.vector.memset(epsc[:], EPS), writes=["epsc"])
        ld(lambda: nc.sync.dma_start(out=bk_bc[:], in_=b_in[KOFF:KOFF + 512].partition_broadcast(128)), writes=["bk_bc"])
        ld(lambda: nc.sync.dma_start(out=bv_bc[:], in_=b_in[VOFF:VOFF + 512].partition_broadcast(128)), writes=["bv_bc"])
        ld(lambda: nc.sync.dma_start(out=subg_bc[:], in_=subg.partition_broadcast(128)), writes=["subg_bc"])
        ld(lambda: nc.sync.dma_start(out=rb31[:], in_=rb31_d.partition_broadcast(128)), writes=["rb31"])
        ld(lambda: nc.sync.dma_start(out=vmk[:], in_=vmk_d), writes=["vmk"])
        ld(lambda: nc.sync.dma_start(out=hm[:], in_=hm_d), writes=["hm"])
        ld(lambda: nc.sync.dma_start(out=iotc[:], in_=iot), writes=["iotc"])
        V(lambda: nc.vector.tensor_scalar(out=subg_bc[:], in0=subg_bc[:], scalar1=1.0 - LAM0, scalar2=None, op0=ALU.mult),
          reads=["subg_bc"], writes=["subg_bc"])
        with ExitStack() as est:
            sbt = lambda n, s, dt=F32: _sbuf(est, n, s, dt)
            brow = sbt("brow", [36, 128]); crow = sbt("crow", [31, 512]); prow = sbt("prow", [12, 128])
            lqt = sbt("lqt", [128, 4, 64]); ptb = sbt("ptb", [128, SS * 16], I32); ptf = sbt("ptf", [128, SS * 16])
            ld(lambda: nc.sync.dma_start(out=brow[:], in_=b_in.rearrange("(c p) -> c p", p=128)), writes=["brow"])
            ld(lambda: nc.sync.dma_start(out=crow[:], in_=convw), writes=["crow"])
            for i, src in enumerate((convb, clng, clnb)):
                ld(lambda: nc.sync.dma_start(out=prow[4 * i:4 * i + 4, :], in_=src.rearrange("(c p) -> c p", p=128)), writes=["prow%d" % i])
            for i, src in enumerate((lq1, lk1, lq2, lk2)):
                ld(lambda: nc.sync.dma_start(out=lqt[:, i, :], in_=src.partition_broadcast(128)), writes=["lqt%d" % i])
            ld(lambda: nc.sync.dma_start(out=ptb[:], in_=ptab.partition_broadcast(128)), writes=["ptb"])
            b, bkey = bank()
            P(lambda: nc.tensor.transpose(out=b[:, 0:36], in_=brow[0:36, :], identity=ident[0:36, 0:36]), reads=["brow", "ident"], writes=[bkey])
            V(lambda: nc.vector.tensor_copy(out=binT[:], in_=b[:, 0:36]), reads=[bkey], writes=["binT"])
            b, bkey = bank()
            for cc in range(4):
                P(lambda: nc.tensor.transpose(out=b[:, cc * 32:cc * 32 + 31], in_=crow[0:31, cc * 128:(cc + 1) * 128],
                                              identity=ident[0:31, 0:31]), reads=["crow", "ident"], writes=[bkey], inc=(cc == 3))
            V(lambda: nc.vector.memset(cwT[:], 0.0), writes=["cwT"])
            V(lambda: nc.vector.tensor_copy(out=cwT[:, :, 0:31], in_=b[:, 0:128].rearrange("p (c w) -> p c w", w=32)[:, :, 0:31]),
              reads=[bkey], writes=["cwT"])
            b, bkey = bank()
            P(lambda: nc.tensor.transpose(out=b[:, 0:12], in_=prow[0:12, :], identity=ident[0:12, 0:12]),
              reads=["prow0", "prow1", "prow2", "ident"], writes=[bkey])
            V(lambda: nc.vector.tensor_copy(out=pT12[:], in_=b[:, 0:12]), reads=[bkey], writes=["pT12"])
            for i in range(2):
                V(lambda: nc.vector.tensor_tensor(out=lqt[:, 2 * i, :], in0=lqt[:, 2 * i, :], in1=lqt[:, 2 * i + 1, :], op=ALU.mult),
                  reads=["lqt%d" % (2 * i), "lqt%d" % (2 * i + 1)], writes=["lqt%d" % (2 * i)])
                V(lambda: nc.vector.reduce_sum(out=lamc[:, i:i + 1], in_=lqt[:, 2 * i, :], axis=AX.X), reads=["lqt%d" % (2 * i)], writes=["lam%d" % i])
                A(lambda: nc.scalar.activation(out=lamc[:, i:i + 1], in_=lamc[:, i:i + 1], func=AF.Exp), reads=["lam%d" % i], writes=["lam%d" % i])
            V(lambda: nc.vector.tensor_tensor(out=lamc[:, 2:3], in0=lamc[:, 0:1], in1=lamc[:, 1:2], op=ALU.subtract), reads=["lam0", "lam1"], writes=["lam2"])
            V(lambda: nc.vector.tensor_scalar(out=lamc[:, 3:4], in0=lamc[:, 2:3], scalar1=LAM0, scalar2=-1.0, op0=ALU.add, op1=ALU.mult),
              reads=["lam2"], writes=["nlam"])
            V(lambda: nc.vector.tensor_copy(out=ptf[:], in_=ptb[:]), reads=["ptb"], writes=["ptf"])
            V(lambda: nc.vector.tensor_scalar(out=ptf[:], in0=ptf[:], scalar1=128.0, scalar2=iotc[:, 0:1], op0=ALU.mult, op1=ALU.add),
              reads=["ptf", "iotc"], writes=["ptf"])
            V(lambda: nc.vector.tensor_copy(out=idx[:], in_=ptf[:]), reads=["ptf"], writes=["idx"])
            S.barrier()
        if KSTOP == 0:
            S.finish("sp")
            raise _Stop()
        cbT = pT12[:, 0:4]; lgT = pT12[:, 4:8]; lbT = pT12[:, 8:12]
        nlam = lamc[:, 3:4]

        ld(lambda: nc.sync.dma_start(out=nc_s[:, 0:26, :], in_=sconv[:, 4:30, :]), writes=["nc_s_a"])

        def ln_rstd(var_ap, out_ap, tmp_ap, keys_r, key_w):
            A(lambda: nc.scalar.activation(out=tmp_ap, in_=var_ap, func=AF.Sqrt, bias=epsc[0:var_ap.shape[0], 0:1], scale=1.0),
              reads=keys_r + ["epsc"], writes=[key_w + "_sd"])
            V(lambda: nc.vector.reciprocal(out=out_ap, in_=tmp_ap), reads=[key_w + "_sd"], writes=[key_w])

        def transpose_rows(src_tile, skey, rows, dst, dkeyp, dt_engine_pair=("act", "dve")):
            keys = []
            skeys = list(skey) if isinstance(skey, (list, tuple)) else [skey]
            for half in range(2):
                b, bkey = bank()
                for q in range(4):
                    kc = half * 4 + q
                    P(lambda: nc.tensor.transpose(out=b[:, q * 128:q * 128 + rows], in_=src_tile[0:rows, kc * 128:(kc + 1) * 128],
                                                  identity=ident[0:rows, 0:rows]), reads=skeys + ["ident"], writes=[bkey], inc=(q == 3))
                sv = b[:, :].rearrange("p (q r) -> p q r", r=128)[:, :, 0:rows]
                src_ap, src_key = sv, bkey
                for di, (d_, dk) in enumerate(zip(dst, dkeyp)):
                    dv = d_[:, half * 4:half * 4 + 4, 0:rows]
                    k = dk + "h%d" % half
                    if di > 0:
                        G(lambda: nc.gpsimd.tensor_copy(out=dv, in_=src_ap), reads=[src_key], writes=[k])
                    elif half == 0:
                        A(lambda: nc.scalar.copy(out=dv, in_=src_ap), reads=[src_key], writes=[k])
                    else:
                        V(lambda: nc.vector.tensor_copy(out=dv, in_=src_ap), reads=[src_key], writes=[k])
                    keys.append(k)
                    if di == 0:
                        src_ap, src_key = dv, k
            return keys

        with ExitStack() as esP:
            sbP = lambda n, s, dt=F32: _sbuf(esP, n, s, dt)
            sT = sbP("sT", [128, 4, TOK + ST], BF16)
            oT = sbP("oT", [128, 4, TOK + ST], BF16)
            bthi = sbP("bthi", [128, 5, 4, 128], BF16); btlo = sbP("btlo", [128, 5, 4, 128], BF16)
            with ExitStack() as est:
                sbt = lambda n, s, dt=F32: _sbuf(est, n, s, dt)
                braw = sbt("braw", [128, 5, 4, 128]); bmsk = sbt("bmsk", [128, 5, 4, 128]); bt32 = sbt("bt32", [128, 5, 4, 128])
                ld(lambda: nc.sync.dma_start(out=braw[:].rearrange("p a b c -> p (a b c)"), in_=braw_d), writes=["braw"])
                ld(lambda: nc.sync.dma_start(out=bmsk[:].rearrange("p a b c -> p (a b c)"), in_=bmask_d), writes=["bmsk"])
                for h in range(4):
                    V(lambda: nc.vector.tensor_scalar(out=bt32[:, :, h, :], in0=braw[:, :, h, :], scalar1=rb31[:, h:h + 1], scalar2=8.0,
                                                      op0=ALU.subtract, op1=ALU.mult), reads=["braw", "rb31"], writes=["bt32"])
                V(lambda: nc.vector.tensor_tensor(out=bt32[:], in0=bt32[:], in1=bmsk[:], op=ALU.add), reads=["bt32", "bmsk"], writes=["bt32"])
                V(lambda: nc.vector.tensor_copy(out=bthi[:], in_=bt32[:]), reads=["bt32"], writes=["bthi"])
                V(lambda: nc.vector.tensor_copy(out=braw[:], in_=bthi[:]), reads=["bthi"], writes=["braw"])
                V(lambda: nc.vector.tensor_tensor(out=bt32[:], in0=bt32[:], in1=braw[:], op=ALU.subtract), reads=["bt32", "braw"], writes=["bt32"])
                V(lambda: nc.vector.tensor_copy(out=btlo[:], in_=bt32[:]), reads=["bt32"], writes=["btlo"])
                S.barrier()

            def subln_store(o_ap, okey, rows, dst_ap, dkey, scr):
                sq, ss, sd, rs = scr
                V(lambda: nc.vector.tensor_tensor(out=sq[0:rows, :], in0=o_ap, in1=o_ap, op=ALU.mult), reads=[okey], writes=["sl_sq"])
                V(lambda: nc.vector.reduce_sum(out=ss[0:rows, :], in_=sq[0:rows, :], axis=AX.X), reads=["sl_sq"], writes=["sl_ss"])
                A(lambda: nc.scalar.activation(out=sd[0:rows, :], in_=ss[0:rows, :], func=AF.Sqrt, bias=epsc[0:rows, 0:1], scale=1.0 / 128.0),
                  reads=["sl_ss", "epsc"], writes=["sl_sd"])
                V(lambda: nc.vector.reciprocal(out=rs[0:rows, :], in_=sd[0:rows, :]), reads=["sl_sd"], writes=["sl_rs"])
                V(lambda: nc.vector.scalar_tensor_tensor(out=dst_ap, in0=o_ap, scalar=rs[0:rows, 0:1], in1=subg_bc[0:rows, :],
                                                         op0=ALU.mult, op1=ALU.mult), reads=[okey, "sl_rs", "subg_bc"], writes=[dkey])

            def combine(acc, akey, rows, osb, okey, scr2):
                r0, r1 = scr2
                V(lambda: nc.vector.reciprocal(out=r0[0:rows, :], in_=acc[0:rows, 0, 128:129]), reads=[akey], writes=["cb_r0"])
                V(lambda: nc.vector.reciprocal(out=r1[0:rows, :], in_=acc[0:rows, 1, 128:129]), reads=[akey], writes=["cb_r1"])
                V(lambda: nc.vector.tensor_tensor(out=r1[0:rows, :], in0=r1[0:rows, :], in1=nlam[0:rows, :], op=ALU.mult), reads=["cb_r1", "nlam"], writes=["cb_r1"])
                V(lambda: nc.vector.tensor_scalar(out=osb, in0=acc[0:rows, 0, 0:128], scalar1=r0[0:rows, 0:1], scalar2=None, op0=ALU.mult),
                  reads=[akey, "cb_r0"], writes=[okey])
                V(lambda: nc.vector.scalar_tensor_tensor(out=osb, in0=acc[0:rows, 1, 0:128], scalar=r1[0:rows, 0:1], in1=osb, op0=ALU.mult, op1=ALU.add),
                  reads=[akey, "cb_r1", okey], writes=[okey])

            with ExitStack() as esKV:
                sbK = lambda n, s, dt=F32: _sbuf(esKV, n, s, dt)
                KT = sbK("KT", [128, 4, L], BF16)
                VA = sbK("VA", [128, NBLK, 4, 130], BF16)
                V(lambda: nc.vector.memset(VA[:, :, :, 128:129], 1.0), writes=["VAones"])
                V(lambda: nc.vector.memset(VA[:, :, :, 129:130], 0.0), writes=["VAz"])
                with ExitStack() as es1:
                    sb1 = lambda n, s, dt=F32: _sbuf(es1, n, s, dt)
                    wk = sb1("wk", [128, 8, 512], BF16); wv = sb1("wv", [128, 8, 512], BF16); wc = sb1("wc", [128, 8, 1024], BF16)
                    xb = [sb1("xb%d" % i, [128, D]) for i in range(2)]
                    xT1 = [sb1("xT1_%d" % i, [128, 8, 512], BF16) for i in range(1)]
                    ub = sb1("ub", [128, 4, 608])
                    sig = sb1("sig", [128, 512])
                    tk = [sb1("tk%d" % i, [128, 512]) for i in range(4)]
                    ycv = sb1("ycv", [128, 4, 256]); ysq = sb1("ysq", [128, 4, 256])
                    mnb = sb1("mnb", [128, 256]); m2 = sb1("m2", [128, 256]); var = sb1("var", [128, 256]); rstd = sb1("rstd", [128, 256])
                    tt = sb1("tt", [128, 256]); nct = sb1("nct", [30, 512]); sdt = sb1("sdt", [128, 256])
                    ldc(lambda: nc.gpsimd.dma_start(out=wk[:], in_=w_in_v[:, :, KOFF:KOFF + 512]), writes=["wk"])
                    ldc(lambda: nc.gpsimd.dma_start(out=wv[:], in_=w_in_v[:, :, VOFF:VOFF + 512]), writes=["wv"])
                    ldc(lambda: nc.gpsimd.dma_start(out=wc[:], in_=w_in_v[:, :, AOFF:AOFF + 1024]), writes=["wc"])
                    V(lambda: nc.vector.memset(ub[:], 0.0), writes=["ub"])
                    tki = 0
                    for c in range(8):
                        xTc, xk = xT1[0], "xT1_0"
                        xkeys = []
                        for b_ in range(4):
                            blk = 4 * c + b_
                            xbi, xbk = xb[blk % 2], "xb%d" % (blk % 2)
                            ld(lambda: nc.sync.dma_start(out=xbi[:], in_=xloc[blk * 128:(blk + 1) * 128, :]), writes=[xbk])
                            for half in range(2):
                                bq, bqk = bank()
                                for q in range(4):
                                    kc = half * 4 + q
                                    P(lambda: nc.tensor.transpose(out=bq[:, q * 128:(q + 1) * 128], in_=xbi[:, kc * 128:(kc + 1) * 128], identity=ident[:]),
                                      reads=[xbk, "ident"], writes=[bqk], inc=(q == 3))
                                dv = xTc[:, half * 4:half * 4 + 4, b_ * 128:(b_ + 1) * 128]
                                sv = bq[:, :].rearrange("p (q r) -> p q r", r=128)
                                k = xk + "_%d_%d" % (b_, half)
                                if half == 0:
                                    A(lambda: nc.scalar.copy(out=dv, in_=sv), reads=[bqk], writes=[k])
                                else:
                                    V(lambda: nc.vector.tensor_copy(out=dv, in_=sv), reads=[bqk], writes=[k])
                                xkeys.append(k)
                        for h in range(4):
                            bq, bqk = bank()
                            mm(bq[:, :], bqk, [(wk[:, kc, h * 128:(h + 1) * 128], xTc[:, kc, :]) for kc in range(8)], xkeys + ["wk"])
                            A(lambda: nc.scalar.activation(out=KT[:, h, c * 512:(c + 1) * 512], in_=bq[:, :], func=AF.Identity,
                                                           bias=binT[:, 4 + h:5 + h], scale=1.0), reads=[bqk, "binT"], writes=[uniq("KT")])
                        for b_ in range(4):
                            blk = 4 * c + b_
                            xr = [xk + "_%d_0" % b_, xk + "_%d_1" % b_]
                            for which in range(2):
                                w_, wkey, bias_, dst = ((wk, "wk", bk_bc, nk_p), (wv, "wv", bv_bc, nv_p))[which]
                                bq, bqk = bank()
                                mm(bq[:, :], bqk, [(xTc[:, kc, b_ * 128:(b_ + 1) * 128], w_[:, kc, :]) for kc in range(8)], xr + [wkey])
                                t_, tkey = tk[tki % 4], "tk%d" % (tki % 4)
                                tki += 1
                                V(lambda: nc.vector.tensor_tensor(out=t_[:], in0=bq[:, :], in1=bias_[:], op=ALU.add), reads=[bqk, "bk_bc", "bv_bc"], writes=[tkey])
                                ld(lambda: nc.sync.dma_start(out=dst[blk * 64:(blk + 1) * 64, :], in_=t_[64:128, :]), reads=[tkey], writes=[uniq("okv")])
                                if which == 1:
                                    vak = "VA%d" % blk
                                    A(lambda: nc.scalar.copy(out=VA[:, blk, :, 0:128], in_=t_[:].rearrange("p (h v) -> p h v", v=128)),
                                      reads=[tkey, "VAones", "VAz"], writes=[vak])
                                    if blk == 0:
                                        V(lambda: nc.vector.tensor_scalar(out=VA[:, 0, :, :], in0=VA[:, 0, :, :], scalar1=vmk[:, 0:1], scalar2=None, op0=ALU.mult),
                                          reads=[vak, "vmk", "VAones", "VAz"], writes=[vak])
                        if c > 0:
                            A(lambda: nc.scalar.copy(out=ub[:, :, 0:32], in_=ub[:, :, 512:544]), reads=["ub"], writes=["ub"])
                        for cc in range(4):
                            ba_, bak = bank()
                            mm(ba_[:, :], bak, [(wc[:, kc, cc * 128:(cc + 1) * 128], xTc[:, kc, :]) for kc in range(8)], xkeys + ["wc"])
                            bg_, bgk = bank()
                            mm(bg_[:, :], bgk, [(wc[:, kc, 512 + cc * 128:512 + (cc + 1) * 128], xTc[:, kc, :]) for kc in range(8)], xkeys + ["wc"])
                            A(lambda: nc.scalar.activation(out=sig[:], in_=bg_[:, :], func=AF.Sigmoid, bias=binT[:, 16 + cc:17 + cc], scale=1.0),
                              reads=[bgk, "binT"], writes=["sig"])
                            V(lambda: nc.vector.scalar_tensor_tensor(out=ub[:, cc, 32:544], in0=ba_[:, :], scalar=binT[:, 12 + cc:13 + cc], in1=sig[:],
                                                                     op0=ALU.add, op1=ALU.mult), reads=[bak, "sig", "binT"], writes=["ub"])
                        if c == 0:
                            V(lambda: nc.vector.tensor_scalar(out=ub[:, :, 32:96], in0=ub[:, :, 32:96], scalar1=hm[:, 0:1], scalar2=None, op0=ALU.mult),
                              reads=["ub", "hm"], writes=["ub"])
                        for cc in range(4):
                            v66 = ub[:, cc, 66:578].rearrange("p (b x) -> p b x", x=128)
                            yv = ycv[:, cc, :].rearrange("p (b t) -> p b t", t=64)
                            V(lambda: nc.vector.tensor_scalar(out=yv, in0=v66[:, :, 0:64], scalar1=cwT[:, cc, 0:1], scalar2=cbT[:, cc:cc + 1],
                                                              op0=ALU.mult, op1=ALU.add), reads=["ub", "cwT", "pT12"], writes=["ycv%d" % cc])
                            for w in range(1, 31):
                                V(lambda: nc.vector.scalar_tensor_tensor(out=yv, in0=v66[:, :, w:w + 64], scalar=cwT[:, cc, w:w + 1], in1=yv,
                                                                         op0=ALU.mult, op1=ALU.add), reads=["ub", "ycv%d" % cc], writes=["ycv%d" % cc])
                        ykeys = ["ycv%d" % cc for cc in range(4)]
                        G(lambda: nc.gpsimd.tensor_tensor(out=ysq[:], in0=ycv[:], in1=ycv[:], op=ALU.mult), reads=ykeys, writes=["ysq"])
                        bm, bmk = bank()
                        mm(bm[:, 0:256], bmk, [(onesM[:], ycv[:, cc, :]) for cc in range(4)], ykeys + ["onesM"])
                        bs, bsk = bank()
                        mm(bs[:, 0:256], bsk, [(onesM[:], ysq[:, cc, :]) for cc in range(4)], ["ysq", "onesM"])
                        A(lambda: nc.scalar.copy(out=mnb[:], in_=bm[:, 0:256]), reads=[bmk], writes=["mnb"])
                        G(lambda: nc.gpsimd.tensor_tensor(out=m2[:], in0=mnb[:], in1=mnb[:], op=ALU.mult), reads=["mnb"], writes=["m2"])
                        V(lambda: nc.vector.tensor_tensor(out=var[:], in0=bs[:, 0:256], in1=m2[:], op=ALU.subtract), reads=[bsk, "m2"], writes=["var"])
                        ln_rstd(var[:], rstd[:], sdt[:], ["var"], "rstd")
                        for cc in range(4):
                            V(lambda: nc.vector.tensor_tensor(out=tt[:], in0=ycv[:, cc, :], in1=mnb[:], op=ALU.subtract), reads=["ycv%d" % cc, "mnb"], writes=["tt"])
                            G(lambda: nc.gpsimd.tensor_tensor(out=tt[:], in0=tt[:], in1=rstd[:], op=ALU.mult), reads=["tt", "rstd"], writes=["tt"])
                            A(lambda: nc.scalar.activation(out=sT[:, cc, c * 256:(c + 1) * 256], in_=tt[:], func=AF.Silu, bias=lbT[:, cc:cc + 1], scale=lgT[:, cc:cc + 1]),
                              reads=["tt", "pT12"], writes=[uniq("sT")])
                        if c == 7:
                            bq, bqk = bank()
                            for cc in range(4):
                                P(lambda: nc.tensor.transpose(out=bq[0:30, cc * 128:(cc + 1) * 128], in_=ub[:, cc, 514:544], identity=ident[:]),
                                  reads=["ub", "ident"], writes=[bqk], inc=(cc == 3))
                            V(lambda: nc.vector.tensor_copy(out=nct[:], in_=bq[0:30, :]), reads=[bqk], writes=["nct"])
                            ld(lambda: nc.sync.dma_start(out=nc_p[:, :], in_=nct[:]), reads=["nct"], writes=["nc_p"])
                    S.barrier()
                if KSTOP == 1:
                    S.finish("sp")
                    raise _Stop()
                with ExitStack() as es2:
                    sb2 = lambda n, s, dt=F32: _sbuf(es2, n, s, dt)
                    wq = sb2("wq", [128, 8, 512], BF16)
                    xo = [sb2("xo%d" % i, [128, D]) for i in range(2)]
                    xTo = [sb2("xTo%d" % i, [128, 8, 128], BF16) for i in range(2)]
                    qT = [sb2("qT%d" % i, [128, 4, 128], BF16) for i in range(2)]
                    PT = [sb2("PT%d" % i, [128, 4, 128], BF16) for i in range(3)]
                    otok = [sb2("otok%d" % i, [128, 4, 128]) for i in range(2)]
                    osb = sb2("osb", [128, 128])
                    scr = (sb2("sl_sq", [128, 128]), sb2("sl_ss", [128, 1]), sb2("sl_sd", [128, 1]), sb2("sl_rs", [128, 1]))
                    scr2 = (sb2("cb_r0", [128, 1]), sb2("cb_r1", [128, 1]))
                    ldc(lambda: nc.gpsimd.dma_start(out=wq[:], in_=w_in_v[:, :, QOFF:QOFF + 512]), writes=["wq"])
                    pti = 0
                    for I in range(NQ):
                        xoi, xok = xo[I % 2], "xo%d" % (I % 2)
                        for hh in range(2):
                            r0 = (2 * I + hh) * 128 + 64
                            ld(lambda: nc.sync.dma_start(out=xoi[hh * 64:(hh + 1) * 64, :], in_=xloc[r0:r0 + 64, :]), writes=[xok])
                        xTi, xTk = xTo[I % 2], "xTo%d" % (I % 2)
                        xkeys = transpose_rows(xoi, xok, 128, [xTi], [xTk])
                        qTi, qk = qT[I % 2], "qT%d" % (I % 2)
                        bq, bqk = bank()
                        for h in range(4):
                            for kc in range(8):
                                P(lambda: nc.tensor.matmul(bq[:, h * 128:(h + 1) * 128], lhsT=wq[:, kc, h * 128:(h + 1) * 128], rhs=xTi[:, kc, :],
                                                           start=(kc == 0), stop=(kc == 7)), reads=xkeys + ["wq"], writes=[bqk], inc=(kc == 7 and h == 3))
                        for h in range(4):
                            A(lambda: nc.scalar.activation(out=qTi[:, h, :], in_=bq[:, h * 128:(h + 1) * 128], func=AF.Identity, bias=binT[:, h:h + 1], scale=1.0),
                              reads=[bqk, "binT"], writes=[qk])
                        oti, otk = otok[I % 2], "otok%d" % (I % 2)
                        nkb = 2 * I + 2
                        for h in range(4):
                            ab, abk = accbank()
                            acc = ab[:, 0:260].rearrange("p (m v) -> p m v", v=130)
                            for m in range(2):
                                ps_ = slice(m * 64, (m + 1) * 64)
                                for g0 in range(0, nkb, 4):
                                    grp = list(range(g0, min(g0 + 4, nkb)))
                                    sbk_, sbkk = bank()
                                    for j, kb in enumerate(grp):
                                        r = kb - (2 * I - 1)
                                        special = 0 <= r <= 2
                                        osl = sbk_[:, j * 128:(j + 1) * 128]
                                        last = (j == len(grp) - 1)
                                        P(lambda: nc.tensor.matmul(osl, lhsT=KT[ps_, h, kb * 128:(kb + 1) * 128], rhs=qTi[ps_, h, :], start=True, stop=not special),
                                          reads=[qk], writes=[sbkk], inc=(last and not special))
                                        if special:
                                            P(lambda: nc.tensor.matmul(osl, lhsT=identb[:], rhs=bthi[:, r, h, :], start=False, stop=False),
                                              reads=["identb"], writes=[sbkk], inc=False)
                                            P(lambda: nc.tensor.matmul(osl, lhsT=identb[:], rhs=btlo[:, r, h, :], start=False, stop=True),
                                              reads=[], writes=[sbkk], inc=last)
                                    n = len(grp)
                                    pt_, ptk = PT[pti % 3], "PT%d" % (pti % 3)
                                    pti += 1
                                    A(lambda: nc.scalar.activation(out=pt_[:, 0:n, :], in_=sbk_[:, 0:n * 128].rearrange("p (j q) -> p j q", q=128),
                                                                   func=AF.Exp, scale=0.125), reads=[sbkk], writes=[ptk])
                                    for j, kb in enumerate(grp):
                                        P(lambda: nc.tensor.matmul(acc[:, m, :], lhsT=pt_[:, j, :], rhs=VA[:, kb, h, :], start=(kb == 0), stop=(kb == nkb - 1)),
                                          reads=[ptk], writes=[abk], inc=(j == n - 1))
                            combine(acc, abk, 128, osb[:], "osb", scr2)
                            subln_store(osb[:], "osb", 128, oti[:, h, :], otk, scr)
                        bq, bqk = bank()
                        for h in range(4):
                            P(lambda: nc.tensor.transpose(out=bq[:, h * 128:(h + 1) * 128], in_=oti[:, h, :], identity=ident[:]),
                              reads=[otk, "ident"], writes=[bqk], inc=(h == 3))
                        A(lambda: nc.scalar.copy(out=oT[:, :, I * 128:(I + 1) * 128], in_=bq[:, :].rearrange("p (h q) -> p h q", q=128)),
                          reads=[bqk], writes=[uniq("oT")])
                    S.barrier()
                if KSTOP == 2:
                    S.finish("sp")
                    raise _Stop()
            with ExitStack() as esS:
                sbS = lambda n, s, dt=F32: _sbuf(esS, n, s, dt)
                wq = sbS("wq", [128, 8, 512], BF16); wk = sbS("wk", [128, 8, 512], BF16)
                wv = sbS("wv", [128, 8, 512], BF16); wc = sbS("wc", [128, 8, 1024], BF16)
                xss = sbS("xss", [128, D]); xTs = sbS("xTs", [128, 8, 128], BF16)
                qTs = sbS("qTs", [128, 4, ST], BF16); kTs = sbS("kTs", [128, 4, ST], BF16)
                tks = sbS("tks", [128, 512]); vnew = sbS("vnew", [4, SS, 4, 130], BF16)
                uT = sbS("uT", [128, 4, ST]); sig = sbS("sig", [128, ST]); utok = sbS("utok", [ST, 512])
                cbuf = sbS("cbuf", [128, 4, SS, 34]); scv = [sbS("scv%d" % i, [120, 512]) for i in range(2)]
                ycs = sbS("ycs", [128, 4, ST]); ysqs = sbS("ysqs", [128, 4, ST])
                mnb = sbS("mnb", [128, ST]); m2 = sbS("m2", [128, ST]); var = sbS("var", [128, ST]); rstd = sbS("rstd", [128, ST]); tt = sbS("tt", [128, ST]); sdt = sbS("sdt", [128, ST])
                kpg = [sbS("kpg%d" % i, [128, 512]) for i in range(4)]
                vpg = [sbS("vpg%d" % i, [128, 512], BF16) for i in range(4)]
                KTs = sbS("KTs", [128, 4, 16, 128], BF16); VAs = sbS("VAs", [128, 16, 4, 130], BF16)
                PTs = [sbS("PTs%d" % i, [128, 16, 4], BF16) for i in range(2)]
                PTn = [sbS("PTn%d" % i, [4, 4], BF16) for i in range(2)]
                os_ = sbS("os_", [4, 128]); otoks = sbS("otoks", [4, 4, 128])
                scr = (sbS("sl_sq", [128, 128]), sbS("sl_ss", [128, 1]), sbS("sl_sd", [128, 1]), sbS("sl_rs", [128, 1]))
                scr2 = (sbS("cb_r0", [128, 1]), sbS("cb_r1", [128, 1]))
                for wt, off, n_, nm in ((wq, QOFF, 512, "wq"), (wk, KOFF, 512, "wk"), (wv, VOFF, 512, "wv"), (wc, AOFF, 1024, "wc")):
                    ldc(lambda: nc.gpsimd.dma_start(out=wt[:], in_=w_in_v[:, :, off:off + n_]), writes=[nm])
                V(lambda: nc.vector.memset(VAs[:, :, :, 128:129], 1.0), writes=["VAs1"])
                V(lambda: nc.vector.memset(VAs[:, :, :, 129:130], 0.0), writes=["VAs0"])
                V(lambda: nc.vector.memset(vnew[:, :, :, 128:129], 1.0), writes=["vn1"])
                V(lambda: nc.vector.memset(vnew[:, :, :, 129:130], 0.0), writes=["vn0"])
                ld(lambda: nc.sync.dma_start(out=xss[0:ST, :], in_=xs[:, :]), writes=["xss"])
                xkeys = transpose_rows(xss, "xss", ST, [xTs], ["xTs"])
                xc = xTs
                for (w_, wkey, dstT, dk, c0) in ((wq, "wq", qTs, "qTs", 0), (wk, "wk", kTs, "kTs", 4)):
                    bq, bqk = bank()
                    for h in range(4):
                        for kc in range(8):
                            P(lambda: nc.tensor.matmul(bq[:, h * ST:(h + 1) * ST], lhsT=w_[:, kc, h * 128:(h + 1) * 128], rhs=xc[:, kc, 0:ST],
                                                       start=(kc == 0), stop=(kc == 7)), reads=xkeys + [wkey], writes=[bqk], inc=(kc == 7 and h == 3))
                    for h in range(4):
                        A(lambda: nc.scalar.activation(out=dstT[:, h, :], in_=bq[:, h * ST:(h + 1) * ST], func=AF.Identity, bias=binT[:, c0 + h:c0 + h + 1], scale=1.0),
                          reads=[bqk, "binT"], writes=[dk])
                for (w_, wkey, bias_, dst) in ((wk, "wk", bk_bc, nk_s), (wv, "wv", bv_bc, nv_s)):
                    bq, bqk = bank()
                    mm(bq[0:ST, :], bqk, [(xc[:, kc, 0:ST], w_[:, kc, :]) for kc in range(8)], xkeys + [wkey])
                    V(lambda: nc.vector.tensor_tensor(out=tks[0:ST, :], in0=bq[0:ST, :], in1=bias_[0:ST, :], op=ALU.add), reads=[bqk, "bk_bc", "bv_bc"], writes=["tks"])
                    ld(lambda: nc.sync.dma_start(out=dst[:, :], in_=tks[0:ST, :]), reads=["tks"], writes=[uniq("oks")])
                for s in range(SS):
                    bq, bqk = bank()
                    mm(bq[0:4, :], bqk, [(xc[:, kc, 4 * s:4 * s + 4], wv[:, kc, :]) for kc in range(8)], xkeys + ["wv"])
                    V(lambda: nc.vector.tensor_tensor(out=vnew[0:4, s, :, 0:128], in0=bq[0:4, :].rearrange("p (h v) -> p h v", v=128),
                                                      in1=bv_bc[0:4, :].rearrange("p (h v) -> p h v", v=128), op=ALU.add),
                      reads=[bqk, "bv_bc", "vn1", "vn0"], writes=["vnew"])
                for cc in range(4):
                    ba_, bak = bank()
                    mm(ba_[:, 0:ST], bak, [(wc[:, kc, cc * 128:(cc + 1) * 128], xc[:, kc, 0:ST]) for kc in range(8)], xkeys + ["wc"])
                    bg_, bgk = bank()
                    mm(bg_[:, 0:ST], bgk, [(wc[:, kc, 512 + cc * 128:512 + (cc + 1) * 128], xc[:, kc, 0:ST]) for kc in range(8)], xkeys + ["wc"])
                    A(lambda: nc.scalar.activation(out=sig[:], in_=bg_[:, 0:ST], func=AF.Sigmoid, bias=binT[:, 16 + cc:17 + cc], scale=1.0), reads=[bgk, "binT"], writes=["sig"])
                    V(lambda: nc.vector.scalar_tensor_tensor(out=uT[:, cc, :], in0=ba_[:, 0:ST], scalar=binT[:, 12 + cc:13 + cc], in1=sig[:], op0=ALU.add, op1=ALU.mult),
                      reads=[bak, "sig", "binT"], writes=["uT"])
                bq, bqk = bank()
                for cc in range(4):
                    P(lambda: nc.tensor.transpose(out=bq[0:ST, cc * 128:(cc + 1) * 128], in_=uT[:, cc, :], identity=ident[:]), reads=["uT", "ident"], writes=[bqk], inc=(cc == 3))
                V(lambda: nc.vector.tensor_copy(out=utok[:], in_=bq[0:ST, :]), reads=[bqk], writes=["utok"])
                for s in range(SS):
                    ld(lambda: nc.sync.dma_start(out=nc_s[s, 26:30, :], in_=utok[4 * s:4 * s + 4, :]), reads=["utok"], writes=["nc_s_b%d" % s])
                for g in range(4):
                    sc, sck = scv[g % 2], "scv%d" % (g % 2)
                    ld(lambda: nc.sync.dma_start(out=sc[:], in_=sconv[4 * g:4 * g + 4].rearrange("s r c -> (s r) c")), writes=[sck])
                    for cc in range(4):
                        bq, bqk = bank()
                        P(lambda: nc.tensor.transpose(out=bq[:, 0:120], in_=sc[0:120, cc * 128:(cc + 1) * 128], identity=ident[0:120, 0:120]), reads=[sck, "ident"], writes=[bqk])
                        A(lambda: nc.scalar.copy(out=cbuf[:, cc, 4 * g:4 * g + 4, 0:30], in_=bq[:, 0:120].rearrange("p (s r) -> p s r", r=30)), reads=[bqk], writes=[uniq("cbuf")])
                S.barrier()
                V(lambda: nc.vector.tensor_copy(out=cbuf[:, :, :, 30:34], in_=uT[:].rearrange("p c (s t) -> p c s t", t=4)), reads=[], writes=["cbuf"])
                for cc in range(4):
                    yv = ycs[:, cc, :].rearrange("p (s t) -> p s t", t=4)
                    V(lambda: nc.vector.tensor_scalar(out=yv, in0=cbuf[:, cc, :, 0:4], scalar1=cwT[:, cc, 0:1], scalar2=cbT[:, cc:cc + 1], op0=ALU.mult, op1=ALU.add),
                      reads=["cbuf"], writes=["ycs%d" % cc])
                    for w in range(1, 31):
                        V(lambda: nc.vector.scalar_tensor_tensor(out=yv, in0=cbuf[:, cc, :, w:w + 4], scalar=cwT[:, cc, w:w + 1], in1=yv, op0=ALU.mult, op1=ALU.add),
                          reads=["cbuf", "ycs%d" % cc], writes=["ycs%d" % cc])
                ykeys = ["ycs%d" % cc for cc in range(4)]
                G(lambda: nc.gpsimd.tensor_tensor(out=ysqs[:], in0=ycs[:], in1=ycs[:], op=ALU.mult), reads=ykeys, writes=["ysqs"])
                bm, bmk = bank()
                mm(bm[:, 0:ST], bmk, [(onesM[:], ycs[:, cc, :]) for cc in range(4)], ykeys)
                bs, bsk = bank()
                mm(bs[:, 0:ST], bsk, [(onesM[:], ysqs[:, cc, :]) for cc in range(4)], ["ysqs"])
                A(lambda: nc.scalar.copy(out=mnb[:], in_=bm[:, 0:ST]), reads=[bmk], writes=["mnb"])
                G(lambda: nc.gpsimd.tensor_tensor(out=m2[:], in0=mnb[:], in1=mnb[:], op=ALU.mult), reads=["mnb"], writes=["m2"])
                V(lambda: nc.vector.tensor_tensor(out=var[:], in0=bs[:, 0:ST], in1=m2[:], op=ALU.subtract), reads=[bsk, "m2"], writes=["var"])
                ln_rstd(var[:], rstd[:], sdt[:], ["var"], "rstd")
                for cc in range(4):
                    V(lambda: nc.vector.tensor_tensor(out=tt[:], in0=ycs[:, cc, :], in1=mnb[:], op=ALU.subtract), reads=["ycs%d" % cc, "mnb"], writes=["tt"])
                    G(lambda: nc.gpsimd.tensor_tensor(out=tt[:], in0=tt[:], in1=rstd[:], op=ALU.mult), reads=["tt", "rstd"], writes=["tt"])
                    A(lambda: nc.scalar.activation(out=sT[:, cc, TOK:TOK + ST], in_=tt[:], func=AF.Silu, bias=lbT[:, cc:cc + 1], scale=lgT[:, cc:cc + 1]),
                      reads=["tt"], writes=[uniq("sTs")])
                kpi = 0
                for s in range(SS):
                    for pg in range(16):
                        col = s * 16 + pg
                        kp, kpk = kpg[kpi % 4], "kpg%d" % (kpi % 4)
                        kpi += 1
                        ldc(lambda: nc.gpsimd.indirect_dma_start(out=kp[:, :], out_offset=None, in_=ck, in_offset=bass.IndirectOffsetOnAxis(ap=idx[:, col:col + 1], axis=0)),
                            reads=["idx"], writes=[kpk])
                        vp, vpk = vpg[(kpi - 1) % 4], "vpg%d" % ((kpi - 1) % 4)
                        ldc(lambda: nc.gpsimd.indirect_dma_start(out=vp[:, :], out_offset=None, in_=cv, in_offset=bass.IndirectOffsetOnAxis(ap=idx[:, col:col + 1], axis=0)),
                            reads=["idx"], writes=[vpk])
                        G(lambda: nc.gpsimd.tensor_copy(out=VAs[:, pg, :, 0:128], in_=vp[:, :].rearrange("p (h v) -> p h v", v=128)),
                          reads=[vpk, "VAs1", "VAs0"], writes=["VAs_%d" % pg])
                        bq, bqk = bank()
                        for h in range(4):
                            P(lambda: nc.tensor.transpose(out=bq[:, h * 128:(h + 1) * 128], in_=kp[:, h * 128:(h + 1) * 128], identity=ident[:]),
                              reads=[kpk, "ident"], writes=[bqk], inc=(h == 3))
                        cpk = "KTs_%d" % pg
                        if pg % 2 == 0:
                            A(lambda: nc.scalar.copy(out=KTs[:, :, pg, :], in_=bq[:, :].rearrange("p (h k) -> p h k", k=128)), reads=[bqk], writes=[cpk])
                        else:
                            V(lambda: nc.vector.tensor_copy(out=KTs[:, :, pg, :], in_=bq[:, :].rearrange("p (h k) -> p h k", k=128)), reads=[bqk], writes=[cpk])
                    ktkeys = ["KTs_%d" % pg for pg in range(16)]
                    vakeys = ["VAs_%d" % pg for pg in range(16)]
                    qs = slice(4 * s, 4 * s + 4)
                    for h in range(4):
                        ab, abk = accbank()
                        acc = ab[:, 0:260].rearrange("p (m v) -> p m v", v=130)
                        for m in range(2):
                            ps_ = slice(m * 64, (m + 1) * 64)
                            sbk_, sbkk = bank()
                            for pg in range(16):
                                osl = sbk_[:, pg * 4:(pg + 1) * 4]
                                sp_ = (pg == 15)
                                P(lambda: nc.tensor.matmul(osl, lhsT=KTs[ps_, h, pg, :], rhs=qTs[ps_, h, qs], start=True, stop=not sp_),
                                  reads=ktkeys + ["qTs"], writes=[sbkk], inc=False)
                                if sp_:
                                    P(lambda: nc.tensor.matmul(osl, lhsT=identb[:], rhs=bthi[:, 3, h, 0:4], start=False, stop=False), reads=[], writes=[sbkk], inc=False)
                                    P(lambda: nc.tensor.matmul(osl, lhsT=identb[:], rhs=btlo[:, 3, h, 0:4], start=False, stop=True), reads=[], writes=[sbkk], inc=False)
                            osn = sbk_[0:4, 64:68]
                            P(lambda: nc.tensor.matmul(osn, lhsT=kTs[ps_, h, qs], rhs=qTs[ps_, h, qs], start=True, stop=False), reads=["kTs", "qTs"], writes=[sbkk], inc=False)
                            P(lambda: nc.tensor.matmul(osn, lhsT=identb[0:4, 0:4], rhs=bthi[0:4, 4, h, 0:4], start=False, stop=False), reads=[], writes=[sbkk], inc=False)
                            P(lambda: nc.tensor.matmul(osn, lhsT=identb[0:4, 0:4], rhs=btlo[0:4, 4, h, 0:4], start=False, stop=True), reads=[], writes=[sbkk], inc=True)
                            pi = (2 * h + m) % 2
                            pt_, ptk = PTs[pi], "PTs%d" % pi
                            pn_, pnk = PTn[pi], "PTn%d" % pi
                            A(lambda: nc.scalar.activation(out=pt_[:], in_=sbk_[:, 0:64].rearrange("p (g q) -> p g q", q=4), func=AF.Exp, scale=0.125), reads=[sbkk], writes=[ptk])
                            A(lambda: nc.scalar.activation(out=pn_[:], in_=sbk_[0:4, 64:68], func=AF.Exp, scale=0.125), reads=[sbkk], writes=[pnk])
                            for pg in range(16):
                                P(lambda: nc.tensor.matmul(acc[0:4, m, :], lhsT=pt_[:, pg, :], rhs=VAs[:, pg, h, :], start=(pg == 0), stop=False),
                                  reads=[ptk] + vakeys, writes=[abk], inc=False)
                            P(lambda: nc.tensor.matmul(acc[0:4, m, :], lhsT=pn_[:], rhs=vnew[0:4, s, h, :], start=False, stop=True), reads=[pnk, "vnew"], writes=[abk], inc=True)
                        combine(acc, abk, 4, os_[:], "os_", scr2)
                        subln_store(os_[:], "os_", 4, otoks[0:4, h, :], "otoks", scr)
                    bq, bqk = bank()
                    for h in range(4):
                        P(lambda: nc.tensor.transpose(out=bq[:, h * 4:(h + 1) * 4], in_=otoks[0:4, h, :], identity=ident[0:4, 0:4]), reads=["otoks", "ident"], writes=[bqk], inc=(h == 3))
                    V(lambda: nc.vector.tensor_copy(out=oT[:, :, TOK + 4 * s:TOK + 4 * s + 4], in_=bq[:, 0:16].rearrange("p (h q) -> p h q", q=4)), reads=[bqk], writes=[uniq("oTs")])
                S.barrier()
                if KSTOP == 3:
                    S.finish("sp")
                    raise _Stop()
            with ExitStack() as es3:
                sb3 = lambda n, s, dt=F32: _sbuf(es3, n, s, dt)
                wg = sb3("wg", [128, 8, 2048], BF16); wap = sb3("wap", [128, 4, D], BF16); wcp = sb3("wcp", [128, 4, D], BF16)
                wout = sb3("wout", [128, 8, D], BF16)
                bgg = sb3("bgg", [128, 2048]); bcpb = sb3("bcpb", [128, D]); g1b = sb3("g1b", [128, D]); b1b = sb3("b1b", [128, D])
                rw = sb3("rw", [128, 8, 32]); rbb = sb3("rbb", [128, 32]); b2all = sb3("b2all", [32, D])
                xo = [sb3("xo%d" % i, [128, D]) for i in range(2)]
                xTo = sb3("xTo", [128, 8, 128], BF16)
                sgt = sb3("sgt", [128, 2048]); mt = sb3("mt", [128, D]); t2 = sb3("t2", [128, 512]); mT = sb3("mT", [128, 8, 128], BF16)
                pre = sb3("pre", [128, D]); h1 = sb3("h1", [128, D]); ya = sb3("ya", [128, D])
                h1Tf = sb3("h1Tf", [128, 8, 128]); h1Tb = sb3("h1Tb", [128, 8, 128], BF16)
                bn = sb3("bn", [128, 2, 6]); mv = sb3("mv", [128, 2]); sd = sb3("sd", [128, 1]); rs1 = sb3("rs1", [128, 1])
                lg = sb3("lg", [128, 32]); t8 = sb3("t8", [128, 8]); msk = sb3("msk", [128, 32]); nmx = sb3("nmx", [128, 1])
                ex = sb3("ex", [128, 32]); ssum = sb3("ssum", [128, 1]); gt = sb3("gt", [128, 32]); gtT = sb3("gtT", [32, 128])
                ldc(lambda: nc.gpsimd.dma_start(out=wg[:], in_=w_in_v[:, :, GAOFF:GAOFF + 2048]), writes=["wg"])
                ldc(lambda: nc.gpsimd.dma_start(out=wap[:], in_=wap_d.rearrange("(c p) n -> p c n", p=128)), writes=["wap"])
                ldc(lambda: nc.gpsimd.dma_start(out=wcp[:], in_=wcp_d.rearrange("(c p) n -> p c n", p=128)), writes=["wcp"])
                ldc(lambda: nc.gpsimd.dma_start(out=wout[:], in_=wout_d.rearrange("(c p) n -> p c n", p=128)), writes=["wout"])
                ld(lambda: nc.sync.dma_start(out=bgg[:], in_=b_in[GAOFF:GAOFF + 2048].partition_broadcast(128)), writes=["bgg"])
                ld(lambda: nc.sync.dma_start(out=bcpb[:], in_=bcp_d.partition_broadcast(128)), writes=["bcpb"])
                ld(lambda: nc.sync.dma_start(out=g1b[:], in_=ln1g.partition_broadcast(128)), writes=["g1b"])
                ld(lambda: nc.sync.dma_start(out=b1b[:], in_=ln1b.partition_broadcast(128)), writes=["b1b"])
                ld(lambda: nc.sync.dma_start(out=rw[:], in_=rw_d.rearrange("(c p) e -> p c e", p=128)), writes=["rw"])
                ld(lambda: nc.sync.dma_start(out=rbb[:], in_=rbias.partition_broadcast(128)), writes=["rbb"])
                ld(lambda: nc.sync.dma_start(out=b2all[:], in_=b2_d), writes=["b2all"])
                V(lambda: nc.vector.memset(h1Tb[:], 0.0), writes=["h1Tbh0", "h1Tbh1"])
                V(lambda: nc.vector.memset(h1Tf[:], 0.0), writes=["h1Tfh0", "h1Tfh1"])
                for I in range(NTILE):
                    rows = 128 if I < NQ else ST
                    R = slice(0, rows)
                    cols = slice(I * 128, I * 128 + rows)
                    xoi, xok = xo[I % 2], "xo%d" % (I % 2)
                    if I < NQ:
                        for hh in range(2):
                            r0 = (2 * I + hh) * 128 + 64
                            ld(lambda: nc.sync.dma_start(out=xoi[hh * 64:(hh + 1) * 64, :], in_=xloc[r0:r0 + 64, :]), writes=[xok])
                    else:
                        ld(lambda: nc.sync.dma_start(out=xoi[R, :], in_=xs[:, :]), writes=[xok])
                    xkeys = transpose_rows(xoi, xok, rows, [xTo], ["xTo"])
                    for p_ in range(4):
                        bq, bqk = bank()
                        mm(bq[R, :], bqk, [(xTo[:, kc, R], wg[:, kc, p_ * 512:(p_ + 1) * 512]) for kc in range(8)], xkeys + ["wg"])
                        V(lambda: nc.vector.tensor_tensor(out=sgt[R, p_ * 512:(p_ + 1) * 512], in0=bq[R, :], in1=bgg[R, p_ * 512:(p_ + 1) * 512], op=ALU.add),
                          reads=[bqk, "bgg"], writes=["sgt"])
                    A(lambda: nc.scalar.activation(out=sgt[R, :], in_=sgt[R, :], func=AF.Sigmoid), reads=["sgt"], writes=["sgt"])
                    stop_here(41)
                    for half in range(2):
                        hs = slice(half * 512, (half + 1) * 512)
                        bq, bqk = bank()
                        mm(bq[R, :], bqk, [(oT[:, h, cols], wap[:, h, hs]) for h in range(4)], ["wap"])
                        V(lambda: nc.vector.tensor_tensor(out=mt[R, hs], in0=bq[R, :], in1=sgt[R, hs], op=ALU.mult), reads=[bqk, "sgt"], writes=["mt%d" % half])
                        bq, bqk = bank()
                        mm(bq[R, :], bqk, [(sT[:, cc, cols], wcp[:, cc, hs]) for cc in range(4)], ["wcp"])
                        V(lambda: nc.vector.tensor_tensor(out=t2[R, :], in0=bq[R, :], in1=bcpb[R, hs], op=ALU.add), reads=[bqk, "bcpb"], writes=["t2"])
                        G(lambda: nc.gpsimd.tensor_tensor(out=t2[R, :], in0=t2[R, :], in1=sgt[R, 1024 + half * 512:1024 + (half + 1) * 512], op=ALU.mult),
                          reads=["t2", "sgt"], writes=["t2"])
                        G(lambda: nc.gpsimd.tensor_tensor(out=mt[R, hs], in0=mt[R, hs], in1=t2[R, :], op=ALU.add), reads=["t2", "mt%d" % half], writes=["mt%d" % half])
                    stop_here(42)
                    mkeys = transpose_rows(mt, ["mt0", "mt1"], rows, [mT], ["mT"])
                    for half in range(2):
                        hs = slice(half * 512, (half + 1) * 512)
                        bq, bqk = bank()
                        mm(bq[R, :], bqk, [(mT[:, kc, R], wout[:, kc, hs]) for kc in range(8)], mkeys + ["wout"])
                        V(lambda: nc.vector.scalar_tensor_tensor(out=pre[R, hs], in0=xoi[R, hs], scalar=ALPHA, in1=bq[R, :], op0=ALU.mult, op1=ALU.add),
                          reads=[bqk, xok], writes=["pre%d" % half])
                        V(lambda: nc.vector.bn_stats(out=bn[R, half, :], in_=pre[R, hs]), reads=["pre%d" % half], writes=["bn%d" % half])
                    V(lambda: nc.vector.bn_aggr(out=mv[R, :], in_=bn[R, :, :]), reads=["bn0", "bn1"], writes=["mv"])
                    ln_rstd(mv[R, 1:2], rs1[R, :], sd[R, :], ["mv"], "rs1")
                    V(lambda: nc.vector.tensor_scalar(out=h1[R, :], in0=pre[R, :], scalar1=mv[R, 0:1], scalar2=rs1[R, 0:1], op0=ALU.subtract, op1=ALU.mult),
                      reads=["pre0", "pre1", "mv", "rs1"], writes=["h1"])
                    G(lambda: nc.gpsimd.tensor_tensor(out=h1[R, :], in0=h1[R, :], in1=g1b[R, :], op=ALU.mult), reads=["h1", "g1b"], writes=["h1"])
                    G(lambda: nc.gpsimd.tensor_tensor(out=h1[R, :], in0=h1[R, :], in1=b1b[R, :], op=ALU.add), reads=["h1", "b1b"], writes=["h1"])
                    stop_here(43)
                    hkeys = transpose_rows(h1, "h1", rows, [h1Tf, h1Tb], ["h1Tf", "h1Tb"])
                    stop_here(44)
                    ld(lambda: nc.sync.dma_start(out=h1T_scr[:, :, I * 128:(I + 1) * 128], in_=h1Tb[:]), reads=["h1Tbh0", "h1Tbh1"], writes=[uniq("h1Ts")])
                    bq, bqk = bank()
                    mm(bq[R, 0:32], bqk, [(h1Tf[:, kc, R], rw[:, kc, :]) for kc in range(8)], ["h1Tfh0", "h1Tfh1", "rw"])
                    V(lambda: nc.vector.tensor_tensor(out=lg[R, :], in0=bq[R, 0:32], in1=rbb[R, :], op=ALU.add), reads=[bqk, "rbb"], writes=["lg"])
                    V(lambda: nc.vector.max(out=t8[R, :], in_=lg[R, :]), reads=["lg"], writes=["t8"])
                    V(lambda: nc.vector.tensor_scalar(out=msk[R, :], in0=lg[R, :], scalar1=t8[R, 3:4], scalar2=None, op0=ALU.is_ge), reads=["lg", "t8"], writes=["msk"])
                    V(lambda: nc.vector.tensor_scalar(out=nmx[R, :], in0=t8[R, 0:1], scalar1=-1.0, scalar2=None, op0=ALU.mult), reads=["t8"], writes=["nmx"])
                    A(lambda: nc.scalar.activation(out=ex[R, :], in_=lg[R, :], func=AF.Exp, bias=nmx[R, 0:1], scale=1.0), reads=["lg", "nmx"], writes=["ex"])
                    V(lambda: nc.vector.tensor_tensor(out=ex[R, :], in0=ex[R, :], in1=msk[R, :], op=ALU.mult), reads=["ex", "msk"], writes=["ex"])
                    V(lambda: nc.vector.reduce_sum(out=ssum[R, :], in_=ex[R, :], axis=AX.X), reads=["ex"], writes=["ssum"])
                    V(lambda: nc.vector.reciprocal(out=ssum[R, :], in_=ssum[R, :]), reads=["ssum"], writes=["ssum"])
                    V(lambda: nc.vector.tensor_scalar(out=gt[R, :], in0=ex[R, :], scalar1=ssum[R, 0:1], scalar2=None, op0=ALU.mult), reads=["ex", "ssum"], writes=["gt"])
                    stop_here(45)
                    ld(lambda: nc.sync.dma_start(out=gates_scr[I, R, :], in_=gt[R, :]), reads=["gt"], writes=[uniq("gts")])
                    bq, bqk = bank()
                    P(lambda: nc.tensor.transpose(out=bq[0:32, 0:rows], in_=gt[R, :], identity=ident[R, R]), reads=["gt", "ident"], writes=[bqk])
                    A(lambda: nc.scalar.copy(out=gtT[:, R], in_=bq[0:32, 0:rows]), reads=[bqk], writes=["gtT"])
                    for half in range(2):
                        hs = slice(half * 512, (half + 1) * 512)
                        bq, bqk = bank()
                        mm(bq[R, :], bqk, [(gtT[:, R], b2all[:, hs])], ["gtT", "b2all"])
                        V(lambda: nc.vector.scalar_tensor_tensor(out=ya[R, hs], in0=h1[R, hs], scalar=ALPHA, in1=bq[R, :], op0=ALU.mult, op1=ALU.add),
                          reads=[bqk, "h1"], writes=["ya%d" % half])
                    ld(lambda: nc.sync.dma_start(out=yacc_scr[I, R, :], in_=ya[R, :]), reads=["ya0", "ya1"], writes=[uniq("yas")])
                    stop_here(46)
                    if I == 15:
                        stop_here(47)
                S.barrier()
                if KSTOP == 4:
                    S.finish("sp")
                    raise _Stop()
        with ExitStack() as esB:
            sbB = lambda n, s, dt=F32: _sbuf(esB, n, s, dt)
            b1T = sbB("b1T", [128, 16, 32])
            with ExitStack() as est:
                b1rows = _sbuf(est, "b1rows", [32, 2048], F32)
                ld(lambda: nc.sync.dma_start(out=b1rows[:], in_=b1_d), writes=["b1rows"])
                bq, bqk = bank()
                for c_ in range(16):
                    P(lambda: nc.tensor.transpose(out=bq[:, c_ * 32:(c_ + 1) * 32], in_=b1rows[0:32, c_ * 128:(c_ + 1) * 128], identity=ident[0:32, 0:32]),
                      reads=["b1rows", "ident"], writes=[bqk], inc=(c_ == 15))
                V(lambda: nc.vector.tensor_copy(out=b1T[:], in_=bq[:, :].rearrange("p (c e) -> p c e", e=32)), reads=[bqk], writes=["b1T"])
                S.barrier()
            b1T1 = sbB("b1T1", [128, 8, 32])
            V(lambda: nc.vector.tensor_scalar(out=b1T1[:], in0=b1T[:, 8:16, :], scalar1=1.0, scalar2=None, op0=ALU.add), writes=["b1T1"])
            w1t = [sbB("w1t%d" % i, [128, 8, 2048], BF16) for i in range(2)]
            w2t = [sbB("w2t%d" % i, [128, 8, D], BF16) for i in range(2)]
            h1Th = sbB("h1Th", [128, 8, 9 * 128], BF16); yacc = sbB("yacc", [128, 9, D]); gts = sbB("gts", [128, 9, 32])
            g2b = sbB("g2b", [128, D]); b2b = sbB("b2b", [128, D])
            g32 = [sbB("g32_%d" % i, [128, 512]) for i in range(2)]; sg32 = [sbB("sg32_%d" % i, [128, 512]) for i in range(2)]
            u32 = [sbB("u32_%d" % i, [128, 512]) for i in range(2)]
            actT = [sbB("actT%d" % i, [128, 8, 512], BF16) for i in range(2)]
            bn = sbB("bn", [128, 2, 6]); mv = sbB("mv", [128, 2]); sd = sbB("sd", [128, 1]); rs1 = sbB("rs1", [128, 1]); yo = [sbB("yo%d" % i, [128, D]) for i in range(2)]
            ld(lambda: nc.sync.dma_start(out=g2b[:], in_=ln2g.partition_broadcast(128)), writes=["g2b"])
            ld(lambda: nc.sync.dma_start(out=b2b[:], in_=ln2b.partition_broadcast(128)), writes=["b2b"])
            def load_expert(e_, slot):
                w1e_, w1k_ = w1t[slot % 2], "w1t%d" % (slot % 2)
                w2e_, w2k_ = w2t[slot % 2], "w2t%d" % (slot % 2)
                for q in range(4):
                    ldc(lambda: nc.gpsimd.dma_start(out=w1e_[:, :, q * 512:(q + 1) * 512], in_=w1_d[e_].rearrange("(c p) f -> p c f", p=128)[:, :, q * 512:(q + 1) * 512]),
                        writes=[w1k_ + "_%d" % q])
                for q in range(2):
                    ldc(lambda: nc.gpsimd.dma_start(out=w2e_[:, :, q * 512:(q + 1) * 512], in_=w2_d[e_].rearrange("(c p) f -> p c f", p=128)[:, :, q * 512:(q + 1) * 512]),
                        writes=[w2k_ + "_%d" % q])

            for hf, tiles in enumerate(HALVES):
                nt = len(tiles)
                ncols = nt * 128
                t0 = tiles[0]
                ld(lambda: nc.sync.dma_start(out=h1Th[:, :, 0:ncols], in_=h1T_scr[:, :, t0 * 128:t0 * 128 + ncols]), writes=["h1Th"])
                ld(lambda: nc.sync.dma_start(out=yacc[:, 0:nt, :], in_=yacc_scr[t0:t0 + nt].rearrange("t p d -> p t d")), writes=["yacc%d" % i for i in range(nt)])
                ld(lambda: nc.sync.dma_start(out=gts[:, 0:nt, :], in_=gates_scr[t0:t0 + nt].rearrange("t p e -> p t e")), writes=["gts"])
                groups = [(g0, min(512, ncols - g0)) for g0 in range(0, ncols, 512)]
                items = [(e, gi) for e in range(32) for gi in range(len(groups))]

                def stage_a(it):
                    e, gi = items[it]
                    g0, n = groups[gi]
                    slot = (hf * 32 + e) % 2
                    w1e, w1k = w1t[slot], "w1t%d" % slot
                    at, atk = actT[it % 2], "actT%d" % (it % 2)
                    akeys = []
                    for fc in range(8):
                        bi = fc % 2
                        gg, ggk = g32[bi], "g32_%d" % bi
                        sgg, sgk = sg32[bi], "sg32_%d" % bi
                        uu, uuk = u32[bi], "u32_%d" % bi
                        bgq, bgk = bank()
                        mm(bgq[:, 0:n], bgk, [(w1e[:, kc, fc * 128:(fc + 1) * 128], h1Th[:, kc, g0:g0 + n]) for kc in range(8)], ["h1Th", w1k + "_%d" % (fc // 4)])
                        buq, buk = bank()
                        mm(buq[:, 0:n], buk, [(w1e[:, kc, 1024 + fc * 128:1024 + (fc + 1) * 128], h1Th[:, kc, g0:g0 + n]) for kc in range(8)],
                           ["h1Th", w1k + "_%d" % (2 + fc // 4)])
                        V(lambda: nc.vector.tensor_scalar(out=gg[:, 0:n], in0=bgq[:, 0:n], scalar1=b1T[:, fc, e:e + 1], scalar2=7.0, op0=ALU.add, op1=ALU.min),
                          reads=[bgk, "b1T"], writes=[ggk])
                        A(lambda: nc.scalar.activation(out=sgg[:, 0:n], in_=gg[:, 0:n], func=AF.Sigmoid, scale=1.702), reads=[ggk], writes=[sgk])
                        V(lambda: nc.vector.tensor_scalar(out=uu[:, 0:n], in0=buq[:, 0:n], scalar1=b1T1[:, fc, e:e + 1], scalar2=8.0, op0=ALU.add, op1=ALU.min),
                          reads=[buk, "b1T1"], writes=[uuk])
                        G(lambda: nc.gpsimd.tensor_tensor(out=sgg[:, 0:n], in0=sgg[:, 0:n], in1=gg[:, 0:n], op=ALU.mult), reads=[sgk, ggk], writes=[sgk])
                        k = atk + "_%d" % fc
                        V(lambda: nc.vector.scalar_tensor_tensor(out=at[:, fc, 0:n], in0=uu[:, 0:n], scalar=-6.0, in1=sgg[:, 0:n], op0=ALU.max, op1=ALU.mult),
                          reads=[uuk, sgk], writes=[k])
                        akeys.append(k)
                    return akeys

                def stage_b(it, akeys):
                    e, gi = items[it]
                    g0, n = groups[gi]
                    slot = (hf * 32 + e) % 2
                    w2e, w2k = w2t[slot], "w2t%d" % slot
                    at = actT[it % 2]
                    for tt_ in range(n // 128):
                        ti = g0 // 128 + tt_
                        for half in range(2):
                            hs = slice(half * 512, (half + 1) * 512)
                            bq, bqk = bank()
                            mm(bq[:, :], bqk, [(at[:, fc, tt_ * 128:(tt_ + 1) * 128], w2e[:, fc, hs]) for fc in range(8)], akeys + [w2k + "_%d" % half])
                            V(lambda: nc.vector.scalar_tensor_tensor(out=yacc[:, ti, hs], in0=bq[:, :], scalar=gts[:, ti, e:e + 1], in1=yacc[:, ti, hs],
                                                                     op0=ALU.mult, op1=ALU.add), reads=[bqk, "gts", "yacc%d" % ti], writes=["yacc%d" % ti])

                load_expert(0, hf * 32 + 0)
                load_expert(1, hf * 32 + 1)
                pend = stage_a(0)
                for it in range(len(items)):
                    nxt = stage_a(it + 1) if it + 1 < len(items) else None
                    stage_b(it, pend)
                    pend = nxt
                    e, gi = items[it]
                    if gi == len(groups) - 1 and e + 2 < 32:
                        load_expert(e + 2, hf * 32 + e + 2)
                for ti, I in enumerate(tiles):
                    rows = 128 if I < NQ else ST
                    R = slice(0, rows)
                    yk = "yacc%d" % ti
                    for half in range(2):
                        V(lambda: nc.vector.bn_stats(out=bn[R, half, :], in_=yacc[R, ti, half * 512:(half + 1) * 512]), reads=[yk], writes=["bn%d" % half])
                    V(lambda: nc.vector.bn_aggr(out=mv[R, :], in_=bn[R, :, :]), reads=["bn0", "bn1"], writes=["mv"])
                    ln_rstd(mv[R, 1:2], rs1[R, :], sd[R, :], ["mv"], "rs1")
                    yoi, yok = yo[ti % 2], "yo%d" % (ti % 2)
                    V(lambda: nc.vector.tensor_scalar(out=yoi[R, :], in0=yacc[R, ti, :], scalar1=mv[R, 0:1], scalar2=rs1[R, 0:1], op0=ALU.subtract, op1=ALU.mult),
                      reads=[yk, "mv", "rs1"], writes=[yok])
                    G(lambda: nc.gpsimd.tensor_tensor(out=yoi[R, :], in0=yoi[R, :], in1=g2b[R, :], op=ALU.mult), reads=[yok, "g2b"], writes=[yok])
                    G(lambda: nc.gpsimd.tensor_tensor(out=yoi[R, :], in0=yoi[R, :], in1=b2b[R, :], op=ALU.add), reads=[yok, "b2b"], writes=[yok])
                    if I < NQ:
                        ld(lambda: nc.sync.dma_start(out=y_p[I * 128:(I + 1) * 128, :], in_=yoi[:, :]), reads=[yok], writes=[uniq("yout")])
                    else:
                        ld(lambda: nc.sync.dma_start(out=y_s[:, :], in_=yoi[R, :]), reads=[yok], writes=[uniq("yout")])
            S.finish("sp")


def _bucket_table():
    n = np.arange(0, 512)
    nf = np.maximum(n, 1).astype(np.float32)
    large = 16 + (np.log(nf / np.float32(16.0)) / np.float32(math.log(128 / 16)) * np.float32(16.0)).astype(np.int32)
    large = np.minimum(large, 31)
    return np.where(n < 16, n, large).astype(np.int64)


def _bias_tables(rel_bias):
    bt = _bucket_table()
    kk = np.arange(128)[:, None]
    cc = np.arange(128)[None, :]
    dist = np.zeros((5, 128, 128), np.int64)
    for r in range(3):
        d = np.where(cc < 64, 128 * (1 - r) + 64 + cc - kk, 128 * (2 - r) + cc - kk)
        dist[r] = d
    dist[3] = 128 + cc - kk
    dist[4] = cc - kk
    valid = dist >= 0
    valid[3][:, 4:] = True
    valid[4][4:, :] = True
    valid[4][:, 4:] = True
    dcl = np.clip(dist, 0, 511)
    braw = rel_bias[bt[dcl]]
    braw = np.where(valid[..., None], braw, 0.0).astype(np.float32)
    braw = np.ascontiguousarray(braw.transpose(1, 0, 3, 2)).reshape(128, 5 * 4 * 128)
    bmask = np.where(valid, 0.0, 8.0 * NEG).astype(np.float32)
    bmask = np.ascontiguousarray(np.broadcast_to(bmask[:, :, None, :], (5, 128, 4, 128)).transpose(1, 0, 2, 3)).reshape(128, 5 * 4 * 128)
    return braw, bmask


def kernel(x_prompt, x_sample, cache_k, cache_v, page_table, state_conv, w_in, b_in, lambda_q1, lambda_k1,
           lambda_q2, lambda_k2, subln_g, rel_bias, w_attn_proj, conv_w, conv_b, conv_ln_g, conv_ln_b,
           w_conv_proj, b_conv_proj, w_out, ln1_g, ln1_b, router_w, router_b, expert_w1, expert_b1,
           expert_w2, expert_b2, ln2_g, ln2_b):
    f = lambda a: np.ascontiguousarray(np.asarray(a, dtype=np.float32))
    x_prompt, x_sample, state_conv = f(x_prompt), f(x_sample), f(state_conv)
    rel_bias = f(rel_bias)
    braw, bmask = _bias_tables(rel_bias)
    shared = {
        "ck": f(cache_k).reshape(NPOOL * 128, 512), "cv": f(cache_v).reshape(NPOOL * 128, 512),
        "iot": np.arange(128, dtype=np.float32).reshape(128, 1), "braw": braw, "bmask": bmask,
        "w_in": f(w_in)[0], "b_in": f(b_in)[0],
        "lq1": f(lambda_q1)[0], "lk1": f(lambda_k1)[0], "lq2": f(lambda_q2)[0], "lk2": f(lambda_k2)[0],
        "subg": f(subln_g)[0], "rb31": np.ascontiguousarray(rel_bias[31]),
        "wap": f(w_attn_proj)[0], "convw": f(conv_w)[0], "convb": f(conv_b)[0], "clng": f(conv_ln_g)[0], "clnb": f(conv_ln_b)[0],
        "wcp": f(w_conv_proj)[0], "bcp": f(b_conv_proj)[0], "wout": f(w_out)[0], "ln1g": f(ln1_g)[0], "ln1b": f(ln1_b)[0],
        "rw": f(router_w)[0], "rbias": f(router_b)[0], "w1": f(expert_w1)[0], "b1": f(expert_b1)[0],
        "w2": f(expert_w2)[0], "b2": f(expert_b2)[0], "ln2g": f(ln2_g)[0], "ln2b": f(ln2_b)[0],
    }
    pt = np.ascontiguousarray(np.asarray(page_table, dtype=np.int32))
    nc = build_program()
    in_maps = []
    for c in range(NCORES):
        b, j = c // 2, c % 2
        if j == 1:
            xl = x_prompt[b]
        else:
            xl = np.concatenate([np.zeros((64, D), np.float32), x_prompt[b, :L - 64]], axis=0)
        one = np.ones((128, 1), np.float32)
        vm = one.copy()
        if j == 0:
            vm[:64] = 0.0
        m = dict(shared)
        m.update({
            "xloc": np.ascontiguousarray(xl),
            "xs": np.ascontiguousarray(x_sample[c * SS:(c + 1) * SS].reshape(ST, D)),
            "ptab": np.ascontiguousarray(pt[c * SS:(c + 1) * SS].reshape(-1)),
            "sconv": np.ascontiguousarray(state_conv[0, c * SS:(c + 1) * SS]),
            "vmk": vm, "hm": (one * float(j)).astype(np.float32),
        })
        in_maps.append(m)
    res = run_bass_kernel_spmd(nc, in_maps, core_ids=list(range(NCORES))).results
    B = x_prompt.shape[0]
    y_p = np.zeros((B, L, D), np.float32)
    nk_p = np.zeros((1, B, L, 4, 128), np.float32)
    nv_p = np.zeros((1, B, L, 4, 128), np.float32)
    nc_p = np.zeros((1, B, 30, 512), np.float32)
    y_s = np.zeros((128, 4, D), np.float32)
    nk_s = np.zeros((1, 128, 4, 4, 128), np.float32)
    nv_s = np.zeros((1, 128, 4, 4, 128), np.float32)
    nc_s = np.zeros((1, 128, 30, 512), np.float32)
    for c in range(NCORES):
        b, j = c // 2, c % 2
        r = res[c]
        rows = (np.arange(NBLK)[:, None] * 128 + 64 * j + np.arange(64)[None, :]).reshape(-1)
        y_p[b, rows] = r["y_p"]
        nk_p[0, b, rows] = r["nk_p"].reshape(TOK, 4, 128)
        nv_p[0, b, rows] = r["nv_p"].reshape(TOK, 4, 128)
        if j == 1:
            nc_p[0, b] = r["nc_p"]
        ss = slice(c * SS, (c + 1) * SS)
        y_s[ss] = r["y_s"].reshape(SS, 4, D)
        nk_s[0, ss] = r["nk_s"].reshape(SS, 4, 4, 128)
        nv_s[0, ss] = r["nv_s"].reshape(SS, 4, 4, 128)
        nc_s[0, ss] = r["nc_s"]
    return (y_p, y_s, nk_p, nv_p, nc_p, nk_s, nv_s, nc_s)
```

```python
import math
import os
import numpy as np
from contextlib import ExitStack
import concourse.bass as bass
import concourse.mybir as mybir
from concourse.bass_utils import run_bass_kernel_spmd

F32 = mybir.dt.float32
BF16 = mybir.dt.bfloat16
I32 = mybir.dt.int32
AF = mybir.ActivationFunctionType
ALU = mybir.AluOpType
AX = mybir.AxisListType

NCORES = 8
D = 1024
L = 4096
NBLK = 32
NQ = 16
TOK = 2048
SS = 16
ST = 64
NTILE = 17
QOFF, KOFF, VOFF, AOFF, GOFF, GAOFF = 0, 512, 1024, 1536, 2048, 2560
ALPHA = float(2.0 ** 0.25)
LAM0 = 0.8 - 0.6 * math.exp(0.0)
EPS = 1e-5
NEG = -30000.0
NPOOL = int(os.environ.get('KPOOL', '2560'))
KSTOP = int(os.environ.get('KSTOP', '99'))
HALVES = (list(range(0, 9)), list(range(9, 17)))


class Sched:
    def __init__(self, nc, es, n_dma_sems=32):
        self.nc = nc
        self.eng = {"pe": nc.tensor, "act": nc.scalar, "dve": nc.vector, "pool": nc.gpsimd, "sp": nc.sync}
        self.sem = {k: es.enter_context(nc.semaphore("s_" + k)) for k in self.eng}
        self.cnt = {k: 0 for k in self.eng}
        self.dsem = [es.enter_context(nc.semaphore("d%d" % i)) for i in range(n_dma_sems)]
        self.dcnt = [0] * n_dma_sems
        self.dnext = 0
        self.known = {k: {} for k in self.eng}
        self.lastw = {}
        self.readers = {}

    def _wait(self, e, tok):
        kind, key, val = tok
        if kind == "e" and key == e and (e == "pe" or val > self.cnt[e]):
            return
        if self.known[e].get((kind, key), 0) >= val:
            return
        self.known[e][(kind, key)] = val
        s = self.sem[key] if kind == "e" else self.dsem[key]
        self.eng[e].wait_ge(s, val)

    def _deps(self, e, reads, writes):
        toks = []
        for r in reads:
            if r in self.lastw:
                toks.append(self.lastw[r])
        for w in writes:
            if w in self.lastw:
                toks.append(self.lastw[w])
            toks.extend(self.readers.get(w, []))
        for t in toks:
            self._wait(e, t)

    def _record(self, tok, reads, writes):
        for r in reads:
            self.readers.setdefault(r, []).append(tok)
        for w in writes:
            self.lastw[w] = tok
            self.readers[w] = []

    def op(self, e, fn, reads=(), writes=(), inc=True):
        self._deps(e, reads, writes)
        ins = fn()
        tok = ("e", e, self.cnt[e] + 1)
        if inc:
            self.cnt[e] += 1
            ins.then_inc(self.sem[e], 1)
        self._record(tok, reads, writes)
        return ins

    MAX_OUTSTANDING = {"pool": 4, "sp": 8}

    def dma(self, q, fn, reads=(), writes=()):
        i = self.dnext
        self.dnext = (self.dnext + 1) % len(self.dsem)
        if self.dcnt[i] > 0:
            self._wait(q, ("d", i, self.dcnt[i]))
        hist = self.__dict__.setdefault("dhist", {}).setdefault(q, [])
        k = self.MAX_OUTSTANDING.get(q, 8)
        if len(hist) >= k:
            self._wait(q, hist[-k])
        self._deps(q, reads, writes)
        ins = fn()
        self.dcnt[i] += 16
        ins.then_inc(self.dsem[i], 16)
        tok = ("d", i, self.dcnt[i])
        hist.append(tok)
        self._record(tok, reads, writes)
        return ins

    def barrier(self):
        for e in self.eng:
            for f in self.eng:
                if f != e and self.cnt[f] > 0:
                    self._wait(e, ("e", f, self.cnt[f]))
            for i, c in enumerate(self.dcnt):
                if c > 0:
                    self._wait(e, ("d", i, c))
        self.lastw.clear()
        self.readers.clear()

    def finish(self, e="sp"):
        for f in self.eng:
            if f != e and self.cnt[f] > 0:
                self._wait(e, ("e", f, self.cnt[f]))
        for i, c in enumerate(self.dcnt):
            if c > 0:
                self._wait(e, ("d", i, c))


class _Stop(Exception):
    pass


def build_program():
    nc = bass.Bass("TRN2", target_bir_lowering=False)
    try:
        _build_body(nc)
    except _Stop:
        pass
    return nc


def _build_body(nc):
    din = lambda n, s, dt=F32: nc.dram_tensor(n, s, dt, kind="ExternalInput").ap()
    dout = lambda n, s: nc.dram_tensor(n, s, F32, kind="ExternalOutput").ap()
    dscr = lambda n, s, dt=F32: nc.dram_tensor(n, s, dt, kind="Internal").ap()
    xloc = din("xloc", [L, D]); xs = din("xs", [ST, D])
    ck = din("ck", [NPOOL * 128, 512]); cv = din("cv", [NPOOL * 128, 512])
    ptab = din("ptab", [SS * 16], I32); sconv = din("sconv", [SS, 30, 512])
    iot = din("iot", [128, 1]); vmk_d = din("vmk", [128, 1]); hm_d = din("hm", [128, 1])
    braw_d = din("braw", [128, 5 * 4 * 128]); bmask_d = din("bmask", [128, 5 * 4 * 128])
    w_in = din("w_in", [D, 4608]); b_in = din("b_in", [4608])
    lq1 = din("lq1", [64]); lk1 = din("lk1", [64]); lq2 = din("lq2", [64]); lk2 = din("lk2", [64])
    subg = din("subg", [128]); rb31_d = din("rb31", [4])
    wap_d = din("wap", [512, D]); convw = din("convw", [31, 512]); convb = din("convb", [512])
    clng = din("clng", [512]); clnb = din("clnb", [512]); wcp_d = din("wcp", [512, D]); bcp_d = din("bcp", [D])
    wout_d = din("wout", [D, D]); ln1g = din("ln1g", [D]); ln1b = din("ln1b", [D])
    rw_d = din("rw", [D, 32]); rbias = din("rbias", [32])
    w1_d = din("w1", [32, D, 2048]); b1_d = din("b1", [32, 2048]); w2_d = din("w2", [32, D, D]); b2_d = din("b2", [32, D])
    ln2g = din("ln2g", [D]); ln2b = din("ln2b", [D])
    y_p = dout("y_p", [TOK, D]); y_s = dout("y_s", [ST, D])
    nk_p = dout("nk_p", [TOK, 512]); nv_p = dout("nv_p", [TOK, 512]); nc_p = dout("nc_p", [30, 512])
    nk_s = dout("nk_s", [ST, 512]); nv_s = dout("nv_s", [ST, 512]); nc_s = dout("nc_s", [SS, 30, 512])
    yacc_scr = dscr("yacc_scr", [NTILE, 128, D]); h1T_scr = dscr("h1T_scr", [128, 8, NTILE * 128], BF16)
    gates_scr = dscr("gates_scr", [NTILE, 128, 32])
    w_in_v = w_in.rearrange("(kc p) n -> p kc n", p=128)

    with ExitStack() as es0:
        _nm = {"i": 0}

        def _sbuf(stack, n, s, dt):
            _nm["i"] += 1
            return stack.enter_context(nc.sbuf_tensor("sb%d_%s" % (_nm["i"], n), s, dt))
        sb0 = lambda n, s, dt=F32: _sbuf(es0, n, s, dt)
        pb = [es0.enter_context(nc.psum_tensor("pb%d" % i, [128, 512], F32)) for i in range(8)]
        S = Sched(nc, es0)
        st = {"bank": 0, "acc": 0, "u": 0}

        def bank():
            i = st["bank"]
            st["bank"] = (i + 1) % 6
            return pb[i], "pb%d" % i

        def accbank():
            i = 6 + st["acc"]
            st["acc"] = (st["acc"] + 1) % 2
            return pb[i], "pb%d" % i

        def uniq(p):
            st["u"] += 1
            return "%s_%d" % (p, st["u"])

        V = lambda fn, **kw: S.op("dve", fn, **kw)
        A = lambda fn, **kw: S.op("act", fn, **kw)
        G = lambda fn, **kw: S.op("pool", fn, **kw)
        P = lambda fn, **kw: S.op("pe", fn, **kw)

        def stop_here(k):
            if KSTOP == k:
                S.finish("sp")
                raise _Stop()
        ld = lambda fn, **kw: S.dma("sp", fn, **kw)
        ldc = lambda fn, **kw: S.dma("pool", fn, **kw)

        def mm(out, okey, pairs, reads):
            n = len(pairs)
            for i, (l, r) in enumerate(pairs):
                P(lambda: nc.tensor.matmul(out, lhsT=l, rhs=r, start=(i == 0), stop=(i == n - 1)),
                  reads=reads, writes=[okey], inc=(i == n - 1))

        ident = sb0("ident", [128, 128]); identb = sb0("identb", [128, 128], BF16)
        onesM = sb0("onesM", [128, 128]); epsc = sb0("epsc", [128, 1])
        binT = sb0("binT", [128, 36]); bk_bc = sb0("bk_bc", [128, 512]); bv_bc = sb0("bv_bc", [128, 512])
        cwT = sb0("cwT", [128, 4, 32]); pT12 = sb0("pT12", [128, 12])
        lamc = sb0("lamc", [128, 4]); subg_bc = sb0("subg_bc", [128, 128]); rb31 = sb0("rb31", [128, 4])
        vmk = sb0("vmk", [128, 1]); hm = sb0("hm", [128, 1]); iotc = sb0("iotc", [128, 1])
        idx = sb0("idx", [128, SS * 16], I32)
        G(lambda: nc.gpsimd.memset(ident[:], 0.0), writes=["ident"])
        G(lambda: nc.gpsimd.affine_select(out=ident[:], in_=ident[:], pattern=[[-1, 128]], compare_op=ALU.not_equal,
                                          fill=1.0, base=0, channel_multiplier=1), reads=["ident"], writes=["ident"])
        V(lambda: nc.vector.tensor_copy(out=identb[:], in_=ident[:]), reads=["ident"], writes=["identb"])
        V(lambda: nc.vector.memset(onesM[:], 1.0 / 512.0), writes=["onesM"])
        V(lambda: nc.vector.memset(epsc[:], EPS), writes=["epsc"])
        ld(lambda: nc.sync.dma_start(out=bk_bc[:], in_=b_in[KOFF:KOFF + 512].partition_broadcast(128)), writes=["bk_bc"])
        ld(lambda: nc.sync.dma_start(out=bv_bc[:], in_=b_in[VOFF:VOFF + 512].partition_broadcast(128)), writes=["bv_bc"])
        ld(lambda: nc.sync.dma_start(out=subg_bc[:], in_=subg.partition_broadcast(128)), writes=["subg_bc"])
        ld(lambda: nc.sync.dma_start(out=rb31[:], in_=rb31_d.partition_broadcast(128)), writes=["rb31"])
        ld(lambda: nc.sync.dma_start(out=vmk[:], in_=vmk_d), writes=["vmk"])
        ld(lambda: nc.sync.dma_start(out=hm[:], in_=hm_d), writes=["hm"])
        ld(lambda: nc.sync.dma_start(out=iotc[:], in_=iot), writes=["iotc"])
        V(lambda: nc.vector.tensor_scalar(out=subg_bc[:], in0=subg_bc[:], scalar1=1.0 - LAM0, scalar2=None, op0=ALU.mult),
          reads=["subg_bc"], writes=["subg_bc"])
        with ExitStack() as est:
            sbt = lambda n, s, dt=F32: _sbuf(est, n, s, dt)
            brow = sbt("brow", [36, 128]); crow = sbt("crow", [31, 512]); prow = sbt("prow", [12, 128])
            lqt = sbt("lqt", [128, 4, 64]); ptb = sbt("ptb", [128, SS * 16], I32); ptf = sbt("ptf", [128, SS * 16])
            ld(lambda: nc.sync.dma_start(out=brow[:], in_=b_in.rearrange("(c p) -> c p", p=128)), writes=["brow"])
            ld(lambda: nc.sync.dma_start(out=crow[:], in_=convw), writes=["crow"])
            for i, src in enumerate((convb, clng, clnb)):
                ld(lambda: nc.sync.dma_start(out=prow[4 * i:4 * i + 4, :], in_=src.rearrange("(c p) -> c p", p=128)), writes=["prow%d" % i])
            for i, src in enumerate((lq1, lk1, lq2, lk2)):
                ld(lambda: nc.sync.dma_start(out=lqt[:, i, :], in_=src.partition_broadcast(128)), writes=["lqt%d" % i])
            ld(lambda: nc.sync.dma_start(out=ptb[:], in_=ptab.partition_broadcast(128)), writes=["ptb"])
            b, bkey = bank()
            P(lambda: nc.tensor.transpose(out=b[:, 0:36], in_=brow[0:36, :], identity=ident[0:36, 0:36]), reads=["brow", "ident"], writes=[bkey])
            V(lambda: nc.vector.tensor_copy(out=binT[:], in_=b[:, 0:36]), reads=[bkey], writes=["binT"])
            b, bkey = bank()
            for cc in range(4):
                P(lambda: nc.tensor.transpose(out=b[:, cc * 32:cc * 32 + 31], in_=crow[0:31, cc * 128:(cc + 1) * 128],
                                              identity=ident[0:31, 0:31]), reads=["crow", "ident"], writes=[bkey], inc=(cc == 3))
            V(lambda: nc.vector.memset(cwT[:], 0.0), writes=["cwT"])
            V(lambda: nc.vector.tensor_copy(out=cwT[:, :, 0:31], in_=b[:, 0:128].rearrange("p (c w) -> p c w", w=32)[:, :, 0:31]),
              reads=[bkey], writes=["cwT"])
            b, bkey = bank()
            P(lambda: nc.tensor.transpose(out=b[:, 0:12], in_=prow[0:12, :], identity=ident[0:12, 0:12]),
              reads=["prow0", "prow1", "prow2", "ident"], writes=[bkey])
            V(lambda: nc.vector.tensor_copy(out=pT12[:], in_=b[:, 0:12]), reads=[bkey], writes=["pT12"])
            for i in range(2):
                V(lambda: nc.vector.tensor_tensor(out=lqt[:, 2 * i, :], in0=lqt[:, 2 * i, :], in1=lqt[:, 2 * i + 1, :], op=ALU.mult),
                  reads=["lqt%d" % (2 * i), "lqt%d" % (2 * i + 1)], writes=["lqt%d" % (2 * i)])
                V(lambda: nc.vector.reduce_sum(out=lamc[:, i:i + 1], in_=lqt[:, 2 * i, :], axis=AX.X), reads=["lqt%d" % (2 * i)], writes=["lam%d" % i])
                A(lambda: nc.scalar.activation(out=lamc[:, i:i + 1], in_=lamc[:, i:i + 1], func=AF.Exp), reads=["lam%d" % i], writes=["lam%d" % i])
            V(lambda: nc.vector.tensor_tensor(out=lamc[:, 2:3], in0=lamc[:, 0:1], in1=lamc[:, 1:2], op=ALU.subtract), reads=["lam0", "lam1"], writes=["lam2"])
            V(lambda: nc.vector.tensor_scalar(out=lamc[:, 3:4], in0=lamc[:, 2:3], scalar1=LAM0, scalar2=-1.0, op0=ALU.add, op1=ALU.mult),
              reads=["lam2"], writes=["nlam"])
            V(lambda: nc.vector.tensor_copy(out=ptf[:], in_=ptb[:]), reads=["ptb"], writes=["ptf"])
            V(lambda: nc.vector.tensor_scalar(out=ptf[:], in0=ptf[:], scalar1=128.0, scalar2=iotc[:, 0:1], op0=ALU.mult, op1=ALU.add),
              reads=["ptf", "iotc"], writes=["ptf"])
            V(lambda: nc.vector.tensor_copy(out=idx[:], in_=ptf[:]), reads=["ptf"], writes=["idx"])
            S.barrier()
        if KSTOP == 0:
            S.finish("sp")
            raise _Stop()
        cbT = pT12[:, 0:4]; lgT = pT12[:, 4:8]; lbT = pT12[:, 8:12]
        nlam = lamc[:, 3:4]

        ld(lambda: nc.sync.dma_start(out=nc_s[:, 0:26, :], in_=sconv[:, 4:30, :]), writes=["nc_s_a"])

        def ln_rstd(var_ap, out_ap, tmp_ap, keys_r, key_w):
            A(lambda: nc.scalar.activation(out=tmp_ap, in_=var_ap, func=AF.Sqrt, bias=epsc[0:var_ap.shape[0], 0:1], scale=1.0),
              reads=keys_r + ["epsc"], writes=[key_w + "_sd"])
            V(lambda: nc.vector.reciprocal(out=out_ap, in_=tmp_ap), reads=[key_w + "_sd"], writes=[key_w])

        def transpose_rows(src_tile, skey, rows, dst, dkeyp, dt_engine_pair=("act", "dve")):
            keys = []
            skeys = list(skey) if isinstance(skey, (list, tuple)) else [skey]
            for half in range(2):
                b, bkey = bank()
                for q in range(4):
                    kc = half * 4 + q
                    P(lambda: nc.tensor.transpose(out=b[:, q * 128:q * 128 + rows], in_=src_tile[0:rows, kc * 128:(kc + 1) * 128],
                                                  identity=ident[0:rows, 0:rows]), reads=skeys + ["ident"], writes=[bkey], inc=(q == 3))
                sv = b[:, :].rearrange("p (q r) -> p q r", r=128)[:, :, 0:rows]
                src_ap, src_key = sv, bkey
                for di, (d_, dk) in enumerate(zip(dst, dkeyp)):
                    dv = d_[:, half * 4:half * 4 + 4, 0:rows]
                    k = dk + "h%d" % half
                    if di > 0:
                        G(lambda: nc.gpsimd.tensor_copy(out=dv, in_=src_ap), reads=[src_key], writes=[k])
                    elif half == 0:
                        A(lambda: nc.scalar.copy(out=dv, in_=src_ap), reads=[src_key], writes=[k])
                    else:
                        V(lambda: nc.vector.tensor_copy(out=dv, in_=src_ap), reads=[src_key], writes=[k])
                    keys.append(k)
                    if di == 0:
                        src_ap, src_key = dv, k
            return keys

        with ExitStack() as esP:
            sbP = lambda n, s, dt=F32: _sbuf(esP, n, s, dt)
            sT = sbP("sT", [128, 4, TOK + ST], BF16)
            oT = sbP("oT", [128, 4, TOK + ST], BF16)
            bthi = sbP("bthi", [128, 5, 4, 128], BF16); btlo = sbP("btlo", [128, 5, 4, 128], BF16)
            with ExitStack() as est:
                sbt = lambda n, s, dt=F32: _sbuf(est, n, s, dt)
                braw = sbt("braw", [128, 5, 4, 128]); bmsk = sbt("bmsk", [128, 5, 4, 128]); bt32 = sbt("bt32", [128, 5, 4, 128])
                ld(lambda: nc.sync.dma_start(out=braw[:].rearrange("p a b c -> p (a b c)"), in_=braw_d), writes=["braw"])
                ld(lambda: nc.sync.dma_start(out=bmsk[:].rearrange("p a b c -> p (a b c)"), in_=bmask_d), writes=["bmsk"])
                for h in range(4):
                    V(lambda: nc.vector.tensor_scalar(out=bt32[:, :, h, :], in0=braw[:, :, h, :], scalar1=rb31[:, h:h + 1], scalar2=8.0,
                                                      op0=ALU.subtract, op1=ALU.mult), reads=["braw", "rb31"], writes=["bt32"])
                V(lambda: nc.vector.tensor_tensor(out=bt32[:], in0=bt32[:], in1=bmsk[:], op=ALU.add), reads=["bt32", "bmsk"], writes=["bt32"])
                V(lambda: nc.vector.tensor_copy(out=bthi[:], in_=bt32[:]), reads=["bt32"], writes=["bthi"])
                V(lambda: nc.vector.tensor_copy(out=braw[:], in_=bthi[:]), reads=["bthi"], writes=["braw"])
                V(lambda: nc.vector.tensor_tensor(out=bt32[:], in0=bt32[:], in1=braw[:], op=ALU.subtract), reads=["bt32", "braw"], writes=["bt32"])
                V(lambda: nc.vector.tensor_copy(out=btlo[:], in_=bt32[:]), reads=["bt32"], writes=["btlo"])
                S.barrier()

            def subln_store(o_ap, okey, rows, dst_ap, dkey, scr):
                sq, ss, sd, rs = scr
                V(lambda: nc.vector.tensor_tensor(out=sq[0:rows, :], in0=o_ap, in1=o_ap, op=ALU.mult), reads=[okey], writes=["sl_sq"])
                V(lambda: nc.vector.reduce_sum(out=ss[0:rows, :], in_=sq[0:rows, :], axis=AX.X), reads=["sl_sq"], writes=["sl_ss"])
                A(lambda: nc.scalar.activation(out=sd[0:rows, :], in_=ss[0:rows, :], func=AF.Sqrt, bias=epsc[0:rows, 0:1], scale=1.0 / 128.0),
                  reads=["sl_ss", "epsc"], writes=["sl_sd"])
                V(lambda: nc.vector.reciprocal(out=rs[0:rows, :], in_=sd[0:rows, :]), reads=["sl_sd"], writes=["sl_rs"])
                V(lambda: nc.vector.scalar_tensor_tensor(out=dst_ap, in0=o_ap, scalar=rs[0:rows, 0:1], in1=subg_bc[0:rows, :],
                                                         op0=ALU.mult, op1=ALU.mult), reads=[okey, "sl_rs", "subg_bc"], writes=[dkey])

            def combine(acc, akey, rows, osb, okey, scr2):
                r0, r1 = scr2
                V(lambda: nc.vector.reciprocal(out=r0[0:rows, :], in_=acc[0:rows, 0, 128:129]), reads=[akey], writes=["cb_r0"])
                V(lambda: nc.vector.reciprocal(out=r1[0:rows, :], in_=acc[0:rows, 1, 128:129]), reads=[akey], writes=["cb_r1"])
                V(lambda: nc.vector.tensor_tensor(out=r1[0:rows, :], in0=r1[0:rows, :], in1=nlam[0:rows, :], op=ALU.mult), reads=["cb_r1", "nlam"], writes=["cb_r1"])
                V(lambda: nc.vector.tensor_scalar(out=osb, in0=acc[0:rows, 0, 0:128], scalar1=r0[0:rows, 0:1], scalar2=None, op0=ALU.mult),
                  reads=[akey, "cb_r0"], writes=[okey])
                V(lambda: nc.vector.scalar_tensor_tensor(out=osb, in0=acc[0:rows, 1, 0:128], scalar=r1[0:rows, 0:1], in1=osb, op0=ALU.mult, op1=ALU.add),
                  reads=[akey, "cb_r1", okey], writes=[okey])

            with ExitStack() as esKV:
                sbK = lambda n, s, dt=F32: _sbuf(esKV, n, s, dt)
                KT = sbK("KT", [128, 4, L], BF16)
                VA = sbK("VA", [128, NBLK, 4, 130], BF16)
                V(lambda: nc.vector.memset(VA[:, :, :, 128:129], 1.0), writes=["VAones"])
                V(lambda: nc.vector.memset(VA[:, :, :, 129:130], 0.0), writes=["VAz"])
                with ExitStack() as es1:
                    sb1 = lambda n, s, dt=F32: _sbuf(es1, n, s, dt)
                    wk = sb1("wk", [128, 8, 512], BF16); wv = sb1("wv", [128, 8, 512], BF16); wc = sb1("wc", [128, 8, 1024], BF16)
                    xb = [sb1("xb%d" % i, [128, D]) for i in range(2)]
                    xT1 = [sb1("xT1_%d" % i, [128, 8, 512], BF16) for i in range(1)]
                    ub = sb1("ub", [128, 4, 608])
                    sig = sb1("sig", [128, 512])
                    tk = [sb1("tk%d" % i, [128, 512]) for i in range(4)]
                    ycv = sb1("ycv", [128, 4, 256]); ysq = sb1("ysq", [128, 4, 256])
                    mnb = sb1("mnb", [128, 256]); m2 = sb1("m2", [128, 256]); var = sb1("var", [128, 256]); rstd = sb1("rstd", [128, 256])
                    tt = sb1("tt", [128, 256]); nct = sb1("nct", [30, 512]); sdt = sb1("sdt", [128, 256])
                    ldc(lambda: nc.gpsimd.dma_start(out=wk[:], in_=w_in_v[:, :, KOFF:KOFF + 512]), writes=["wk"])
                    ldc(lambda: nc.gpsimd.dma_start(out=wv[:], in_=w_in_v[:, :, VOFF:VOFF + 512]), writes=["wv"])
                    ldc(lambda: nc.gpsimd.dma_start(out=wc[:], in_=w_in_v[:, :, AOFF:AOFF + 1024]), writes=["wc"])
                    V(lambda: nc.vector.memset(ub[:], 0.0), writes=["ub"])
                    tki = 0
                    for c in range(8):
                        xTc, xk = xT1[0], "xT1_0"
                        xkeys = []
                        for b_ in range(4):
                            blk = 4 * c + b_
                            xbi, xbk = xb[blk % 2], "xb%d" % (blk % 2)
                            ld(lambda: nc.sync.dma_start(out=xbi[:], in_=xloc[blk * 128:(blk + 1) * 128, :]), writes=[xbk])
                            for half in range(2):
                                bq, bqk = bank()
                                for q in range(4):
                                    kc = half * 4 + q
                                    P(lambda: nc.tensor.transpose(out=bq[:, q * 128:(q + 1) * 128], in_=xbi[:, kc * 128:(kc + 1) * 128], identity=ident[:]),
                                      reads=[xbk, "ident"], writes=[bqk], inc=(q == 3))
                                dv = xTc[:, half * 4:half * 4 + 4, b_ * 128:(b_ + 1) * 128]
                                sv = bq[:, :].rearrange("p (q r) -> p q r", r=128)
                                k = xk + "_%d_%d" % (b_, half)
                                if half == 0:
                                    A(lambda: nc.scalar.copy(out=dv, in_=sv), reads=[bqk], writes=[k])
                                else:
                                    V(lambda: nc.vector.tensor_copy(out=dv, in_=sv), reads=[bqk], writes=[k])
                                xkeys.append(k)
                        for h in range(4):
                            bq, bqk = bank()
                            mm(bq[:, :], bqk, [(wk[:, kc, h * 128:(h + 1) * 128], xTc[:, kc, :]) for kc in range(8)], xkeys + ["wk"])
                            A(lambda: nc.scalar.activation(out=KT[:, h, c * 512:(c + 1) * 512], in_=bq[:, :], func=AF.Identity,
                                                           bias=binT[:, 4 + h:5 + h], scale=1.0), reads=[bqk, "binT"], writes=[uniq("KT")])
                        for b_ in range(4):
                            blk = 4 * c + b_
                            xr = [xk + "_%d_0" % b_, xk + "_%d_1" % b_]
                            for which in range(2):
                                w_, wkey, bias_, dst = ((wk, "wk", bk_bc, nk_p), (wv, "wv", bv_bc, nv_p))[which]
                                bq, bqk = bank()
                                mm(bq[:, :], bqk, [(xTc[:, kc, b_ * 128:(b_ + 1) * 128], w_[:, kc, :]) for kc in range(8)], xr + [wkey])
                                t_, tkey = tk[tki % 4], "tk%d" % (tki % 4)
                                tki += 1
                                V(lambda: nc.vector.tensor_tensor(out=t_[:], in0=bq[:, :], in1=bias_[:], op=ALU.add), reads=[bqk, "bk_bc", "bv_bc"], writes=[tkey])
                                ld(lambda: nc.sync.dma_start(out=dst[blk * 64:(blk + 1) * 64, :], in_=t_[64:128, :]), reads=[tkey], writes=[uniq("okv")])
                                if which == 1:
                                    vak = "VA%d" % blk
                                    A(lambda: nc.scalar.copy(out=VA[:, blk, :, 0:128], in_=t_[:].rearrange("p (h v) -> p h v", v=128)),
                                      reads=[tkey, "VAones", "VAz"], writes=[vak])
                                    if blk == 0:
                                        V(lambda: nc.vector.tensor_scalar(out=VA[:, 0, :, :], in0=VA[:, 0, :, :], scalar1=vmk[:, 0:1], scalar2=None, op0=ALU.mult),
                                          reads=[vak, "vmk", "VAones", "VAz"], writes=[vak])
                        if c > 0:
                            A(lambda: nc.scalar.copy(out=ub[:, :, 0:32], in_=ub[:, :, 512:544]), reads=["ub"], writes=["ub"])
                        for cc in range(4):
                            ba_, bak = bank()
                            mm(ba_[:, :], bak, [(wc[:, kc, cc * 128:(cc + 1) * 128], xTc[:, kc, :]) for kc in range(8)], xkeys + ["wc"])
                            bg_, bgk = bank()
                            mm(bg_[:, :], bgk, [(wc[:, kc, 512 + cc * 128:512 + (cc + 1) * 128], xTc[:, kc, :]) for kc in range(8)], xkeys + ["wc"])
                            A(lambda: nc.scalar.activation(out=sig[:], in_=bg_[:, :], func=AF.Sigmoid, bias=binT[:, 16 + cc:17 + cc], scale=1.0),
                              reads=[bgk, "binT"], writes=["sig"])
                            V(lambda: nc.vector.scalar_tensor_tensor(out=ub[:, cc, 32:544], in0=ba_[:, :], scalar=binT[:, 12 + cc:13 + cc], in1=sig[:],
                                                                     op0=ALU.add, op1=ALU.mult), reads=[bak, "sig", "binT"], writes=["ub"])
                        if c == 0:
                            V(lambda: nc.vector.tensor_scalar(out=ub[:, :, 32:96], in0=ub[:, :, 32:96], scalar1=hm[:, 0:1], scalar2=None, op0=ALU.mult),
                              reads=["ub", "hm"], writes=["ub"])
                        for cc in range(4):
                            v66 = ub[:, cc, 66:578].rearrange("p (b x) -> p b x", x=128)
                            yv = ycv[:, cc, :].rearrange("p (b t) -> p b t", t=64)
                            V(lambda: nc.vector.tensor_scalar(out=yv, in0=v66[:, :, 0:64], scalar1=cwT[:, cc, 0:1], scalar2=cbT[:, cc:cc + 1],
                                                              op0=ALU.mult, op1=ALU.add), reads=["ub", "cwT", "pT12"], writes=["ycv%d" % cc])
                            for w in range(1, 31):
                                V(lambda: nc.vector.scalar_tensor_tensor(out=yv, in0=v66[:, :, w:w + 64], scalar=cwT[:, cc, w:w + 1], in1=yv,
                                                                         op0=ALU.mult, op1=ALU.add), reads=["ub", "ycv%d" % cc], writes=["ycv%d" % cc])
                        ykeys = ["ycv%d" % cc for cc in range(4)]
                        G(lambda: nc.gpsimd.tensor_tensor(out=ysq[:], in0=ycv[:], in1=ycv[:], op=ALU.mult), reads=ykeys, writes=["ysq"])
                        bm, bmk = bank()
                        mm(bm[:, 0:256], bmk, [(onesM[:], ycv[:, cc, :]) for cc in range(4)], ykeys + ["onesM"])
                        bs, bsk = bank()
                        mm(bs[:, 0:256], bsk, [(onesM[:], ysq[:, cc, :]) for cc in range(4)], ["ysq", "onesM"])
                        A(lambda: nc.scalar.copy(out=mnb[:], in_=bm[:, 0:256]), reads=[bmk], writes=["mnb"])
                        G(lambda: nc.gpsimd.tensor_tensor(out=m2[:], in0=mnb[:], in1=mnb[:], op=ALU.mult), reads=["mnb"], writes=["m2"])
                        V(lambda: nc.vector.tensor_tensor(out=var[:], in0=bs[:, 0:256], in1=m2[:], op=ALU.subtract), reads=[bsk, "m2"], writes=["var"])
                        ln_rstd(var[:], rstd[:], sdt[:], ["var"], "rstd")
                        for cc in range(4):
                            V(lambda: nc.vector.tensor_tensor(out=tt[:], in0=ycv[:, cc, :], in1=mnb[:], op=ALU.subtract), reads=["ycv%d" % cc, "mnb"], writes=["tt"])
                            G(lambda: nc.gpsimd.tensor_tensor(out=tt[:], in0=tt[:], in1=rstd[:], op=ALU.mult), reads=["tt", "rstd"], writes=["tt"])
                            A(lambda: nc.scalar.activation(out=sT[:, cc, c * 256:(c + 1) * 256], in_=tt[:], func=AF.Silu, bias=lbT[:, cc:cc + 1], scale=lgT[:, cc:cc + 1]),
                              reads=["tt", "pT12"], writes=[uniq("sT")])
                        if c == 7:
                            bq, bqk = bank()
                            for cc in range(4):
                                P(lambda: nc.tensor.transpose(out=bq[0:30, cc * 128:(cc + 1) * 128], in_=ub[:, cc, 514:544], identity=ident[:]),
                                  reads=["ub", "ident"], writes=[bqk], inc=(cc == 3))
                            V(lambda: nc.vector.tensor_copy(out=nct[:], in_=bq[0:30, :]), reads=[bqk], writes=["nct"])
                            ld(lambda: nc.sync.dma_start(out=nc_p[:, :], in_=nct[:]), reads=["nct"], writes=["nc_p"])
                    S.barrier()
                if KSTOP == 1:
                    S.finish("sp")
                    raise _Stop()
                with ExitStack() as es2:
                    sb2 = lambda n, s, dt=F32: _sbuf(es2, n, s, dt)
                    wq = sb2("wq", [128, 8, 512], BF16)
                    xo = [sb2("xo%d" % i, [128, D]) for i in range(2)]
                    xTo = [sb2("xTo%d" % i, [128, 8, 128], BF16) for i in range(2)]
                    qT = [sb2("qT%d" % i, [128, 4, 128], BF16) for i in range(2)]
                    PT = [sb2("PT%d" % i, [128, 4, 128], BF16) for i in range(3)]
                    otok = [sb2("otok%d" % i, [128, 4, 128]) for i in range(2)]
                    osb = sb2("osb", [128, 128])
                    scr = (sb2("sl_sq", [128, 128]), sb2("sl_ss", [128, 1]), sb2("sl_sd", [128, 1]), sb2("sl_rs", [128, 1]))
                    scr2 = (sb2("cb_r0", [128, 1]), sb2("cb_r1", [128, 1]))
                    ldc(lambda: nc.gpsimd.dma_start(out=wq[:], in_=w_in_v[:, :, QOFF:QOFF + 512]), writes=["wq"])
                    pti = 0
                    for I in range(NQ):
                        xoi, xok = xo[I % 2], "xo%d" % (I % 2)
                        for hh in range(2):
                            r0 = (2 * I + hh) * 128 + 64
                            ld(lambda: nc.sync.dma_start(out=xoi[hh * 64:(hh + 1) * 64, :], in_=xloc[r0:r0 + 64, :]), writes=[xok])
                        xTi, xTk = xTo[I % 2], "xTo%d" % (I % 2)
                        xkeys = transpose_rows(xoi, xok, 128, [xTi], [xTk])
                        qTi, qk = qT[I % 2], "qT%d" % (I % 2)
                        bq, bqk = bank()
                        for h in range(4):
                            for kc in range(8):
                                P(lambda: nc.tensor.matmul(bq[:, h * 128:(h + 1) * 128], lhsT=wq[:, kc, h * 128:(h + 1) * 128], rhs=xTi[:, kc, :],
                                                           start=(kc == 0), stop=(kc == 7)), reads=xkeys + ["wq"], writes=[bqk], inc=(kc == 7 and h == 3))
                        for h in range(4):
                            A(lambda: nc.scalar.activation(out=qTi[:, h, :], in_=bq[:, h * 128:(h + 1) * 128], func=AF.Identity, bias=binT[:, h:h + 1], scale=1.0),
                              reads=[bqk, "binT"], writes=[qk])
                        oti, otk = otok[I % 2], "otok%d" % (I % 2)
                        nkb = 2 * I + 2
                        for h in range(4):
                            ab, abk = accbank()
                            acc = ab[:, 0:260].rearrange("p (m v) -> p m v", v=130)
                            for m in range(2):
                                ps_ = slice(m * 64, (m + 1) * 64)
                                for g0 in range(0, nkb, 4):
                                    grp = list(range(g0, min(g0 + 4, nkb)))
                                    sbk_, sbkk = bank()
                                    for j, kb in enumerate(grp):
                                        r = kb - (2 * I - 1)
                                        special = 0 <= r <= 2
                                        osl = sbk_[:, j * 128:(j + 1) * 128]
                                        last = (j == len(grp) - 1)
                                        P(lambda: nc.tensor.matmul(osl, lhsT=KT[ps_, h, kb * 128:(kb + 1) * 128], rhs=qTi[ps_, h, :], start=True, stop=not special),
                                          reads=[qk], writes=[sbkk], inc=(last and not special))
                                        if special:
                                            P(lambda: nc.tensor.matmul(osl, lhsT=identb[:], rhs=bthi[:, r, h, :], start=False, stop=False),
                                              reads=["identb"], writes=[sbkk], inc=False)
                                            P(lambda: nc.tensor.matmul(osl, lhsT=identb[:], rhs=btlo[:, r, h, :], start=False, stop=True),
                                              reads=[], writes=[sbkk], inc=last)
                                    n = len(grp)
                                    pt_, ptk = PT[pti % 3], "PT%d" % (pti % 3)
                                    pti += 1
                                    A(lambda: nc.scalar.activation(out=pt_[:, 0:n, :], in_=sbk_[:, 0:n * 128].rearrange("p (j q) -> p j q", q=128),
                                                                   func=AF.Exp, scale=0.125), reads=[sbkk], writes=[ptk])
                                    for j, kb in enumerate(grp):
                                        P(lambda: nc.tensor.matmul(acc[:, m, :], lhsT=pt_[:, j, :], rhs=VA[:, kb, h, :], start=(kb == 0), stop=(kb == nkb - 1)),
                                          reads=[ptk], writes=[abk], inc=(j == n - 1))
                            combine(acc, abk, 128, osb[:], "osb", scr2)
                            subln_store(osb[:], "osb", 128, oti[:, h, :], otk, scr)
                        bq, bqk = bank()
                        for h in range(4):
                            P(lambda: nc.tensor.transpose(out=bq[:, h * 128:(h + 1) * 128], in_=oti[:, h, :], identity=ident[:]),
                              reads=[otk, "ident"], writes=[bqk], inc=(h == 3))
                        A(lambda: nc.scalar.copy(out=oT[:, :, I * 128:(I + 1) * 128], in_=bq[:, :].rearrange("p (h q) -> p h q", q=128)),
                          reads=[bqk], writes=[uniq("oT")])
                    S.barrier()
                if KSTOP == 2:
                    S.finish("sp")
                    raise _Stop()
            with ExitStack() as esS:
                sbS = lambda n, s, dt=F32: _sbuf(esS, n, s, dt)
                wq = sbS("wq", [128, 8, 512], BF16); wk = sbS("wk", [128, 8, 512], BF16)
                wv = sbS("wv", [128, 8, 512], BF16); wc = sbS("wc", [128, 8, 1024], BF16)
                xss = sbS("xss", [128, D]); xTs = sbS("xTs", [128, 8, 128], BF16)
                qTs = sbS("qTs", [128, 4, ST], BF16); kTs = sbS("kTs", [128, 4, ST], BF16)
                tks = sbS("tks", [128, 512]); vnew = sbS("vnew", [4, SS, 4, 130], BF16)
                uT = sbS("uT", [128, 4, ST]); sig = sbS("sig", [128, ST]); utok = sbS("utok", [ST, 512])
                cbuf = sbS("cbuf", [128, 4, SS, 34]); scv = [sbS("scv%d" % i, [120, 512]) for i in range(1)]
                ycs = sbS("ycs", [128, 4, ST]); ysqs = sbS("ysqs", [128, 4, ST])
                mnb = sbS("mnb", [128, ST]); m2 = sbS("m2", [128, ST]); var = sbS("var", [128, ST]); rstd = sbS("rstd", [128, ST]); tt = sbS("tt", [128, ST]); sdt = sbS("sdt", [128, ST])
                kpg = [sbS("kpg%d" % i, [128, 512]) for i in range(2)]
                vpg = [sbS("vpg%d" % i, [128, 512], BF16) for i in range(2)]
                KTs2 = [sbS("KTs%d" % i, [128, 4, 16, 128], BF16) for i in range(2)]
                VAs2 = [sbS("VAs%d" % i, [128, 16, 4, 130], BF16) for i in range(2)]
                PTs = [sbS("PTs%d" % i, [128, 16, 4], BF16) for i in range(2)]
                PTn = [sbS("PTn%d" % i, [4, 4], BF16) for i in range(2)]
                os_ = sbS("os_", [4, 128]); otoks = sbS("otoks", [4, 4, 128])
                scr = (sbS("sl_sq", [128, 128]), sbS("sl_ss", [128, 1]), sbS("sl_sd", [128, 1]), sbS("sl_rs", [128, 1]))
                scr2 = (sbS("cb_r0", [128, 1]), sbS("cb_r1", [128, 1]))
                for wt, off, n_, nm in ((wq, QOFF, 512, "wq"), (wk, KOFF, 512, "wk"), (wv, VOFF, 512, "wv"), (wc, AOFF, 1024, "wc")):
                    ldc(lambda: nc.gpsimd.dma_start(out=wt[:], in_=w_in_v[:, :, off:off + n_]), writes=[nm])
                for VAs in VAs2:
                    V(lambda: nc.vector.memset(VAs[:, :, :, 128:129], 1.0), writes=["VAs1"])
                    V(lambda: nc.vector.memset(VAs[:, :, :, 129:130], 0.0), writes=["VAs0"])
                V(lambda: nc.vector.memset(vnew[:, :, :, 128:129], 1.0), writes=["vn1"])
                V(lambda: nc.vector.memset(vnew[:, :, :, 129:130], 0.0), writes=["vn0"])
                ld(lambda: nc.sync.dma_start(out=xss[0:ST, :], in_=xs[:, :]), writes=["xss"])
                xkeys = transpose_rows(xss, "xss", ST, [xTs], ["xTs"])
                xc = xTs
                for (w_, wkey, dstT, dk, c0) in ((wq, "wq", qTs, "qTs", 0), (wk, "wk", kTs, "kTs", 4)):
                    bq, bqk = bank()
                    for h in range(4):
                        for kc in range(8):
                            P(lambda: nc.tensor.matmul(bq[:, h * ST:(h + 1) * ST], lhsT=w_[:, kc, h * 128:(h + 1) * 128], rhs=xc[:, kc, 0:ST],
                                                       start=(kc == 0), stop=(kc == 7)), reads=xkeys + [wkey], writes=[bqk], inc=(kc == 7 and h == 3))
                    for h in range(4):
                        A(lambda: nc.scalar.activation(out=dstT[:, h, :], in_=bq[:, h * ST:(h + 1) * ST], func=AF.Identity, bias=binT[:, c0 + h:c0 + h + 1], scale=1.0),
                          reads=[bqk, "binT"], writes=[dk])
                for (w_, wkey, bias_, dst) in ((wk, "wk", bk_bc, nk_s), (wv, "wv", bv_bc, nv_s)):
                    bq, bqk = bank()
                    mm(bq[0:ST, :], bqk, [(xc[:, kc, 0:ST], w_[:, kc, :]) for kc in range(8)], xkeys + [wkey])
                    V(lambda: nc.vector.tensor_tensor(out=tks[0:ST, :], in0=bq[0:ST, :], in1=bias_[0:ST, :], op=ALU.add), reads=[bqk, "bk_bc", "bv_bc"], writes=["tks"])
                    ld(lambda: nc.sync.dma_start(out=dst[:, :], in_=tks[0:ST, :]), reads=["tks"], writes=[uniq("oks")])
                for s in range(SS):
                    bq, bqk = bank()
                    mm(bq[0:4, :], bqk, [(xc[:, kc, 4 * s:4 * s + 4], wv[:, kc, :]) for kc in range(8)], xkeys + ["wv"])
                    V(lambda: nc.vector.tensor_tensor(out=vnew[0:4, s, :, 0:128], in0=bq[0:4, :].rearrange("p (h v) -> p h v", v=128),
                                                      in1=bv_bc[0:4, :].rearrange("p (h v) -> p h v", v=128), op=ALU.add),
                      reads=[bqk, "bv_bc", "vn1", "vn0"], writes=["vnew"])
                for cc in range(4):
                    ba_, bak = bank()
                    mm(ba_[:, 0:ST], bak, [(wc[:, kc, cc * 128:(cc + 1) * 128], xc[:, kc, 0:ST]) for kc in range(8)], xkeys + ["wc"])
                    bg_, bgk = bank()
                    mm(bg_[:, 0:ST], bgk, [(wc[:, kc, 512 + cc * 128:512 + (cc + 1) * 128], xc[:, kc, 0:ST]) for kc in range(8)], xkeys + ["wc"])
                    A(lambda: nc.scalar.activation(out=sig[:], in_=bg_[:, 0:ST], func=AF.Sigmoid, bias=binT[:, 16 + cc:17 + cc], scale=1.0), reads=[bgk, "binT"], writes=["sig"])
                    V(lambda: nc.vector.scalar_tensor_tensor(out=uT[:, cc, :], in0=ba_[:, 0:ST], scalar=binT[:, 12 + cc:13 + cc], in1=sig[:], op0=ALU.add, op1=ALU.mult),
                      reads=[bak, "sig", "binT"], writes=["uT"])
                bq, bqk = bank()
                for cc in range(4):
                    P(lambda: nc.tensor.transpose(out=bq[0:ST, cc * 128:(cc + 1) * 128], in_=uT[:, cc, :], identity=ident[:]), reads=["uT", "ident"], writes=[bqk], inc=(cc == 3))
                V(lambda: nc.vector.tensor_copy(out=utok[:], in_=bq[0:ST, :]), reads=[bqk], writes=["utok"])
                for s in range(SS):
                    ld(lambda: nc.sync.dma_start(out=nc_s[s, 26:30, :], in_=utok[4 * s:4 * s + 4, :]), reads=["utok"], writes=["nc_s_b%d" % s])
                for g in range(4):
                    sc, sck = scv[0], "scv0"
                    ld(lambda: nc.sync.dma_start(out=sc[:], in_=sconv[4 * g:4 * g + 4].rearrange("s r c -> (s r) c")), writes=[sck])
                    for cc in range(4):
                        bq, bqk = bank()
                        P(lambda: nc.tensor.transpose(out=bq[:, 0:120], in_=sc[0:120, cc * 128:(cc + 1) * 128], identity=ident[0:120, 0:120]), reads=[sck, "ident"], writes=[bqk])
                        A(lambda: nc.scalar.copy(out=cbuf[:, cc, 4 * g:4 * g + 4, 0:30], in_=bq[:, 0:120].rearrange("p (s r) -> p s r", r=30)), reads=[bqk], writes=[uniq("cbuf")])
                S.barrier()
                V(lambda: nc.vector.tensor_copy(out=cbuf[:, :, :, 30:34], in_=uT[:].rearrange("p c (s t) -> p c s t", t=4)), reads=[], writes=["cbuf"])
                for cc in range(4):
                    yv = ycs[:, cc, :].rearrange("p (s t) -> p s t", t=4)
                    V(lambda: nc.vector.tensor_scalar(out=yv, in0=cbuf[:, cc, :, 0:4], scalar1=cwT[:, cc, 0:1], scalar2=cbT[:, cc:cc + 1], op0=ALU.mult, op1=ALU.add),
                      reads=["cbuf"], writes=["ycs%d" % cc])
                    for w in range(1, 31):
                        V(lambda: nc.vector.scalar_tensor_tensor(out=yv, in0=cbuf[:, cc, :, w:w + 4], scalar=cwT[:, cc, w:w + 1], in1=yv, op0=ALU.mult, op1=ALU.add),
                          reads=["cbuf", "ycs%d" % cc], writes=["ycs%d" % cc])
                ykeys = ["ycs%d" % cc for cc in range(4)]
                G(lambda: nc.gpsimd.tensor_tensor(out=ysqs[:], in0=ycs[:], in1=ycs[:], op=ALU.mult), reads=ykeys, writes=["ysqs"])
                bm, bmk = bank()
                mm(bm[:, 0:ST], bmk, [(onesM[:], ycs[:, cc, :]) for cc in range(4)], ykeys)
                bs, bsk = bank()
                mm(bs[:, 0:ST], bsk, [(onesM[:], ysqs[:, cc, :]) for cc in range(4)], ["ysqs"])
                A(lambda: nc.scalar.copy(out=mnb[:], in_=bm[:, 0:ST]), reads=[bmk], writes=["mnb"])
                G(lambda: nc.gpsimd.tensor_tensor(out=m2[:], in0=mnb[:], in1=mnb[:], op=ALU.mult), reads=["mnb"], writes=["m2"])
                V(lambda: nc.vector.tensor_tensor(out=var[:], in0=bs[:, 0:ST], in1=m2[:], op=ALU.subtract), reads=[bsk, "m2"], writes=["var"])
                ln_rstd(var[:], rstd[:], sdt[:], ["var"], "rstd")
                for cc in range(4):
                    V(lambda: nc.vector.tensor_tensor(out=tt[:], in0=ycs[:, cc, :], in1=mnb[:], op=ALU.subtract), reads=["ycs%d" % cc, "mnb"], writes=["tt"])
                    G(lambda: nc.gpsimd.tensor_tensor(out=tt[:], in0=tt[:], in1=rstd[:], op=ALU.mult), reads=["tt", "rstd"], writes=["tt"])
                    A(lambda: nc.scalar.activation(out=sT[:, cc, TOK:TOK + ST], in_=tt[:], func=AF.Silu, bias=lbT[:, cc:cc + 1], scale=lgT[:, cc:cc + 1]),
                      reads=["tt"], writes=[uniq("sTs")])
                kpi = 0
                for s in range(SS):
                    KTs, VAs, sb_ = KTs2[s % 2], VAs2[s % 2], s % 2
                    for pg in range(16):
                        col = s * 16 + pg
                        kp, kpk = kpg[kpi % 2], "kpg%d" % (kpi % 2)
                        kpi += 1
                        ldc(lambda: nc.gpsimd.indirect_dma_start(out=kp[:, :], out_offset=None, in_=ck, in_offset=bass.IndirectOffsetOnAxis(ap=idx[:, col:col + 1], axis=0)),
                            reads=["idx"], writes=[kpk])
                        vp, vpk = vpg[(kpi - 1) % 2], "vpg%d" % ((kpi - 1) % 2)
                        ldc(lambda: nc.gpsimd.indirect_dma_start(out=vp[:, :], out_offset=None, in_=cv, in_offset=bass.IndirectOffsetOnAxis(ap=idx[:, col:col + 1], axis=0)),
                            reads=["idx"], writes=[vpk])
                        G(lambda: nc.gpsimd.tensor_copy(out=VAs[:, pg, :, 0:128], in_=vp[:, :].rearrange("p (h v) -> p h v", v=128)),
                          reads=[vpk, "VAs1", "VAs0"], writes=["VAs%d_%d" % (sb_, pg)])
                        bq, bqk = bank()
                        for h in range(4):
                            P(lambda: nc.tensor.transpose(out=bq[:, h * 128:(h + 1) * 128], in_=kp[:, h * 128:(h + 1) * 128], identity=ident[:]),
                              reads=[kpk, "ident"], writes=[bqk], inc=(h == 3))
                        cpk = "KTs%d_%d" % (sb_, pg)
                        if pg % 2 == 0:
                            A(lambda: nc.scalar.copy(out=KTs[:, :, pg, :], in_=bq[:, :].rearrange("p (h k) -> p h k", k=128)), reads=[bqk], writes=[cpk])
                        else:
                            V(lambda: nc.vector.tensor_copy(out=KTs[:, :, pg, :], in_=bq[:, :].rearrange("p (h k) -> p h k", k=128)), reads=[bqk], writes=[cpk])
                    ktkeys = ["KTs%d_%d" % (sb_, pg) for pg in range(16)]
                    vakeys = ["VAs%d_%d" % (sb_, pg) for pg in range(16)]
                    qs = slice(4 * s, 4 * s + 4)
                    for h in range(4):
                        ab, abk = accbank()
                        acc = ab[:, 0:260].rearrange("p (m v) -> p m v", v=130)
                        for m in range(2):
                            ps_ = slice(m * 64, (m + 1) * 64)
                            sbk_, sbkk = bank()
                            for pg in range(16):
                                osl = sbk_[:, pg * 4:(pg + 1) * 4]
                                sp_ = (pg == 15)
                                P(lambda: nc.tensor.matmul(osl, lhsT=KTs[ps_, h, pg, :], rhs=qTs[ps_, h, qs], start=True, stop=not sp_),
                                  reads=ktkeys + ["qTs"], writes=[sbkk], inc=False)
                                if sp_:
                                    P(lambda: nc.tensor.matmul(osl, lhsT=identb[:], rhs=bthi[:, 3, h, 0:4], start=False, stop=False), reads=[], writes=[sbkk], inc=False)
                                    P(lambda: nc.tensor.matmul(osl, lhsT=identb[:], rhs=btlo[:, 3, h, 0:4], start=False, stop=True), reads=[], writes=[sbkk], inc=False)
                            osn = sbk_[0:4, 64:68]
                            P(lambda: nc.tensor.matmul(osn, lhsT=kTs[ps_, h, qs], rhs=qTs[ps_, h, qs], start=True, stop=False), reads=["kTs", "qTs"], writes=[sbkk], inc=False)
                            P(lambda: nc.tensor.matmul(osn, lhsT=identb[0:4, 0:4], rhs=bthi[0:4, 4, h, 0:4], start=False, stop=False), reads=[], writes=[sbkk], inc=False)
                            P(lambda: nc.tensor.matmul(osn, lhsT=identb[0:4, 0:4], rhs=btlo[0:4, 4, h, 0:4], start=False, stop=True), reads=[], writes=[sbkk], inc=True)
                            pi = (2 * h + m) % 2
                            pt_, ptk = PTs[pi], "PTs%d" % pi
                            pn_, pnk = PTn[pi], "PTn%d" % pi
                            A(lambda: nc.scalar.activation(out=pt_[:], in_=sbk_[:, 0:64].rearrange("p (g q) -> p g q", q=4), func=AF.Exp, scale=0.125), reads=[sbkk], writes=[ptk])
                            A(lambda: nc.scalar.activation(out=pn_[:], in_=sbk_[0:4, 64:68], func=AF.Exp, scale=0.125), reads=[sbkk], writes=[pnk])
                            for pg in range(16):
                                P(lambda: nc.tensor.matmul(acc[0:4, m, :], lhsT=pt_[:, pg, :], rhs=VAs[:, pg, h, :], start=(pg == 0), stop=False),
                                  reads=[ptk] + vakeys, writes=[abk], inc=False)
                            P(lambda: nc.tensor.matmul(acc[0:4, m, :], lhsT=pn_[:], rhs=vnew[0:4, s, h, :], start=False, stop=True), reads=[pnk, "vnew"], writes=[abk], inc=True)
                        combine(acc, abk, 4, os_[:], "os_", scr2)
                        subln_store(os_[:], "os_", 4, otoks[0:4, h, :], "otoks", scr)
                    bq, bqk = bank()
                    for h in range(4):
                        P(lambda: nc.tensor.transpose(out=bq[:, h * 4:(h + 1) * 4], in_=otoks[0:4, h, :], identity=ident[0:4, 0:4]), reads=["otoks", "ident"], writes=[bqk], inc=(h == 3))
                    V(lambda: nc.vector.tensor_copy(out=oT[:, :, TOK + 4 * s:TOK + 4 * s + 4], in_=bq[:, 0:16].rearrange("p (h q) -> p h q", q=4)), reads=[bqk], writes=[uniq("oTs")])
                S.barrier()
                if KSTOP == 3:
                    S.finish("sp")
                    raise _Stop()
            with ExitStack() as es3:
                sb3 = lambda n, s, dt=F32: _sbuf(es3, n, s, dt)
                wg = sb3("wg", [128, 8, 2048], BF16); wap = sb3("wap", [128, 4, D], BF16); wcp = sb3("wcp", [128, 4, D], BF16)
                wout = sb3("wout", [128, 8, D], BF16)
                bgg = sb3("bgg", [128, 2048]); bcpb = sb3("bcpb", [128, D]); g1b = sb3("g1b", [128, D]); b1b = sb3("b1b", [128, D])
                rw = sb3("rw", [128, 8, 32]); rbb = sb3("rbb", [128, 32]); b2all = sb3("b2all", [32, D])
                xo = [sb3("xo%d" % i, [128, D]) for i in range(2)]
                xTo = sb3("xTo", [128, 8, 128], BF16)
                sgt = sb3("sgt", [128, 2048]); mt = sb3("mt", [128, D]); t2 = sb3("t2", [128, 512]); mT = sb3("mT", [128, 8, 128], BF16)
                pre = sb3("pre", [128, D]); h1 = sb3("h1", [128, D]); ya = sb3("ya", [128, D])
                h1Tf = sb3("h1Tf", [128, 8, 128]); h1Tb = sb3("h1Tb", [128, 8, 128], BF16)
                bn = sb3("bn", [128, 2, 6]); mv = sb3("mv", [128, 2]); sd = sb3("sd", [128, 1]); rs1 = sb3("rs1", [128, 1])
                lg = sb3("lg", [128, 32]); t8 = sb3("t8", [128, 8]); msk = sb3("msk", [128, 32]); nmx = sb3("nmx", [128, 1])
                ex = sb3("ex", [128, 32]); ssum = sb3("ssum", [128, 1]); gt = sb3("gt", [128, 32]); gtT = sb3("gtT", [32, 128])
                ldc(lambda: nc.gpsimd.dma_start(out=wg[:], in_=w_in_v[:, :, GAOFF:GAOFF + 2048]), writes=["wg"])
                ldc(lambda: nc.gpsimd.dma_start(out=wap[:], in_=wap_d.rearrange("(c p) n -> p c n", p=128)), writes=["wap"])
                ldc(lambda: nc.gpsimd.dma_start(out=wcp[:], in_=wcp_d.rearrange("(c p) n -> p c n", p=128)), writes=["wcp"])
                ldc(lambda: nc.gpsimd.dma_start(out=wout[:], in_=wout_d.rearrange("(c p) n -> p c n", p=128)), writes=["wout"])
                ld(lambda: nc.sync.dma_start(out=bgg[:], in_=b_in[GAOFF:GAOFF + 2048].partition_broadcast(128)), writes=["bgg"])
                ld(lambda: nc.sync.dma_start(out=bcpb[:], in_=bcp_d.partition_broadcast(128)), writes=["bcpb"])
                ld(lambda: nc.sync.dma_start(out=g1b[:], in_=ln1g.partition_broadcast(128)), writes=["g1b"])
                ld(lambda: nc.sync.dma_start(out=b1b[:], in_=ln1b.partition_broadcast(128)), writes=["b1b"])
                ld(lambda: nc.sync.dma_start(out=rw[:], in_=rw_d.rearrange("(c p) e -> p c e", p=128)), writes=["rw"])
                ld(lambda: nc.sync.dma_start(out=rbb[:], in_=rbias.partition_broadcast(128)), writes=["rbb"])
                ld(lambda: nc.sync.dma_start(out=b2all[:], in_=b2_d), writes=["b2all"])
                V(lambda: nc.vector.memset(h1Tb[:], 0.0), writes=["h1Tbh0", "h1Tbh1"])
                V(lambda: nc.vector.memset(h1Tf[:], 0.0), writes=["h1Tfh0", "h1Tfh1"])
                for I in range(NTILE):
                    rows = 128 if I < NQ else ST
                    R = slice(0, rows)
                    cols = slice(I * 128, I * 128 + rows)
                    xoi, xok = xo[I % 2], "xo%d" % (I % 2)
                    if I < NQ:
                        for hh in range(2):
                            r0 = (2 * I + hh) * 128 + 64
                            ld(lambda: nc.sync.dma_start(out=xoi[hh * 64:(hh + 1) * 64, :], in_=xloc[r0:r0 + 64, :]), writes=[xok])
                    else:
                        ld(lambda: nc.sync.dma_start(out=xoi[R, :], in_=xs[:, :]), writes=[xok])
                    xkeys = transpose_rows(xoi, xok, rows, [xTo], ["xTo"])
                    for p_ in range(4):
                        bq, bqk = bank()
                        mm(bq[R, :], bqk, [(xTo[:, kc, R], wg[:, kc, p_ * 512:(p_ + 1) * 512]) for kc in range(8)], xkeys + ["wg"])
                        V(lambda: nc.vector.tensor_tensor(out=sgt[R, p_ * 512:(p_ + 1) * 512], in0=bq[R, :], in1=bgg[R, p_ * 512:(p_ + 1) * 512], op=ALU.add),
                          reads=[bqk, "bgg"], writes=["sgt"])
                    A(lambda: nc.scalar.activation(out=sgt[R, :], in_=sgt[R, :], func=AF.Sigmoid), reads=["sgt"], writes=["sgt"])
                    stop_here(41)
                    for half in range(2):
                        hs = slice(half * 512, (half + 1) * 512)
                        bq, bqk = bank()
                        mm(bq[R, :], bqk, [(oT[:, h, cols], wap[:, h, hs]) for h in range(4)], ["wap"])
                        V(lambda: nc.vector.tensor_tensor(out=mt[R, hs], in0=bq[R, :], in1=sgt[R, hs], op=ALU.mult), reads=[bqk, "sgt"], writes=["mt%d" % half])
                        bq, bqk = bank()
                        mm(bq[R, :], bqk, [(sT[:, cc, cols], wcp[:, cc, hs]) for cc in range(4)], ["wcp"])
                        V(lambda: nc.vector.tensor_tensor(out=t2[R, :], in0=bq[R, :], in1=bcpb[R, hs], op=ALU.add), reads=[bqk, "bcpb"], writes=["t2"])
                        G(lambda: nc.gpsimd.tensor_tensor(out=t2[R, :], in0=t2[R, :], in1=sgt[R, 1024 + half * 512:1024 + (half + 1) * 512], op=ALU.mult),
                          reads=["t2", "sgt"], writes=["t2"])
                        G(lambda: nc.gpsimd.tensor_tensor(out=mt[R, hs], in0=mt[R, hs], in1=t2[R, :], op=ALU.add), reads=["t2", "mt%d" % half], writes=["mt%d" % half])
                    stop_here(42)
                    mkeys = transpose_rows(mt, ["mt0", "mt1"], rows, [mT], ["mT"])
                    for half in range(2):
                        hs = slice(half * 512, (half + 1) * 512)
                        bq, bqk = bank()
                        mm(bq[R, :], bqk, [(mT[:, kc, R], wout[:, kc, hs]) for kc in range(8)], mkeys + ["wout"])
                        V(lambda: nc.vector.scalar_tensor_tensor(out=pre[R, hs], in0=xoi[R, hs], scalar=ALPHA, in1=bq[R, :], op0=ALU.mult, op1=ALU.add),
                          reads=[bqk, xok], writes=["pre%d" % half])
                        V(lambda: nc.vector.bn_stats(out=bn[R, half, :], in_=pre[R, hs]), reads=["pre%d" % half], writes=["bn%d" % half])
                    V(lambda: nc.vector.bn_aggr(out=mv[R, :], in_=bn[R, :, :]), reads=["bn0", "bn1"], writes=["mv"])
                    ln_rstd(mv[R, 1:2], rs1[R, :], sd[R, :], ["mv"], "rs1")
                    V(lambda: nc.vector.tensor_scalar(out=h1[R, :], in0=pre[R, :], scalar1=mv[R, 0:1], scalar2=rs1[R, 0:1], op0=ALU.subtract, op1=ALU.mult),
                      reads=["pre0", "pre1", "mv", "rs1"], writes=["h1"])
                    G(lambda: nc.gpsimd.tensor_tensor(out=h1[R, :], in0=h1[R, :], in1=g1b[R, :], op=ALU.mult), reads=["h1", "g1b"], writes=["h1"])
                    G(lambda: nc.gpsimd.tensor_tensor(out=h1[R, :], in0=h1[R, :], in1=b1b[R, :], op=ALU.add), reads=["h1", "b1b"], writes=["h1"])
                    stop_here(43)
                    hkeys = transpose_rows(h1, "h1", rows, [h1Tf, h1Tb], ["h1Tf", "h1Tb"])
                    stop_here(44)
                    ld(lambda: nc.sync.dma_start(out=h1T_scr[:, :, I * 128:(I + 1) * 128], in_=h1Tb[:]), reads=["h1Tbh0", "h1Tbh1"], writes=[uniq("h1Ts")])
                    bq, bqk = bank()
                    mm(bq[R, 0:32], bqk, [(h1Tf[:, kc, R], rw[:, kc, :]) for kc in range(8)], ["h1Tfh0", "h1Tfh1", "rw"])
                    V(lambda: nc.vector.tensor_tensor(out=lg[R, :], in0=bq[R, 0:32], in1=rbb[R, :], op=ALU.add), reads=[bqk, "rbb"], writes=["lg"])
                    V(lambda: nc.vector.max(out=t8[R, :], in_=lg[R, :]), reads=["lg"], writes=["t8"])
                    V(lambda: nc.vector.tensor_scalar(out=msk[R, :], in0=lg[R, :], scalar1=t8[R, 3:4], scalar2=None, op0=ALU.is_ge), reads=["lg", "t8"], writes=["msk"])
                    V(lambda: nc.vector.tensor_scalar(out=nmx[R, :], in0=t8[R, 0:1], scalar1=-1.0, scalar2=None, op0=ALU.mult), reads=["t8"], writes=["nmx"])
                    A(lambda: nc.scalar.activation(out=ex[R, :], in_=lg[R, :], func=AF.Exp, bias=nmx[R, 0:1], scale=1.0), reads=["lg", "nmx"], writes=["ex"])
                    V(lambda: nc.vector.tensor_tensor(out=ex[R, :], in0=ex[R, :], in1=msk[R, :], op=ALU.mult), reads=["ex", "msk"], writes=["ex"])
                    V(lambda: nc.vector.reduce_sum(out=ssum[R, :], in_=ex[R, :], axis=AX.X), reads=["ex"], writes=["ssum"])
                    V(lambda: nc.vector.reciprocal(out=ssum[R, :], in_=ssum[R, :]), reads=["ssum"], writes=["ssum"])
                    V(lambda: nc.vector.tensor_scalar(out=gt[R, :], in0=ex[R, :], scalar1=ssum[R, 0:1], scalar2=None, op0=ALU.mult), reads=["ex", "ssum"], writes=["gt"])
                    stop_here(45)
                    ld(lambda: nc.sync.dma_start(out=gates_scr[I, R, :], in_=gt[R, :]), reads=["gt"], writes=[uniq("gts")])
                    bq, bqk = bank()
                    P(lambda: nc.tensor.transpose(out=bq[0:32, 0:rows], in_=gt[R, :], identity=ident[R, R]), reads=["gt", "ident"], writes=[bqk])
                    A(lambda: nc.scalar.copy(out=gtT[:, R], in_=bq[0:32, 0:rows]), reads=[bqk], writes=["gtT"])
                    for half in range(2):
                        hs = slice(half * 512, (half + 1) * 512)
                        bq, bqk = bank()
                        mm(bq[R, :], bqk, [(gtT[:, R], b2all[:, hs])], ["gtT", "b2all"])
                        V(lambda: nc.vector.scalar_tensor_tensor(out=ya[R, hs], in0=h1[R, hs], scalar=ALPHA, in1=bq[R, :], op0=ALU.mult, op1=ALU.add),
                          reads=[bqk, "h1"], writes=["ya%d" % half])
                    ld(lambda: nc.sync.dma_start(out=yacc_scr[I, R, :], in_=ya[R, :]), reads=["ya0", "ya1"], writes=[uniq("yas")])
                    stop_here(46)
                    if I == 15:
                        stop_here(47)
                S.barrier()
                if KSTOP == 4:
                    S.finish("sp")
                    raise _Stop()
        with ExitStack() as esB:
            sbB = lambda n, s, dt=F32: _sbuf(esB, n, s, dt)
            b1T = sbB("b1T", [128, 16, 32])
            with ExitStack() as est:
                b1rows = _sbuf(est, "b1rows", [32, 2048], F32)
                ld(lambda: nc.sync.dma_start(out=b1rows[:], in_=b1_d), writes=["b1rows"])
                bq, bqk = bank()
                for c_ in range(16):
                    P(lambda: nc.tensor.transpose(out=bq[:, c_ * 32:(c_ + 1) * 32], in_=b1rows[0:32, c_ * 128:(c_ + 1) * 128], identity=ident[0:32, 0:32]),
                      reads=["b1rows", "ident"], writes=[bqk], inc=(c_ == 15))
                V(lambda: nc.vector.tensor_copy(out=b1T[:], in_=bq[:, :].rearrange("p (c e) -> p c e", e=32)), reads=[bqk], writes=["b1T"])
                S.barrier()
            b1T1 = sbB("b1T1", [128, 8, 32])
            V(lambda: nc.vector.tensor_scalar(out=b1T1[:], in0=b1T[:, 8:16, :], scalar1=1.0, scalar2=None, op0=ALU.add), writes=["b1T1"])
            w1t = [sbB("w1t%d" % i, [128, 8, 2048], BF16) for i in range(2)]
            w2t = [sbB("w2t%d" % i, [128, 8, D], BF16) for i in range(2)]
            h1Th = sbB("h1Th", [128, 8, 9 * 128], BF16); yacc = sbB("yacc", [128, 9, D]); gts = sbB("gts", [128, 9, 32])
            g2b = sbB("g2b", [128, D]); b2b = sbB("b2b", [128, D])
            g32 = [sbB("g32_%d" % i, [128, 512]) for i in range(2)]; sg32 = [sbB("sg32_%d" % i, [128, 512]) for i in range(2)]
            u32 = [sbB("u32_%d" % i, [128, 512]) for i in range(2)]
            actT = [sbB("actT%d" % i, [128, 8, 512], BF16) for i in range(2)]
            bn = sbB("bn", [128, 2, 6]); mv = sbB("mv", [128, 2]); sd = sbB("sd", [128, 1]); rs1 = sbB("rs1", [128, 1]); yo = [sbB("yo%d" % i, [128, D]) for i in range(2)]
            ld(lambda: nc.sync.dma_start(out=g2b[:], in_=ln2g.partition_broadcast(128)), writes=["g2b"])
            ld(lambda: nc.sync.dma_start(out=b2b[:], in_=ln2b.partition_broadcast(128)), writes=["b2b"])
            def load_expert(e_, slot):
                w1e_, w1k_ = w1t[slot % 2], "w1t%d" % (slot % 2)
                w2e_, w2k_ = w2t[slot % 2], "w2t%d" % (slot % 2)
                for q in range(4):
                    ldc(lambda: nc.gpsimd.dma_start(out=w1e_[:, :, q * 512:(q + 1) * 512], in_=w1_d[e_].rearrange("(c p) f -> p c f", p=128)[:, :, q * 512:(q + 1) * 512]),
                        writes=[w1k_ + "_%d" % q])
                for q in range(2):
                    ldc(lambda: nc.gpsimd.dma_start(out=w2e_[:, :, q * 512:(q + 1) * 512], in_=w2_d[e_].rearrange("(c p) f -> p c f", p=128)[:, :, q * 512:(q + 1) * 512]),
                        writes=[w2k_ + "_%d" % q])

            for hf, tiles in enumerate(HALVES):
                nt = len(tiles)
                ncols = nt * 128
                t0 = tiles[0]
                ld(lambda: nc.sync.dma_start(out=h1Th[:, :, 0:ncols], in_=h1T_scr[:, :, t0 * 128:t0 * 128 + ncols]), writes=["h1Th"])
                ld(lambda: nc.sync.dma_start(out=yacc[:, 0:nt, :], in_=yacc_scr[t0:t0 + nt].rearrange("t p d -> p t d")), writes=["yacc%d" % i for i in range(nt)])
                ld(lambda: nc.sync.dma_start(out=gts[:, 0:nt, :], in_=gates_scr[t0:t0 + nt].rearrange("t p e -> p t e")), writes=["gts"])
                groups = [(g0, min(512, ncols - g0)) for g0 in range(0, ncols, 512)]
                items = [(e, gi) for e in range(32) for gi in range(len(groups))]

                def stage_a(it):
                    e, gi = items[it]
                    g0, n = groups[gi]
                    slot = (hf * 32 + e) % 2
                    w1e, w1k = w1t[slot], "w1t%d" % slot
                    at, atk = actT[it % 2], "actT%d" % (it % 2)
                    akeys = []
                    for fc in range(8):
                        bi = fc % 2
                        gg, ggk = g32[bi], "g32_%d" % bi
                        sgg, sgk = sg32[bi], "sg32_%d" % bi
                        uu, uuk = u32[bi], "u32_%d" % bi
                        bgq, bgk = bank()
                        mm(bgq[:, 0:n], bgk, [(w1e[:, kc, fc * 128:(fc + 1) * 128], h1Th[:, kc, g0:g0 + n]) for kc in range(8)], ["h1Th", w1k + "_%d" % (fc // 4)])
                        buq, buk = bank()
                        mm(buq[:, 0:n], buk, [(w1e[:, kc, 1024 + fc * 128:1024 + (fc + 1) * 128], h1Th[:, kc, g0:g0 + n]) for kc in range(8)],
                           ["h1Th", w1k + "_%d" % (2 + fc // 4)])
                        V(lambda: nc.vector.tensor_scalar(out=gg[:, 0:n], in0=bgq[:, 0:n], scalar1=b1T[:, fc, e:e + 1], scalar2=7.0, op0=ALU.add, op1=ALU.min),
                          reads=[bgk, "b1T"], writes=[ggk])
                        A(lambda: nc.scalar.activation(out=sgg[:, 0:n], in_=gg[:, 0:n], func=AF.Sigmoid, scale=1.702), reads=[ggk], writes=[sgk])
                        V(lambda: nc.vector.tensor_scalar(out=uu[:, 0:n], in0=buq[:, 0:n], scalar1=b1T1[:, fc, e:e + 1], scalar2=8.0, op0=ALU.add, op1=ALU.min),
                          reads=[buk, "b1T1"], writes=[uuk])
                        G(lambda: nc.gpsimd.tensor_tensor(out=sgg[:, 0:n], in0=sgg[:, 0:n], in1=gg[:, 0:n], op=ALU.mult), reads=[sgk, ggk], writes=[sgk])
                        k = atk + "_%d" % fc
                        V(lambda: nc.vector.scalar_tensor_tensor(out=at[:, fc, 0:n], in0=uu[:, 0:n], scalar=-6.0, in1=sgg[:, 0:n], op0=ALU.max, op1=ALU.mult),
                          reads=[uuk, sgk], writes=[k])
                        akeys.append(k)
                    return akeys

                def stage_b(it, akeys):
                    e, gi = items[it]
                    g0, n = groups[gi]
                    slot = (hf * 32 + e) % 2
                    w2e, w2k = w2t[slot], "w2t%d" % slot
                    at = actT[it % 2]
                    for tt_ in range(n // 128):
                        ti = g0 // 128 + tt_
                        for half in range(2):
                            hs = slice(half * 512, (half + 1) * 512)
                            bq, bqk = bank()
                            mm(bq[:, :], bqk, [(at[:, fc, tt_ * 128:(tt_ + 1) * 128], w2e[:, fc, hs]) for fc in range(8)], akeys + [w2k + "_%d" % half])
                            V(lambda: nc.vector.scalar_tensor_tensor(out=yacc[:, ti, hs], in0=bq[:, :], scalar=gts[:, ti, e:e + 1], in1=yacc[:, ti, hs],
                                                                     op0=ALU.mult, op1=ALU.add), reads=[bqk, "gts", "yacc%d" % ti], writes=["yacc%d" % ti])

                load_expert(0, hf * 32 + 0)
                load_expert(1, hf * 32 + 1)
                pend = stage_a(0)
                for it in range(len(items)):
                    nxt = stage_a(it + 1) if it + 1 < len(items) else None
                    stage_b(it, pend)
                    pend = nxt
                    e, gi = items[it]
                    if gi == len(groups) - 1 and e + 2 < 32:
                        load_expert(e + 2, hf * 32 + e + 2)
                for ti, I in enumerate(tiles):
                    rows = 128 if I < NQ else ST
                    R = slice(0, rows)
                    yk = "yacc%d" % ti
                    for half in range(2):
                        V(lambda: nc.vector.bn_stats(out=bn[R, half, :], in_=yacc[R, ti, half * 512:(half + 1) * 512]), reads=[yk], writes=["bn%d" % half])
                    V(lambda: nc.vector.bn_aggr(out=mv[R, :], in_=bn[R, :, :]), reads=["bn0", "bn1"], writes=["mv"])
                    ln_rstd(mv[R, 1:2], rs1[R, :], sd[R, :], ["mv"], "rs1")
                    yoi, yok = yo[ti % 2], "yo%d" % (ti % 2)
                    V(lambda: nc.vector.tensor_scalar(out=yoi[R, :], in0=yacc[R, ti, :], scalar1=mv[R, 0:1], scalar2=rs1[R, 0:1], op0=ALU.subtract, op1=ALU.mult),
                      reads=[yk, "mv", "rs1"], writes=[yok])
                    G(lambda: nc.gpsimd.tensor_tensor(out=yoi[R, :], in0=yoi[R, :], in1=g2b[R, :], op=ALU.mult), reads=[yok, "g2b"], writes=[yok])
                    G(lambda: nc.gpsimd.tensor_tensor(out=yoi[R, :], in0=yoi[R, :], in1=b2b[R, :], op=ALU.add), reads=[yok, "b2b"], writes=[yok])
                    if I < NQ:
                        ld(lambda: nc.sync.dma_start(out=y_p[I * 128:(I + 1) * 128, :], in_=yoi[:, :]), reads=[yok], writes=[uniq("yout")])
                    else:
                        ld(lambda: nc.sync.dma_start(out=y_s[:, :], in_=yoi[R, :]), reads=[yok], writes=[uniq("yout")])
            S.finish("sp")


def _bucket_table():
    n = np.arange(0, 512)
    nf = np.maximum(n, 1).astype(np.float32)
    large = 16 + (np.log(nf / np.float32(16.0)) / np.float32(math.log(128 / 16)) * np.float32(16.0)).astype(np.int32)
    large = np.minimum(large, 31)
    return np.where(n < 16, n, large).astype(np.int64)


def _bias_tables(rel_bias):
    bt = _bucket_table()
    kk = np.arange(128)[:, None]
    cc = np.arange(128)[None, :]
    dist = np.zeros((5, 128, 128), np.int64)
    for r in range(3):
        d = np.where(cc < 64, 128 * (1 - r) + 64 + cc - kk, 128 * (2 - r) + cc - kk)
        dist[r] = d
    dist[3] = 128 + cc - kk
    dist[4] = cc - kk
    valid = dist >= 0
    valid[3][:, 4:] = True
    valid[4][4:, :] = True
    valid[4][:, 4:] = True
    dcl = np.clip(dist, 0, 511)
    braw = rel_bias[bt[dcl]]
    braw = np.where(valid[..., None], braw, 0.0).astype(np.float32)
    braw = np.ascontiguousarray(braw.transpose(1, 0, 3, 2)).reshape(128, 5 * 4 * 128)
    bmask = np.where(valid, 0.0, 8.0 * NEG).astype(np.float32)
    bmask = np.ascontiguousarray(np.broadcast_to(bmask[:, :, None, :], (5, 128, 4, 128)).transpose(1, 0, 2, 3)).reshape(128, 5 * 4 * 128)
    return braw, bmask


def kernel(x_prompt, x_sample, cache_k, cache_v, page_table, state_conv, w_in, b_in, lambda_q1, lambda_k1,
           lambda_q2, lambda_k2, subln_g, rel_bias, w_attn_proj, conv_w, conv_b, conv_ln_g, conv_ln_b,
           w_conv_proj, b_conv_proj, w_out, ln1_g, ln1_b, router_w, router_b, expert_w1, expert_b1,
           expert_w2, expert_b2, ln2_g, ln2_b):
    f = lambda a: np.ascontiguousarray(np.asarray(a, dtype=np.float32))
    x_prompt, x_sample, state_conv = f(x_prompt), f(x_sample), f(state_conv)
    rel_bias = f(rel_bias)
    braw, bmask = _bias_tables(rel_bias)
    shared = {
        "ck": f(cache_k).reshape(NPOOL * 128, 512), "cv": f(cache_v).reshape(NPOOL * 128, 512),
        "iot": np.arange(128, dtype=np.float32).reshape(128, 1), "braw": braw, "bmask": bmask,
        "w_in": f(w_in)[0], "b_in": f(b_in)[0],
        "lq1": f(lambda_q1)[0], "lk1": f(lambda_k1)[0], "lq2": f(lambda_q2)[0], "lk2": f(lambda_k2)[0],
        "subg": f(subln_g)[0], "rb31": np.ascontiguousarray(rel_bias[31]),
        "wap": f(w_attn_proj)[0], "convw": f(conv_w)[0], "convb": f(conv_b)[0], "clng": f(conv_ln_g)[0], "clnb": f(conv_ln_b)[0],
        "wcp": f(w_conv_proj)[0], "bcp": f(b_conv_proj)[0], "wout": f(w_out)[0], "ln1g": f(ln1_g)[0], "ln1b": f(ln1_b)[0],
        "rw": f(router_w)[0], "rbias": f(router_b)[0], "w1": f(expert_w1)[0], "b1": f(expert_b1)[0],
        "w2": f(expert_w2)[0], "b2": f(expert_b2)[0], "ln2g": f(ln2_g)[0], "ln2b": f(ln2_b)[0],
    }
    pt = np.ascontiguousarray(np.asarray(page_table, dtype=np.int32))
    nc = build_program()
    in_maps = []
    for c in range(NCORES):
        b, j = c // 2, c % 2
        if j == 1:
            xl = x_prompt[b]
        else:
            xl = np.concatenate([np.zeros((64, D), np.float32), x_prompt[b, :L - 64]], axis=0)
        one = np.ones((128, 1), np.float32)
        vm = one.copy()
        if j == 0:
            vm[:64] = 0.0
        m = dict(shared)
        m.update({
            "xloc": np.ascontiguousarray(xl),
            "xs": np.ascontiguousarray(x_sample[c * SS:(c + 1) * SS].reshape(ST, D)),
            "ptab": np.ascontiguousarray(pt[c * SS:(c + 1) * SS].reshape(-1)),
            "sconv": np.ascontiguousarray(state_conv[0, c * SS:(c + 1) * SS]),
            "vmk": vm, "hm": (one * float(j)).astype(np.float32),
        })
        in_maps.append(m)
    res = run_bass_kernel_spmd(nc, in_maps, core_ids=list(range(NCORES))).results
    B = x_prompt.shape[0]
    y_p = np.zeros((B, L, D), np.float32)
    nk_p = np.zeros((1, B, L, 4, 128), np.float32)
    nv_p = np.zeros((1, B, L, 4, 128), np.float32)
    nc_p = np.zeros((1, B, 30, 512), np.float32)
    y_s = np.zeros((128, 4, D), np.float32)
    nk_s = np.zeros((1, 128, 4, 4, 128), np.float32)
    nv_s = np.zeros((1, 128, 4, 4, 128), np.float32)
    nc_s = np.zeros((1, 128, 30, 512), np.float32)
    for c in range(NCORES):
        b, j = c // 2, c % 2
        r = res[c]
        rows = (np.arange(NBLK)[:, None] * 128 + 64 * j + np.arange(64)[None, :]).reshape(-1)
        y_p[b, rows] = r["y_p"]
        nk_p[0, b, rows] = r["nk_p"].reshape(TOK, 4, 128)
        nv_p[0, b, rows] = r["nv_p"].reshape(TOK, 4, 128)
        if j == 1:
            nc_p[0, b] = r["nc_p"]
        ss = slice(c * SS, (c + 1) * SS)
        y_s[ss] = r["y_s"].reshape(SS, 4, D)
        nk_s[0, ss] = r["nk_s"].reshape(SS, 4, 4, 128)
        nv_s[0, ss] = r["nv_s"].reshape(SS, 4, 4, 128)
        nc_s[0, ss] = r["nc_s"]
    return (y_p, y_s, nk_p, nv_p, nc_p, nk_s, nv_s, nc_s)
```

```python
import math
import os
import numpy as np
from contextlib import ExitStack
import concourse.bass as bass
import concourse.mybir as mybir
from concourse.bass_utils import run_bass_kernel_spmd

F32 = mybir.dt.float32
BF16 = mybir.dt.bfloat16
I32 = mybir.dt.int32
AF = mybir.ActivationFunctionType
ALU = mybir.AluOpType
AX = mybir.AxisListType

NCORES = 8
D = 1024
L = 4096
NBLK = 32
NQ = 16
TOK = 2048
SS = 16
ST = 64
NTILE = 17
QOFF, KOFF, VOFF, AOFF, GOFF, GAOFF = 0, 512, 1024, 1536, 2048, 2560
ALPHA = float(2.0 ** 0.25)
LAM0 = 0.8 - 0.6 * math.exp(0.0)
EPS = 1e-5
NEG = -30000.0
NPOOL = int(os.environ.get('KPOOL', '2560'))
KSTOP = int(os.environ.get('KSTOP', '99'))
HALVES = (list(range(0, 9)), list(range(9, 17)))


class Sched:
    def __init__(self, nc, es, n_dma_sems=32):
        self.nc = nc
        self.eng = {"pe": nc.tensor, "act": nc.scalar, "dve": nc.vector, "pool": nc.gpsimd, "sp": nc.sync}
        self.sem = {k: es.enter_context(nc.semaphore("s_" + k)) for k in self.eng}
        self.cnt = {k: 0 for k in self.eng}
        self.dsem = [es.enter_context(nc.semaphore("d%d" % i)) for i in range(n_dma_sems)]
        self.dcnt = [0] * n_dma_sems
        self.dnext = 0
        self.known = {k: {} for k in self.eng}
        self.lastw = {}
        self.readers = {}

    def _wait(self, e, tok):
        kind, key, val = tok
        if kind == "e" and key == e and (e == "pe" or val > self.cnt[e]):
            return
        if self.known[e].get((kind, key), 0) >= val:
            return
        self.known[e][(kind, key)] = val
        s = self.sem[key] if kind == "e" else self.dsem[key]
        self.eng[e].wait_ge(s, val)

    def _deps(self, e, reads, writes):
        toks = []
        for r in reads:
            if r in self.lastw:
                toks.append(self.lastw[r])
        for w in writes:
            if w in self.lastw:
                toks.append(self.lastw[w])
            toks.extend(self.readers.get(w, []))
        for t in toks:
            self._wait(e, t)

    def _record(self, tok, reads, writes):
        for r in reads:
            self.readers.setdefault(r, []).append(tok)
        for w in writes:
            self.lastw[w] = tok
            self.readers[w] = []

    def op(self, e, fn, reads=(), writes=(), inc=True):
        self._deps(e, reads, writes)
        ins = fn()
        tok = ("e", e, self.cnt[e] + 1)
        if inc:
            self.cnt[e] += 1
            ins.then_inc(self.sem[e], 1)
        self._record(tok, reads, writes)
        return ins

    MAX_OUTSTANDING = {"pool": 4, "sp": 8}

    def dma(self, q, fn, reads=(), writes=()):
        i = self.dnext
        self.dnext = (self.dnext + 1) % len(self.dsem)
        if self.dcnt[i] > 0:
            self._wait(q, ("d", i, self.dcnt[i]))
        hist = self.__dict__.setdefault("dhist", {}).setdefault(q, [])
        k = self.MAX_OUTSTANDING.get(q, 8)
        if len(hist) >= k:
            self._wait(q, hist[-k])
        self._deps(q, reads, writes)
        ins = fn()
        self.dcnt[i] += 16
        ins.then_inc(self.dsem[i], 16)
        tok = ("d", i, self.dcnt[i])
        hist.append(tok)
        self._record(tok, reads, writes)
        return ins

    def barrier(self):
        for e in self.eng:
            for f in self.eng:
                if f != e and self.cnt[f] > 0:
                    self._wait(e, ("e", f, self.cnt[f]))
            for i, c in enumerate(self.dcnt):
                if c > 0:
                    self._wait(e, ("d", i, c))
        self.lastw.clear()
        self.readers.clear()

    def finish(self, e="sp"):
        for f in self.eng:
            if f != e and self.cnt[f] > 0:
                self._wait(e, ("e", f, self.cnt[f]))
        for i, c in enumerate(self.dcnt):
            if c > 0:
                self._wait(e, ("d", i, c))


class _Stop(Exception):
    pass


def build_program():
    nc = bass.Bass("TRN2", target_bir_lowering=False)
    try:
        _build_body(nc)
    except _Stop:
        pass
    return nc


def _build_body(nc):
    din = lambda n, s, dt=F32: nc.dram_tensor(n, s, dt, kind="ExternalInput").ap()
    dout = lambda n, s: nc.dram_tensor(n, s, F32, kind="ExternalOutput").ap()
    dscr = lambda n, s, dt=F32: nc.dram_tensor(n, s, dt, kind="Internal").ap()
    xloc = din("xloc", [L, D]); xs = din("xs", [ST, D])
    ck = din("ck", [NPOOL * 128, 512]); cv = din("cv", [NPOOL * 128, 512])
    ptab = din("ptab", [SS * 16], I32); sconv = din("sconv", [SS, 30, 512])
    iot = din("iot", [128, 1]); vmk_d = din("vmk", [128, 1]); hm_d = din("hm", [128, 1])
    braw_d = din("braw", [128, 5 * 4 * 128]); bmask_d = din("bmask", [128, 5 * 4 * 128])
    w_in = din("w_in", [D, 4608]); b_in = din("b_in", [4608])
    lq1 = din("lq1", [64]); lk1 = din("lk1", [64]); lq2 = din("lq2", [64]); lk2 = din("lk2", [64])
    subg = din("subg", [128]); rb31_d = din("rb31", [4])
    wap_d = din("wap", [512, D]); convw = din("convw", [31, 512]); convb = din("convb", [512])
    clng = din("clng", [512]); clnb = din("clnb", [512]); wcp_d = din("wcp", [512, D]); bcp_d = din("bcp", [D])
    wout_d = din("wout", [D, D]); ln1g = din("ln1g", [D]); ln1b = din("ln1b", [D])
    rw_d = din("rw", [D, 32]); rbias = din("rbias", [32])
    w1_d = din("w1", [32, D, 2048]); b1_d = din("b1", [32, 2048]); w2_d = din("w2", [32, D, D]); b2_d = din("b2", [32, D])
    ln2g = din("ln2g", [D]); ln2b = din("ln2b", [D])
    y_p = dout("y_p", [TOK, D]); y_s = dout("y_s", [ST, D])
    nk_p = dout("nk_p", [TOK, 512]); nv_p = dout("nv_p", [TOK, 512]); nc_p = dout("nc_p", [30, 512])
    nk_s = dout("nk_s", [ST, 512]); nv_s = dout("nv_s", [ST, 512]); nc_s = dout("nc_s", [SS, 30, 512])
    yacc_scr = dscr("yacc_scr", [NTILE, 128, D]); h1T_scr = dscr("h1T_scr", [128, 8, NTILE * 128], BF16)
    gates_scr = dscr("gates_scr", [NTILE, 128, 32])
    w_in_v = w_in.rearrange("(kc p) n -> p kc n", p=128)

    with ExitStack() as es0:
        _nm = {"i": 0}

        def _sbuf(stack, n, s, dt):
            _nm["i"] += 1
            return stack.enter_context(nc.sbuf_tensor("sb%d_%s" % (_nm["i"], n), s, dt))
        sb0 = lambda n, s, dt=F32: _sbuf(es0, n, s, dt)
        pb = [es0.enter_context(nc.psum_tensor("pb%d" % i, [128, 512], F32)) for i in range(8)]
        S = Sched(nc, es0)
        st = {"bank": 0, "acc": 0, "u": 0}

        def bank():
            i = st["bank"]
            st["bank"] = (i + 1) % 6
            return pb[i], "pb%d" % i

        def accbank():
            i = 6 + st["acc"]
            st["acc"] = (st["acc"] + 1) % 2
            return pb[i], "pb%d" % i

        def uniq(p):
            st["u"] += 1
            return "%s_%d" % (p, st["u"])

        V = lambda fn, **kw: S.op("dve", fn, **kw)
        A = lambda fn, **kw: S.op("act", fn, **kw)
        G = lambda fn, **kw: S.op("pool", fn, **kw)
        P = lambda fn, **kw: S.op("pe", fn, **kw)

        def stop_here(k):
            if KSTOP == k:
                S.finish("sp")
                raise _Stop()
        ld = lambda fn, **kw: S.dma("sp", fn, **kw)
        ldc = lambda fn, **kw: S.dma("pool", fn, **kw)

        def mm(out, okey, pairs, reads):
            n = len(pairs)
            for i, (l, r) in enumerate(pairs):
                P(lambda: nc.tensor.matmul(out, lhsT=l, rhs=r, start=(i == 0), stop=(i == n - 1)),
                  reads=reads, writes=[okey], inc=(i == n - 1))

        ident = sb0("ident", [128, 128]); identb = sb0("identb", [128, 128], BF16)
        onesM = sb0("onesM", [128, 128]); epsc = sb0("epsc", [128, 1])
        binT = sb0("binT", [128, 36]); bk_bc = sb0("bk_bc", [128, 512]); bv_bc = sb0("bv_bc", [128, 512])
        cwT = sb0("cwT", [128, 4, 32]); pT12 = sb0("pT12", [128, 12])
        lamc = sb0("lamc", [128, 4]); subg_bc = sb0("subg_bc", [128, 128]); rb31 = sb0("rb31", [128, 4])
        vmk = sb0("vmk", [128, 1]); hm = sb0("hm", [128, 1]); iotc = sb0("iotc", [128, 1])
        idx = sb0("idx", [128, SS * 16], I32)
        G(lambda: nc.gpsimd.memset(ident[:], 0.0), writes=["ident"])
        G(lambda: nc.gpsimd.affine_select(out=ident[:], in_=ident[:], pattern=[[-1, 128]], compare_op=ALU.not_equal,
                                          fill=1.0, base=0, channel_multiplier=1), reads=["ident"], writes=["ident"])
        V(lambda: nc.vector.tensor_copy(out=identb[:], in_=ident[:]), reads=["ident"], writes=["identb"])
        V(lambda: nc.vector.memset(onesM[:], 1.0 / 512.0), writes=["onesM"])
        V(lambda: nc.vector.memset(epsc[:], EPS), writes=["epsc"])
        ld(lambda: nc.sync.dma_start(out=bk_bc[:], in_=b_in[KOFF:KOFF + 512].partition_broadcast(128)), writes=["bk_bc"])
        ld(lambda: nc.sync.dma_start(out=bv_bc[:], in_=b_in[VOFF:VOFF + 512].partition_broadcast(128)), writes=["bv_bc"])
        ld(lambda: nc.sync.dma_start(out=subg_bc[:], in_=subg.partition_broadcast(128)), writes=["subg_bc"])
        ld(lambda: nc.sync.dma_start(out=rb31[:], in_=rb31_d.partition_broadcast(128)), writes=["rb31"])
        ld(lambda: nc.sync.dma_start(out=vmk[:], in_=vmk_d), writes=["vmk"])
        ld(lambda: nc.sync.dma_start(out=hm[:], in_=hm_d), writes=["hm"])
        ld(lambda: nc.sync.dma_start(out=iotc[:], in_=iot), writes=["iotc"])
        V(lambda: nc.vector.tensor_scalar(out=subg_bc[:], in0=subg_bc[:], scalar1=1.0 - LAM0, scalar2=None, op0=ALU.mult),
          reads=["subg_bc"], writes=["subg_bc"])
        with ExitStack() as est:
            sbt = lambda n, s, dt=F32: _sbuf(est, n, s, dt)
            brow = sbt("brow", [36, 128]); crow = sbt("crow", [31, 512]); prow = sbt("prow", [12, 128])
            lqt = sbt("lqt", [128, 4, 64]); ptb = sbt("ptb", [128, SS * 16], I32); ptf = sbt("ptf", [128, SS * 16])
            ld(lambda: nc.sync.dma_start(out=brow[:], in_=b_in.rearrange("(c p) -> c p", p=128)), writes=["brow"])
            ld(lambda: nc.sync.dma_start(out=crow[:], in_=convw), writes=["crow"])
            for i, src in enumerate((convb, clng, clnb)):
                ld(lambda: nc.sync.dma_start(out=prow[4 * i:4 * i + 4, :], in_=src.rearrange("(c p) -> c p", p=128)), writes=["prow%d" % i])
            for i, src in enumerate((lq1, lk1, lq2, lk2)):
                ld(lambda: nc.sync.dma_start(out=lqt[:, i, :], in_=src.partition_broadcast(128)), writes=["lqt%d" % i])
            ld(lambda: nc.sync.dma_start(out=ptb[:], in_=ptab.partition_broadcast(128)), writes=["ptb"])
            b, bkey = bank()
            P(lambda: nc.tensor.transpose(out=b[:, 0:36], in_=brow[0:36, :], identity=ident[0:36, 0:36]), reads=["brow", "ident"], writes=[bkey])
            V(lambda: nc.vector.tensor_copy(out=binT[:], in_=b[:, 0:36]), reads=[bkey], writes=["binT"])
            b, bkey = bank()
            for cc in range(4):
                P(lambda: nc.tensor.transpose(out=b[:, cc * 32:cc * 32 + 31], in_=crow[0:31, cc * 128:(cc + 1) * 128],
                                              identity=ident[0:31, 0:31]), reads=["crow", "ident"], writes=[bkey], inc=(cc == 3))
            V(lambda: nc.vector.memset(cwT[:], 0.0), writes=["cwT"])
            V(lambda: nc.vector.tensor_copy(out=cwT[:, :, 0:31], in_=b[:, 0:128].rearrange("p (c w) -> p c w", w=32)[:, :, 0:31]),
              reads=[bkey], writes=["cwT"])
            b, bkey = bank()
            P(lambda: nc.tensor.transpose(out=b[:, 0:12], in_=prow[0:12, :], identity=ident[0:12, 0:12]),
              reads=["prow0", "prow1", "prow2", "ident"], writes=[bkey])
            V(lambda: nc.vector.tensor_copy(out=pT12[:], in_=b[:, 0:12]), reads=[bkey], writes=["pT12"])
            for i in range(2):
                V(lambda: nc.vector.tensor_tensor(out=lqt[:, 2 * i, :], in0=lqt[:, 2 * i, :], in1=lqt[:, 2 * i + 1, :], op=ALU.mult),
                  reads=["lqt%d" % (2 * i), "lqt%d" % (2 * i + 1)], writes=["lqt%d" % (2 * i)])
                V(lambda: nc.vector.reduce_sum(out=lamc[:, i:i + 1], in_=lqt[:, 2 * i, :], axis=AX.X), reads=["lqt%d" % (2 * i)], writes=["lam%d" % i])
                A(lambda: nc.scalar.activation(out=lamc[:, i:i + 1], in_=lamc[:, i:i + 1], func=AF.Exp), reads=["lam%d" % i], writes=["lam%d" % i])
            V(lambda: nc.vector.tensor_tensor(out=lamc[:, 2:3], in0=lamc[:, 0:1], in1=lamc[:, 1:2], op=ALU.subtract), reads=["lam0", "lam1"], writes=["lam2"])
            V(lambda: nc.vector.tensor_scalar(out=lamc[:, 3:4], in0=lamc[:, 2:3], scalar1=LAM0, scalar2=-1.0, op0=ALU.add, op1=ALU.mult),
              reads=["lam2"], writes=["nlam"])
            V(lambda: nc.vector.tensor_copy(out=ptf[:], in_=ptb[:]), reads=["ptb"], writes=["ptf"])
            V(lambda: nc.vector.tensor_scalar(out=ptf[:], in0=ptf[:], scalar1=128.0, scalar2=iotc[:, 0:1], op0=ALU.mult, op1=ALU.add),
              reads=["ptf", "iotc"], writes=["ptf"])
            V(lambda: nc.vector.tensor_copy(out=idx[:], in_=ptf[:]), reads=["ptf"], writes=["idx"])
            S.barrier()
        if KSTOP == 0:
            S.finish("sp")
            raise _Stop()
        cbT = pT12[:, 0:4]; lgT = pT12[:, 4:8]; lbT = pT12[:, 8:12]
        nlam = lamc[:, 3:4]

        ld(lambda: nc.sync.dma_start(out=nc_s[:, 0:26, :], in_=sconv[:, 4:30, :]), writes=["nc_s_a"])

        def ln_rstd(var_ap, out_ap, tmp_ap, keys_r, key_w):
            A(lambda: nc.scalar.activation(out=tmp_ap, in_=var_ap, func=AF.Sqrt, bias=epsc[0:var_ap.shape[0], 0:1], scale=1.0),
              reads=keys_r + ["epsc"], writes=[key_w + "_sd"])
            V(lambda: nc.vector.reciprocal(out=out_ap, in_=tmp_ap), reads=[key_w + "_sd"], writes=[key_w])

        def transpose_rows(src_tile, skey, rows, dst, dkeyp, dt_engine_pair=("act", "dve")):
            keys = []
            skeys = list(skey) if isinstance(skey, (list, tuple)) else [skey]
            for half in range(2):
                b, bkey = bank()
                for q in range(4):
                    kc = half * 4 + q
                    P(lambda: nc.tensor.transpose(out=b[:, q * 128:q * 128 + rows], in_=src_tile[0:rows, kc * 128:(kc + 1) * 128],
                                                  identity=ident[0:rows, 0:rows]), reads=skeys + ["ident"], writes=[bkey], inc=(q == 3))
                sv = b[:, :].rearrange("p (q r) -> p q r", r=128)[:, :, 0:rows]
                src_ap, src_key = sv, bkey
                for di, (d_, dk) in enumerate(zip(dst, dkeyp)):
                    dv = d_[:, half * 4:half * 4 + 4, 0:rows]
                    k = dk + "h%d" % half
                    if di > 0:
                        G(lambda: nc.gpsimd.tensor_copy(out=dv, in_=src_ap), reads=[src_key], writes=[k])
                    elif half == 0:
                        A(lambda: nc.scalar.copy(out=dv, in_=src_ap), reads=[src_key], writes=[k])
                    else:
                        V(lambda: nc.vector.tensor_copy(out=dv, in_=src_ap), reads=[src_key], writes=[k])
                    keys.append(k)
                    if di == 0:
                        src_ap, src_key = dv, k
            return keys

        with ExitStack() as esP:
            sbP = lambda n, s, dt=F32: _sbuf(esP, n, s, dt)
            sT = sbP("sT", [128, 4, TOK + ST], BF16)
            oT = sbP("oT", [128, 4, TOK + ST], BF16)
            bthi = sbP("bthi", [128, 5, 4, 128], BF16); btlo = sbP("btlo", [128, 5, 4, 128], BF16)
            with ExitStack() as est:
                sbt = lambda n, s, dt=F32: _sbuf(est, n, s, dt)
                braw = sbt("braw", [128, 5, 4, 128]); bmsk = sbt("bmsk", [128, 5, 4, 128]); bt32 = sbt("bt32", [128, 5, 4, 128])
                ld(lambda: nc.sync.dma_start(out=braw[:].rearrange("p a b c -> p (a b c)"), in_=braw_d), writes=["braw"])
                ld(lambda: nc.sync.dma_start(out=bmsk[:].rearrange("p a b c -> p (a b c)"), in_=bmask_d), writes=["bmsk"])
                for h in range(4):
                    V(lambda: nc.vector.tensor_scalar(out=bt32[:, :, h, :], in0=braw[:, :, h, :], scalar1=rb31[:, h:h + 1], scalar2=8.0,
                                                      op0=ALU.subtract, op1=ALU.mult), reads=["braw", "rb31"], writes=["bt32"])
                V(lambda: nc.vector.tensor_tensor(out=bt32[:], in0=bt32[:], in1=bmsk[:], op=ALU.add), reads=["bt32", "bmsk"], writes=["bt32"])
                V(lambda: nc.vector.tensor_copy(out=bthi[:], in_=bt32[:]), reads=["bt32"], writes=["bthi"])
                V(lambda: nc.vector.tensor_copy(out=braw[:], in_=bthi[:]), reads=["bthi"], writes=["braw"])
                V(lambda: nc.vector.tensor_tensor(out=bt32[:], in0=bt32[:], in1=braw[:], op=ALU.subtract), reads=["bt32", "braw"], writes=["bt32"])
                V(lambda: nc.vector.tensor_copy(out=btlo[:], in_=bt32[:]), reads=["bt32"], writes=["btlo"])
                S.barrier()

            def subln_store(o_ap, okey, rows, dst_ap, dkey, scr):
                sq, ss, sd, rs = scr
                V(lambda: nc.vector.tensor_tensor(out=sq[0:rows, :], in0=o_ap, in1=o_ap, op=ALU.mult), reads=[okey], writes=["sl_sq"])
                V(lambda: nc.vector.reduce_sum(out=ss[0:rows, :], in_=sq[0:rows, :], axis=AX.X), reads=["sl_sq"], writes=["sl_ss"])
                A(lambda: nc.scalar.activation(out=sd[0:rows, :], in_=ss[0:rows, :], func=AF.Sqrt, bias=epsc[0:rows, 0:1], scale=1.0 / 128.0),
                  reads=["sl_ss", "epsc"], writes=["sl_sd"])
                V(lambda: nc.vector.reciprocal(out=rs[0:rows, :], in_=sd[0:rows, :]), reads=["sl_sd"], writes=["sl_rs"])
                V(lambda: nc.vector.scalar_tensor_tensor(out=dst_ap, in0=o_ap, scalar=rs[0:rows, 0:1], in1=subg_bc[0:rows, :],
                                                         op0=ALU.mult, op1=ALU.mult), reads=[okey, "sl_rs", "subg_bc"], writes=[dkey])

            def combine(acc, akey, rows, osb, okey, scr2):
                r0, r1 = scr2
                V(lambda: nc.vector.reciprocal(out=r0[0:rows, :], in_=acc[0:rows, 0, 128:129]), reads=[akey], writes=["cb_r0"])
                V(lambda: nc.vector.reciprocal(out=r1[0:rows, :], in_=acc[0:rows, 1, 128:129]), reads=[akey], writes=["cb_r1"])
                V(lambda: nc.vector.tensor_tensor(out=r1[0:rows, :], in0=r1[0:rows, :], in1=nlam[0:rows, :], op=ALU.mult), reads=["cb_r1", "nlam"], writes=["cb_r1"])
                V(lambda: nc.vector.tensor_scalar(out=osb, in0=acc[0:rows, 0, 0:128], scalar1=r0[0:rows, 0:1], scalar2=None, op0=ALU.mult),
                  reads=[akey, "cb_r0"], writes=[okey])
                V(lambda: nc.vector.scalar_tensor_tensor(out=osb, in0=acc[0:rows, 1, 0:128], scalar=r1[0:rows, 0:1], in1=osb, op0=ALU.mult, op1=ALU.add),
                  reads=[akey, "cb_r1", okey], writes=[okey])

            with ExitStack() as esKV:
                sbK = lambda n, s, dt=F32: _sbuf(esKV, n, s, dt)
                KT = sbK("KT", [128, 4, L], BF16)
                VA = sbK("VA", [128, NBLK, 4, 130], BF16)
                V(lambda: nc.vector.memset(VA[:, :, :, 128:129], 1.0), writes=["VAones"])
                V(lambda: nc.vector.memset(VA[:, :, :, 129:130], 0.0), writes=["VAz"])
                with ExitStack() as es1:
                    sb1 = lambda n, s, dt=F32: _sbuf(es1, n, s, dt)
                    wk = sb1("wk", [128, 8, 512], BF16); wv = sb1("wv", [128, 8, 512], BF16); wc = sb1("wc", [128, 8, 1024], BF16)
                    xb = [sb1("xb%d" % i, [128, D]) for i in range(2)]
                    xT1 = [sb1("xT1_%d" % i, [128, 8, 512], BF16) for i in range(1)]
                    ub = sb1("ub", [128, 4, 608])
                    sig = sb1("sig", [128, 512])
                    tk = [sb1("tk%d" % i, [128, 512]) for i in range(4)]
                    ycv = sb1("ycv", [128, 4, 256]); ysq = sb1("ysq", [128, 4, 256])
                    mnb = sb1("mnb", [128, 256]); m2 = sb1("m2", [128, 256]); var = sb1("var", [128, 256]); rstd = sb1("rstd", [128, 256])
                    tt = sb1("tt", [128, 256]); nct = sb1("nct", [30, 512]); sdt = sb1("sdt", [128, 256])
                    ldc(lambda: nc.gpsimd.dma_start(out=wk[:], in_=w_in_v[:, :, KOFF:KOFF + 512]), writes=["wk"])
                    ldc(lambda: nc.gpsimd.dma_start(out=wv[:], in_=w_in_v[:, :, VOFF:VOFF + 512]), writes=["wv"])
                    ldc(lambda: nc.gpsimd.dma_start(out=wc[:], in_=w_in_v[:, :, AOFF:AOFF + 1024]), writes=["wc"])
                    V(lambda: nc.vector.memset(ub[:], 0.0), writes=["ub"])
                    tki = 0
                    for c in range(8):
                        xTc, xk = xT1[0], "xT1_0"
                        xkeys = []
                        for b_ in range(4):
                            blk = 4 * c + b_
                            xbi, xbk = xb[blk % 2], "xb%d" % (blk % 2)
                            ld(lambda: nc.sync.dma_start(out=xbi[:], in_=xloc[blk * 128:(blk + 1) * 128, :]), writes=[xbk])
                            for half in range(2):
                                bq, bqk = bank()
                                for q in range(4):
                                    kc = half * 4 + q
                                    P(lambda: nc.tensor.transpose(out=bq[:, q * 128:(q + 1) * 128], in_=xbi[:, kc * 128:(kc + 1) * 128], identity=ident[:]),
                                      reads=[xbk, "ident"], writes=[bqk], inc=(q == 3))
                                dv = xTc[:, half * 4:half * 4 + 4, b_ * 128:(b_ + 1) * 128]
                                sv = bq[:, :].rearrange("p (q r) -> p q r", r=128)
                                k = xk + "_%d_%d" % (b_, half)
                                if half == 0:
                                    A(lambda: nc.scalar.copy(out=dv, in_=sv), reads=[bqk], writes=[k])
                                else:
                                    V(lambda: nc.vector.tensor_copy(out=dv, in_=sv), reads=[bqk], writes=[k])
                                xkeys.append(k)
                        for h in range(4):
                            bq, bqk = bank()
                            mm(bq[:, :], bqk, [(wk[:, kc, h * 128:(h + 1) * 128], xTc[:, kc, :]) for kc in range(8)], xkeys + ["wk"])
                            A(lambda: nc.scalar.activation(out=KT[:, h, c * 512:(c + 1) * 512], in_=bq[:, :], func=AF.Identity,
                                                           bias=binT[:, 4 + h:5 + h], scale=1.0), reads=[bqk, "binT"], writes=[uniq("KT")])
                        for b_ in range(4):
                            blk = 4 * c + b_
                            xr = [xk + "_%d_0" % b_, xk + "_%d_1" % b_]
                            for which in range(2):
                                w_, wkey, bias_, dst = ((wk, "wk", bk_bc, nk_p), (wv, "wv", bv_bc, nv_p))[which]
                                bq, bqk = bank()
                                mm(bq[:, :], bqk, [(xTc[:, kc, b_ * 128:(b_ + 1) * 128], w_[:, kc, :]) for kc in range(8)], xr + [wkey])
                                t_, tkey = tk[tki % 4], "tk%d" % (tki % 4)
                                tki += 1
                                V(lambda: nc.vector.tensor_tensor(out=t_[:], in0=bq[:, :], in1=bias_[:], op=ALU.add), reads=[bqk, "bk_bc", "bv_bc"], writes=[tkey])
                                ld(lambda: nc.sync.dma_start(out=dst[blk * 64:(blk + 1) * 64, :], in_=t_[64:128, :]), reads=[tkey], writes=[uniq("okv")])
                                if which == 1:
                                    vak = "VA%d" % blk
                                    A(lambda: nc.scalar.copy(out=VA[:, blk, :, 0:128], in_=t_[:].rearrange("p (h v) -> p h v", v=128)),
                                      reads=[tkey, "VAones", "VAz"], writes=[vak])
                                    if blk == 0:
                                        V(lambda: nc.vector.tensor_scalar(out=VA[:, 0, :, :], in0=VA[:, 0, :, :], scalar1=vmk[:, 0:1], scalar2=None, op0=ALU.mult),
                                          reads=[vak, "vmk", "VAones", "VAz"], writes=[vak])
                        if c > 0:
                            A(lambda: nc.scalar.copy(out=ub[:, :, 0:32], in_=ub[:, :, 512:544]), reads=["ub"], writes=["ub"])
                        for cc in range(4):
                            ba_, bak = bank()
                            mm(ba_[:, :], bak, [(wc[:, kc, cc * 128:(cc + 1) * 128], xTc[:, kc, :]) for kc in range(8)], xkeys + ["wc"])
                            bg_, bgk = bank()
                            mm(bg_[:, :], bgk, [(wc[:, kc, 512 + cc * 128:512 + (cc + 1) * 128], xTc[:, kc, :]) for kc in range(8)], xkeys + ["wc"])
                            A(lambda: nc.scalar.activation(out=sig[:], in_=bg_[:, :], func=AF.Sigmoid, bias=binT[:, 16 + cc:17 + cc], scale=1.0),
                              reads=[bgk, "binT"], writes=["sig"])
                            V(lambda: nc.vector.scalar_tensor_tensor(out=ub[:, cc, 32:544], in0=ba_[:, :], scalar=binT[:, 12 + cc:13 + cc], in1=sig[:],
                                                                     op0=ALU.add, op1=ALU.mult), reads=[bak, "sig", "binT"], writes=["ub"])
                        if c == 0:
                            V(lambda: nc.vector.tensor_scalar(out=ub[:, :, 32:96], in0=ub[:, :, 32:96], scalar1=hm[:, 0:1], scalar2=None, op0=ALU.mult),
                              reads=["ub", "hm"], writes=["ub"])
                        for cc in range(4):
                            v66 = ub[:, cc, 66:578].rearrange("p (b x) -> p b x", x=128)
                            yv = ycv[:, cc, :].rearrange("p (b t) -> p b t", t=64)
                            V(lambda: nc.vector.tensor_scalar(out=yv, in0=v66[:, :, 0:64], scalar1=cwT[:, cc, 0:1], scalar2=cbT[:, cc:cc + 1],
                                                              op0=ALU.mult, op1=ALU.add), reads=["ub", "cwT", "pT12"], writes=["ycv%d" % cc])
                            for w in range(1, 31):
                                V(lambda: nc.vector.scalar_tensor_tensor(out=yv, in0=v66[:, :, w:w + 64], scalar=cwT[:, cc, w:w + 1], in1=yv,
                                                                         op0=ALU.mult, op1=ALU.add), reads=["ub", "ycv%d" % cc], writes=["ycv%d" % cc])
                        ykeys = ["ycv%d" % cc for cc in range(4)]
                        G(lambda: nc.gpsimd.tensor_tensor(out=ysq[:], in0=ycv[:], in1=ycv[:], op=ALU.mult), reads=ykeys, writes=["ysq"])
                        bm, bmk = bank()
                        mm(bm[:, 0:256], bmk, [(onesM[:], ycv[:, cc, :]) for cc in range(4)], ykeys + ["onesM"])
                        bs, bsk = bank()
                        mm(bs[:, 0:256], bsk, [(onesM[:], ysq[:, cc, :]) for cc in range(4)], ["ysq", "onesM"])
                        A(lambda: nc.scalar.copy(out=mnb[:], in_=bm[:, 0:256]), reads=[bmk], writes=["mnb"])
                        G(lambda: nc.gpsimd.tensor_tensor(out=m2[:], in0=mnb[:], in1=mnb[:], op=ALU.mult), reads=["mnb"], writes=["m2"])
                        V(lambda: nc.vector.tensor_tensor(out=var[:], in0=bs[:, 0:256], in1=m2[:], op=ALU.subtract), reads=[bsk, "m2"], writes=["var"])
                        ln_rstd(var[:], rstd[:], sdt[:], ["var"], "rstd")
                        for cc in range(4):
                            V(lambda: nc.vector.tensor_tensor(out=tt[:], in0=ycv[:, cc, :], in1=mnb[:], op=ALU.subtract), reads=["ycv%d" % cc, "mnb"], writes=["tt"])
                            G(lambda: nc.gpsimd.tensor_tensor(out=tt[:], in0=tt[:], in1=rstd[:], op=ALU.mult), reads=["tt", "rstd"], writes=["tt"])
                            A(lambda: nc.scalar.activation(out=sT[:, cc, c * 256:(c + 1) * 256], in_=tt[:], func=AF.Silu, bias=lbT[:, cc:cc + 1], scale=lgT[:, cc:cc + 1]),
                              reads=["tt", "pT12"], writes=[uniq("sT")])
                        if c == 7:
                            bq, bqk = bank()
                            for cc in range(4):
                                P(lambda: nc.tensor.transpose(out=bq[0:30, cc * 128:(cc + 1) * 128], in_=ub[:, cc, 514:544], identity=ident[:]),
                                  reads=["ub", "ident"], writes=[bqk], inc=(cc == 3))
                            V(lambda: nc.vector.tensor_copy(out=nct[:], in_=bq[0:30, :]), reads=[bqk], writes=["nct"])
                            ld(lambda: nc.sync.dma_start(out=nc_p[:, :], in_=nct[:]), reads=["nct"], writes=["nc_p"])
                    S.barrier()
                if KSTOP == 1:
                    S.finish("sp")
                    raise _Stop()
                with ExitStack() as es2:
                    sb2 = lambda n, s, dt=F32: _sbuf(es2, n, s, dt)
                    wq = sb2("wq", [128, 8, 512], BF16)
                    xo = [sb2("xo%d" % i, [128, D]) for i in range(2)]
                    xTo = [sb2("xTo%d" % i, [128, 8, 128], BF16) for i in range(2)]
                    qT = [sb2("qT%d" % i, [128, 4, 128], BF16) for i in range(2)]
                    PT = [sb2("PT%d" % i, [128, 4, 128], BF16) for i in range(3)]
                    otok = [sb2("otok%d" % i, [128, 4, 128]) for i in range(2)]
                    osb = sb2("osb", [128, 128])
                    scr = (sb2("sl_sq", [128, 128]), sb2("sl_ss", [128, 1]), sb2("sl_sd", [128, 1]), sb2("sl_rs", [128, 1]))
                    scr2 = (sb2("cb_r0", [128, 1]), sb2("cb_r1", [128, 1]))
                    ldc(lambda: nc.gpsimd.dma_start(out=wq[:], in_=w_in_v[:, :, QOFF:QOFF + 512]), writes=["wq"])
                    pti = 0
                    for I in range(NQ):
                        xoi, xok = xo[I % 2], "xo%d" % (I % 2)
                        for hh in range(2):
                            r0 = (2 * I + hh) * 128 + 64
                            ld(lambda: nc.sync.dma_start(out=xoi[hh * 64:(hh + 1) * 64, :], in_=xloc[r0:r0 + 64, :]), writes=[xok])
                        xTi, xTk = xTo[I % 2], "xTo%d" % (I % 2)
                        xkeys = transpose_rows(xoi, xok, 128, [xTi], [xTk])
                        qTi, qk = qT[I % 2], "qT%d" % (I % 2)
                        bq, bqk = bank()
                        for h in range(4):
                            for kc in range(8):
                                P(lambda: nc.tensor.matmul(bq[:, h * 128:(h + 1) * 128], lhsT=wq[:, kc, h * 128:(h + 1) * 128], rhs=xTi[:, kc, :],
                                                           start=(kc == 0), stop=(kc == 7)), reads=xkeys + ["wq"], writes=[bqk], inc=(kc == 7 and h == 3))
                        for h in range(4):
                            A(lambda: nc.scalar.activation(out=qTi[:, h, :], in_=bq[:, h * 128:(h + 1) * 128], func=AF.Identity, bias=binT[:, h:h + 1], scale=1.0),
                              reads=[bqk, "binT"], writes=[qk])
                        oti, otk = otok[I % 2], "otok%d" % (I % 2)
                        nkb = 2 * I + 2
                        for h in range(4):
                            ab, abk = accbank()
                            acc = ab[:, 0:260].rearrange("p (m v) -> p m v", v=130)
                            for m in range(2):
                                ps_ = slice(m * 64, (m + 1) * 64)
                                for g0 in range(0, nkb, 4):
                                    grp = list(range(g0, min(g0 + 4, nkb)))
                                    sbk_, sbkk = bank()
                                    for j, kb in enumerate(grp):
                                        r = kb - (2 * I - 1)
                                        special = 0 <= r <= 2
                                        osl = sbk_[:, j * 128:(j + 1) * 128]
                                        last = (j == len(grp) - 1)
                                        P(lambda: nc.tensor.matmul(osl, lhsT=KT[ps_, h, kb * 128:(kb + 1) * 128], rhs=qTi[ps_, h, :], start=True, stop=not special),
                                          reads=[qk], writes=[sbkk], inc=(last and not special))
                                        if special:
                                            P(lambda: nc.tensor.matmul(osl, lhsT=identb[:], rhs=bthi[:, r, h, :], start=False, stop=False),
                                              reads=["identb"], writes=[sbkk], inc=False)
                                            P(lambda: nc.tensor.matmul(osl, lhsT=identb[:], rhs=btlo[:, r, h, :], start=False, stop=True),
                                              reads=[], writes=[sbkk], inc=last)
                                    n = len(grp)
                                    pt_, ptk = PT[pti % 3], "PT%d" % (pti % 3)
                                    pti += 1
                                    A(lambda: nc.scalar.activation(out=pt_[:, 0:n, :], in_=sbk_[:, 0:n * 128].rearrange("p (j q) -> p j q", q=128),
                                                                   func=AF.Exp, scale=0.125), reads=[sbkk], writes=[ptk])
                                    for j, kb in enumerate(grp):
                                        P(lambda: nc.tensor.matmul(acc[:, m, :], lhsT=pt_[:, j, :], rhs=VA[:, kb, h, :], start=(kb == 0), stop=(kb == nkb - 1)),
                                          reads=[ptk], writes=[abk], inc=(j == n - 1))
                            combine(acc, abk, 128, osb[:], "osb", scr2)
                            subln_store(osb[:], "osb", 128, oti[:, h, :], otk, scr)
                        bq, bqk = bank()
                        for h in range(4):
                            P(lambda: nc.tensor.transpose(out=bq[:, h * 128:(h + 1) * 128], in_=oti[:, h, :], identity=ident[:]),
                              reads=[otk, "ident"], writes=[bqk], inc=(h == 3))
                        A(lambda: nc.scalar.copy(out=oT[:, :, I * 128:(I + 1) * 128], in_=bq[:, :].rearrange("p (h q) -> p h q", q=128)),
                          reads=[bqk], writes=[uniq("oT")])
                    S.barrier()
                if KSTOP == 2:
                    S.finish("sp")
                    raise _Stop()
            with ExitStack() as esS:
                sbS = lambda n, s, dt=F32: _sbuf(esS, n, s, dt)
                wq = sbS("wq", [128, 8, 512], BF16); wk = sbS("wk", [128, 8, 512], BF16)
                wv = sbS("wv", [128, 8, 512], BF16); wc = sbS("wc", [128, 8, 1024], BF16)
                xss = sbS("xss", [128, D]); xTs = sbS("xTs", [128, 8, 128], BF16)
                qTs = sbS("qTs", [128, 4, ST], BF16); kTs = sbS("kTs", [128, 4, ST], BF16)
                tks = sbS("tks", [128, 512]); vnew = sbS("vnew", [4, SS, 4, 130], BF16)
                uT = sbS("uT", [128, 4, ST]); sig = sbS("sig", [128, ST]); utok = sbS("utok", [ST, 512])
                cbuf = sbS("cbuf", [128, 4, SS, 34]); scv = [sbS("scv%d" % i, [120, 512]) for i in range(2)]
                ycs = sbS("ycs", [128, 4, ST]); ysqs = sbS("ysqs", [128, 4, ST])
                mnb = sbS("mnb", [128, ST]); m2 = sbS("m2", [128, ST]); var = sbS("var", [128, ST]); rstd = sbS("rstd", [128, ST]); tt = sbS("tt", [128, ST]); sdt = sbS("sdt", [128, ST])
                kpg = [sbS("kpg%d" % i, [128, 512]) for i in range(4)]
                vpg = [sbS("vpg%d" % i, [128, 512], BF16) for i in range(4)]
                KTs = sbS("KTs", [128, 4, 16, 128], BF16); VAs = sbS("VAs", [128, 16, 4, 130], BF16)
                PTs = [sbS("PTs%d" % i, [128, 16, 4], BF16) for i in range(2)]
                PTn = [sbS("PTn%d" % i, [4, 4], BF16) for i in range(2)]
                os_ = sbS("os_", [4, 128]); otoks = sbS("otoks", [4, 4, 128])
                scr = (sbS("sl_sq", [128, 128]), sbS("sl_ss", [128, 1]), sbS("sl_sd", [128, 1]), sbS("sl_rs", [128, 1]))
                scr2 = (sbS("cb_r0", [128, 1]), sbS("cb_r1", [128, 1]))
                for wt, off, n_, nm in ((wq, QOFF, 512, "wq"), (wk, KOFF, 512, "wk"), (wv, VOFF, 512, "wv"), (wc, AOFF, 1024, "wc")):
                    ldc(lambda: nc.gpsimd.dma_start(out=wt[:], in_=w_in_v[:, :, off:off + n_]), writes=[nm])
                V(lambda: nc.vector.memset(VAs[:, :, :, 128:129], 1.0), writes=["VAs1"])
                V(lambda: nc.vector.memset(VAs[:, :, :, 129:130], 0.0), writes=["VAs0"])
                V(lambda: nc.vector.memset(vnew[:, :, :, 128:129], 1.0), writes=["vn1"])
                V(lambda: nc.vector.memset(vnew[:, :, :, 129:130], 0.0), writes=["vn0"])
                ld(lambda: nc.sync.dma_start(out=xss[0:ST, :], in_=xs[:, :]), writes=["xss"])
                xkeys = transpose_rows(xss, "xss", ST, [xTs], ["xTs"])
                xc = xTs
                for (w_, wkey, dstT, dk, c0) in ((wq, "wq", qTs, "qTs", 0), (wk, "wk", kTs, "kTs", 4)):
                    bq, bqk = bank()
                    for h in range(4):
                        for kc in range(8):
                            P(lambda: nc.tensor.matmul(bq[:, h * ST:(h + 1) * ST], lhsT=w_[:, kc, h * 128:(h + 1) * 128], rhs=xc[:, kc, 0:ST],
                                                       start=(kc == 0), stop=(kc == 7)), reads=xkeys + [wkey], writes=[bqk], inc=(kc == 7 and h == 3))
                    for h in range(4):
                        A(lambda: nc.scalar.activation(out=dstT[:, h, :], in_=bq[:, h * ST:(h + 1) * ST], func=AF.Identity, bias=binT[:, c0 + h:c0 + h + 1], scale=1.0),
                          reads=[bqk, "binT"], writes=[dk])
                for (w_, wkey, bias_, dst) in ((wk, "wk", bk_bc, nk_s), (wv, "wv", bv_bc, nv_s)):
                    bq, bqk = bank()
                    mm(bq[0:ST, :], bqk, [(xc[:, kc, 0:ST], w_[:, kc, :]) for kc in range(8)], xkeys + [wkey])
                    V(lambda: nc.vector.tensor_tensor(out=tks[0:ST, :], in0=bq[0:ST, :], in1=bias_[0:ST, :], op=ALU.add), reads=[bqk, "bk_bc", "bv_bc"], writes=["tks"])
                    ld(lambda: nc.sync.dma_start(out=dst[:, :], in_=tks[0:ST, :]), reads=["tks"], writes=[uniq("oks")])
                for s in range(SS):
                    bq, bqk = bank()
                    mm(bq[0:4, :], bqk, [(xc[:, kc, 4 * s:4 * s + 4], wv[:, kc, :]) for kc in range(8)], xkeys + ["wv"])
                    V(lambda: nc.vector.tensor_tensor(out=vnew[0:4, s, :, 0:128], in0=bq[0:4, :].rearrange("p (h v) -> p h v", v=128),
                                                      in1=bv_bc[0:4, :].rearrange("p (h v) -> p h v", v=128), op=ALU.add),
                      reads=[bqk, "bv_bc", "vn1", "vn0"], writes=["vnew"])
                for cc in range(4):
                    ba_, bak = bank()
                    mm(ba_[:, 0:ST], bak, [(wc[:, kc, cc * 128:(cc + 1) * 128], xc[:, kc, 0:ST]) for kc in range(8)], xkeys + ["wc"])
                    bg_, bgk = bank()
                    mm(bg_[:, 0:ST], bgk, [(wc[:, kc, 512 + cc * 128:512 + (cc + 1) * 128], xc[:, kc, 0:ST]) for kc in range(8)], xkeys + ["wc"])
                    A(lambda: nc.scalar.activation(out=sig[:], in_=bg_[:, 0:ST], func=AF.Sigmoid, bias=binT[:, 16 + cc:17 + cc], scale=1.0), reads=[bgk, "binT"], writes=["sig"])
                    V(lambda: nc.vector.scalar_tensor_tensor(out=uT[:, cc, :], in0=ba_[:, 0:ST], scalar=binT[:, 12 + cc:13 + cc], in1=sig[:], op0=ALU.add, op1=ALU.mult),
                      reads=[bak, "sig", "binT"], writes=["uT"])
                bq, bqk = bank()
                for cc in range(4):
                    P(lambda: nc.tensor.transpose(out=bq[0:ST, cc * 128:(cc + 1) * 128], in_=uT[:, cc, :], identity=ident[:]), reads=["uT", "ident"], writes=[bqk], inc=(cc == 3))
                V(lambda: nc.vector.tensor_copy(out=utok[:], in_=bq[0:ST, :]), reads=[bqk], writes=["utok"])
                for s in range(SS):
                    ld(lambda: nc.sync.dma_start(out=nc_s[s, 26:30, :], in_=utok[4 * s:4 * s + 4, :]), reads=["utok"], writes=["nc_s_b%d" % s])
                for g in range(4):
                    sc, sck = scv[g % 2], "scv%d" % (g % 2)
                    ld(lambda: nc.sync.dma_start(out=sc[:], in_=sconv[4 * g:4 * g + 4].rearrange("s r c -> (s r) c")), writes=[sck])
                    for cc in range(4):
                        bq, bqk = bank()
                        P(lambda: nc.tensor.transpose(out=bq[:, 0:120], in_=sc[0:120, cc * 128:(cc + 1) * 128], identity=ident[0:120, 0:120]), reads=[sck, "ident"], writes=[bqk])
                        A(lambda: nc.scalar.copy(out=cbuf[:, cc, 4 * g:4 * g + 4, 0:30], in_=bq[:, 0:120].rearrange("p (s r) -> p s r", r=30)), reads=[bqk], writes=[uniq("cbuf")])
                S.barrier()
                V(lambda: nc.vector.tensor_copy(out=cbuf[:, :, :, 30:34], in_=uT[:].rearrange("p c (s t) -> p c s t", t=4)), reads=[], writes=["cbuf"])
                for cc in range(4):
                    yv = ycs[:, cc, :].rearrange("p (s t) -> p s t", t=4)
                    V(lambda: nc.vector.tensor_scalar(out=yv, in0=cbuf[:, cc, :, 0:4], scalar1=cwT[:, cc, 0:1], scalar2=cbT[:, cc:cc + 1], op0=ALU.mult, op1=ALU.add),
                      reads=["cbuf"], writes=["ycs%d" % cc])
                    for w in range(1, 31):
                        V(lambda: nc.vector.scalar_tensor_tensor(out=yv, in0=cbuf[:, cc, :, w:w + 4], scalar=cwT[:, cc, w:w + 1], in1=yv, op0=ALU.mult, op1=ALU.add),
                          reads=["cbuf", "ycs%d" % cc], writes=["ycs%d" % cc])
                ykeys = ["ycs%d" % cc for cc in range(4)]
                G(lambda: nc.gpsimd.tensor_tensor(out=ysqs[:], in0=ycs[:], in1=ycs[:], op=ALU.mult), reads=ykeys, writes=["ysqs"])
                bm, bmk = bank()
                mm(bm[:, 0:ST], bmk, [(onesM[:], ycs[:, cc, :]) for cc in range(4)], ykeys)
                bs, bsk = bank()
                mm(bs[:, 0:ST], bsk, [(onesM[:], ysqs[:, cc, :]) for cc in range(4)], ["ysqs"])
                A(lambda: nc.scalar.copy(out=mnb[:], in_=bm[:, 0:ST]), reads=[bmk], writes=["mnb"])
                G(lambda: nc.gpsimd.tensor_tensor(out=m2[:], in0=mnb[:], in1=mnb[:], op=ALU.mult), reads=["mnb"], writes=["m2"])
                V(lambda: nc.vector.tensor_tensor(out=var[:], in0=bs[:, 0:ST], in1=m2[:], op=ALU.subtract), reads=[bsk, "m2"], writes=["var"])
                ln_rstd(var[:], rstd[:], sdt[:], ["var"], "rstd")
                for cc in range(4):
                    V(lambda: nc.vector.tensor_tensor(out=tt[:], in0=ycs[:, cc, :], in1=mnb[:], op=ALU.subtract), reads=["ycs%d" % cc, "mnb"], writes=["tt"])
                    G(lambda: nc.gpsimd.tensor_tensor(out=tt[:], in0=tt[:], in1=rstd[:], op=ALU.mult), reads=["tt", "rstd"], writes=["tt"])
                    A(lambda: nc.scalar.activation(out=sT[:, cc, TOK:TOK + ST], in_=tt[:], func=AF.Silu, bias=lbT[:, cc:cc + 1], scale=lgT[:, cc:cc + 1]),
                      reads=["tt"], writes=[uniq("sTs")])
                kpi = 0
                for s in range(SS):
                    for pg in range(16):
                        col = s * 16 + pg
                        kp, kpk = kpg[kpi % 4], "kpg%d" % (kpi % 4)
                        kpi += 1
                        ldc(lambda: nc.gpsimd.indirect_dma_start(out=kp[:, :], out_offset=None, in_=ck, in_offset=bass.IndirectOffsetOnAxis(ap=idx[:, col:col + 1], axis=0)),
                            reads=["idx"], writes=[kpk])
                        vp, vpk = vpg[(kpi - 1) % 4], "vpg%d" % ((kpi - 1) % 4)
                        ldc(lambda: nc.gpsimd.indirect_dma_start(out=vp[:, :], out_offset=None, in_=cv, in_offset=bass.IndirectOffsetOnAxis(ap=idx[:, col:col + 1], axis=0)),
                            reads=["idx"], writes=[vpk])
                        G(lambda: nc.gpsimd.tensor_copy(out=VAs[:, pg, :, 0:128], in_=vp[:, :].rearrange("p (h v) -> p h v", v=128)),
                          reads=[vpk, "VAs1", "VAs0"], writes=["VAs_%d" % pg])
                        bq, bqk = bank()
                        for h in range(4):
                            P(lambda: nc.tensor.transpose(out=bq[:, h * 128:(h + 1) * 128], in_=kp[:, h * 128:(h + 1) * 128], identity=ident[:]),
                              reads=[kpk, "ident"], writes=[bqk], inc=(h == 3))
                        cpk = "KTs_%d" % pg
                        if pg % 2 == 0:
                            A(lambda: nc.scalar.copy(out=KTs[:, :, pg, :], in_=bq[:, :].rearrange("p (h k) -> p h k", k=128)), reads=[bqk], writes=[cpk])
                        else:
                            V(lambda: nc.vector.tensor_copy(out=KTs[:, :, pg, :], in_=bq[:, :].rearrange("p (h k) -> p h k", k=128)), reads=[bqk], writes=[cpk])
                    ktkeys = ["KTs_%d" % pg for pg in range(16)]
                    vakeys = ["VAs_%d" % pg for pg in range(16)]
                    qs = slice(4 * s, 4 * s + 4)
                    for h in range(4):
                        ab, abk = accbank()
                        acc = ab[:, 0:260].rearrange("p (m v) -> p m v", v=130)
                        for m in range(2):
                            ps_ = slice(m * 64, (m + 1) * 64)
                            sbk_, sbkk = bank()
                            for pg in range(16):
                                osl = sbk_[:, pg * 4:(pg + 1) * 4]
                                sp_ = (pg == 15)
                                P(lambda: nc.tensor.matmul(osl, lhsT=KTs[ps_, h, pg, :], rhs=qTs[ps_, h, qs], start=True, stop=not sp_),
                                  reads=ktkeys + ["qTs"], writes=[sbkk], inc=False)
                                if sp_:
                                    P(lambda: nc.tensor.matmul(osl, lhsT=identb[:], rhs=bthi[:, 3, h, 0:4], start=False, stop=False), reads=[], writes=[sbkk], inc=False)
                                    P(lambda: nc.tensor.matmul(osl, lhsT=identb[:], rhs=btlo[:, 3, h, 0:4], start=False, stop=True), reads=[], writes=[sbkk], inc=False)
                            osn = sbk_[0:4, 64:68]
                            P(lambda: nc.tensor.matmul(osn, lhsT=kTs[ps_, h, qs], rhs=qTs[ps_, h, qs], start=True, stop=False), reads=["kTs", "qTs"], writes=[sbkk], inc=False)
                            P(lambda: nc.tensor.matmul(osn, lhsT=identb[0:4, 0:4], rhs=bthi[0:4, 4, h, 0:4], start=False, stop=False), reads=[], writes=[sbkk], inc=False)
                            P(lambda: nc.tensor.matmul(osn, lhsT=identb[0:4, 0:4], rhs=btlo[0:4, 4, h, 0:4], start=False, stop=True), reads=[], writes=[sbkk], inc=True)
                            pi = (2 * h + m) % 2
                            pt_, ptk = PTs[pi], "PTs%d" % pi
                            pn_, pnk = PTn[pi], "PTn%d" % pi
                            A(lambda: nc.scalar.activation(out=pt_[:], in_=sbk_[:, 0:64].rearrange("p (g q) -> p g q", q=4), func=AF.Exp, scale=0.125), reads=[sbkk], writes=[ptk])
                            A(lambda: nc.scalar.activation(out=pn_[:], in_=sbk_[0:4, 64:68], func=AF.Exp, scale=0.125), reads=[sbkk], writes=[pnk])
                            for pg in range(16):
                                P(lambda: nc.tensor.matmul(acc[0:4, m, :], lhsT=pt_[:, pg, :], rhs=VAs[:, pg, h, :], start=(pg == 0), stop=False),
                                  reads=[ptk] + vakeys, writes=[abk], inc=False)
                            P(lambda: nc.tensor.matmul(acc[0:4, m, :], lhsT=pn_[:], rhs=vnew[0:4, s, h, :], start=False, stop=True), reads=[pnk, "vnew"], writes=[abk], inc=True)
                        combine(acc, abk, 4, os_[:], "os_", scr2)
                        subln_store(os_[:], "os_", 4, otoks[0:4, h, :], "otoks", scr)
                    bq, bqk = bank()
                    for h in range(4):
                        P(lambda: nc.tensor.transpose(out=bq[:, h * 4:(h + 1) * 4], in_=otoks[0:4, h, :], identity=ident[0:4, 0:4]), reads=["otoks", "ident"], writes=[bqk], inc=(h == 3))
                    V(lambda: nc.vector.tensor_copy(out=oT[:, :, TOK + 4 * s:TOK + 4 * s + 4], in_=bq[:, 0:16].rearrange("p (h q) -> p h q", q=4)), reads=[bqk], writes=[uniq("oTs")])
                S.barrier()
                if KSTOP == 3:
                    S.finish("sp")
                    raise _Stop()
            with ExitStack() as es3:
                sb3 = lambda n, s, dt=F32: _sbuf(es3, n, s, dt)
                wg = sb3("wg", [128, 8, 2048], BF16); wap = sb3("wap", [128, 4, D], BF16); wcp = sb3("wcp", [128, 4, D], BF16)
                wout = sb3("wout", [128, 8, D], BF16)
                bgg = sb3("bgg", [128, 2048]); bcpb = sb3("bcpb", [128, D]); g1b = sb3("g1b", [128, D]); b1b = sb3("b1b", [128, D])
                rw = sb3("rw", [128, 8, 32]); rbb = sb3("rbb", [128, 32]); b2all = sb3("b2all", [32, D])
                xo = [sb3("xo%d" % i, [128, D]) for i in range(2)]
                xTo = sb3("xTo", [128, 8, 128], BF16)
                sgt = sb3("sgt", [128, 2048]); mt = sb3("mt", [128, D]); t2 = sb3("t2", [128, 512]); mT = sb3("mT", [128, 8, 128], BF16)
                pre = sb3("pre", [128, D]); h1 = sb3("h1", [128, D]); ya = sb3("ya", [128, D])
                h1Tf = sb3("h1Tf", [128, 8, 128]); h1Tb = sb3("h1Tb", [128, 8, 128], BF16)
                bn = sb3("bn", [128, 2, 6]); mv = sb3("mv", [128, 2]); sd = sb3("sd", [128, 1]); rs1 = sb3("rs1", [128, 1])
                lg = sb3("lg", [128, 32]); t8 = sb3("t8", [128, 8]); msk = sb3("msk", [128, 32]); nmx = sb3("nmx", [128, 1])
                ex = sb3("ex", [128, 32]); ssum = sb3("ssum", [128, 1]); gt = sb3("gt", [128, 32]); gtT = sb3("gtT", [32, 128])
                ldc(lambda: nc.gpsimd.dma_start(out=wg[:], in_=w_in_v[:, :, GAOFF:GAOFF + 2048]), writes=["wg"])
                ldc(lambda: nc.gpsimd.dma_start(out=wap[:], in_=wap_d.rearrange("(c p) n -> p c n", p=128)), writes=["wap"])
                ldc(lambda: nc.gpsimd.dma_start(out=wcp[:], in_=wcp_d.rearrange("(c p) n -> p c n", p=128)), writes=["wcp"])
                ldc(lambda: nc.gpsimd.dma_start(out=wout[:], in_=wout_d.rearrange("(c p) n -> p c n", p=128)), writes=["wout"])
                ld(lambda: nc.sync.dma_start(out=bgg[:], in_=b_in[GAOFF:GAOFF + 2048].partition_broadcast(128)), writes=["bgg"])
                ld(lambda: nc.sync.dma_start(out=bcpb[:], in_=bcp_d.partition_broadcast(128)), writes=["bcpb"])
                ld(lambda: nc.sync.dma_start(out=g1b[:], in_=ln1g.partition_broadcast(128)), writes=["g1b"])
                ld(lambda: nc.sync.dma_start(out=b1b[:], in_=ln1b.partition_broadcast(128)), writes=["b1b"])
                ld(lambda: nc.sync.dma_start(out=rw[:], in_=rw_d.rearrange("(c p) e -> p c e", p=128)), writes=["rw"])
                ld(lambda: nc.sync.dma_start(out=rbb[:], in_=rbias.partition_broadcast(128)), writes=["rbb"])
                ld(lambda: nc.sync.dma_start(out=b2all[:], in_=b2_d), writes=["b2all"])
                V(lambda: nc.vector.memset(h1Tb[:], 0.0), writes=["h1Tbh0", "h1Tbh1"])
                V(lambda: nc.vector.memset(h1Tf[:], 0.0), writes=["h1Tfh0", "h1Tfh1"])
                for I in range(NTILE):
                    rows = 128 if I < NQ else ST
                    R = slice(0, rows)
                    cols = slice(I * 128, I * 128 + rows)
                    xoi, xok = xo[I % 2], "xo%d" % (I % 2)
                    if I < NQ:
                        for hh in range(2):
                            r0 = (2 * I + hh) * 128 + 64
                            ld(lambda: nc.sync.dma_start(out=xoi[hh * 64:(hh + 1) * 64, :], in_=xloc[r0:r0 + 64, :]), writes=[xok])
                    else:
                        ld(lambda: nc.sync.dma_start(out=xoi[R, :], in_=xs[:, :]), writes=[xok])
                    xkeys = transpose_rows(xoi, xok, rows, [xTo], ["xTo"])
                    for p_ in range(4):
                        bq, bqk = bank()
                        mm(bq[R, :], bqk, [(xTo[:, kc, R], wg[:, kc, p_ * 512:(p_ + 1) * 512]) for kc in range(8)], xkeys + ["wg"])
                        V(lambda: nc.vector.tensor_tensor(out=sgt[R, p_ * 512:(p_ + 1) * 512], in0=bq[R, :], in1=bgg[R, p_ * 512:(p_ + 1) * 512], op=ALU.add),
                          reads=[bqk, "bgg"], writes=["sgt"])
                    A(lambda: nc.scalar.activation(out=sgt[R, :], in_=sgt[R, :], func=AF.Sigmoid), reads=["sgt"], writes=["sgt"])
                    stop_here(41)
                    for half in range(2):
                        hs = slice(half * 512, (half + 1) * 512)
                        bq, bqk = bank()
                        mm(bq[R, :], bqk, [(oT[:, h, cols], wap[:, h, hs]) for h in range(4)], ["wap"])
                        V(lambda: nc.vector.tensor_tensor(out=mt[R, hs], in0=bq[R, :], in1=sgt[R, hs], op=ALU.mult), reads=[bqk, "sgt"], writes=["mt%d" % half])
                        bq, bqk = bank()
                        mm(bq[R, :], bqk, [(sT[:, cc, cols], wcp[:, cc, hs]) for cc in range(4)], ["wcp"])
                        V(lambda: nc.vector.tensor_tensor(out=t2[R, :], in0=bq[R, :], in1=bcpb[R, hs], op=ALU.add), reads=[bqk, "bcpb"], writes=["t2"])
                        G(lambda: nc.gpsimd.tensor_tensor(out=t2[R, :], in0=t2[R, :], in1=sgt[R, 1024 + half * 512:1024 + (half + 1) * 512], op=ALU.mult),
                          reads=["t2", "sgt"], writes=["t2"])
                        G(lambda: nc.gpsimd.tensor_tensor(out=mt[R, hs], in0=mt[R, hs], in1=t2[R, :], op=ALU.add), reads=["t2", "mt%d" % half], writes=["mt%d" % half])
                    stop_here(42)
                    mkeys = transpose_rows(mt, ["mt0", "mt1"], rows, [mT], ["mT"])
                    for half in range(2):
                        hs = slice(half * 512, (half + 1) * 512)
                        bq, bqk = bank()
                        mm(bq[R, :], bqk, [(mT[:, kc, R], wout[:, kc, hs]) for kc in range(8)], mkeys + ["wout"])
                        V(lambda: nc.vector.scalar_tensor_tensor(out=pre[R, hs], in0=xoi[R, hs], scalar=ALPHA, in1=bq[R, :], op0=ALU.mult, op1=ALU.add),
                          reads=[bqk, xok], writes=["pre%d" % half])
                        V(lambda: nc.vector.bn_stats(out=bn[R, half, :], in_=pre[R, hs]), reads=["pre%d" % half], writes=["bn%d" % half])
                    V(lambda: nc.vector.bn_aggr(out=mv[R, :], in_=bn[R, :, :]), reads=["bn0", "bn1"], writes=["mv"])
                    ln_rstd(mv[R, 1:2], rs1[R, :], sd[R, :], ["mv"], "rs1")
                    V(lambda: nc.vector.tensor_scalar(out=h1[R, :], in0=pre[R, :], scalar1=mv[R, 0:1], scalar2=rs1[R, 0:1], op0=ALU.subtract, op1=ALU.mult),
                      reads=["pre0", "pre1", "mv", "rs1"], writes=["h1"])
                    G(lambda: nc.gpsimd.tensor_tensor(out=h1[R, :], in0=h1[R, :], in1=g1b[R, :], op=ALU.mult), reads=["h1", "g1b"], writes=["h1"])
                    G(lambda: nc.gpsimd.tensor_tensor(out=h1[R, :], in0=h1[R, :], in1=b1b[R, :], op=ALU.add), reads=["h1", "b1b"], writes=["h1"])
                    stop_here(43)
                    hkeys = transpose_rows(h1, "h1", rows, [h1Tf, h1Tb], ["h1Tf", "h1Tb"])
                    stop_here(44)
                    ld(lambda: nc.sync.dma_start(out=h1T_scr[:, :, I * 128:(I + 1) * 128], in_=h1Tb[:]), reads=["h1Tbh0", "h1Tbh1"], writes=[uniq("h1Ts")])
                    bq, bqk = bank()
                    mm(bq[R, 0:32], bqk, [(h1Tf[:, kc, R], rw[:, kc, :]) for kc in range(8)], ["h1Tfh0", "h1Tfh1", "rw"])
                    V(lambda: nc.vector.tensor_tensor(out=lg[R, :], in0=bq[R, 0:32], in1=rbb[R, :], op=ALU.add), reads=[bqk, "rbb"], writes=["lg"])
                    V(lambda: nc.vector.max(out=t8[R, :], in_=lg[R, :]), reads=["lg"], writes=["t8"])
                    V(lambda: nc.vector.tensor_scalar(out=msk[R, :], in0=lg[R, :], scalar1=t8[R, 3:4], scalar2=None, op0=ALU.is_ge), reads=["lg", "t8"], writes=["msk"])
                    V(lambda: nc.vector.tensor_scalar(out=nmx[R, :], in0=t8[R, 0:1], scalar1=-1.0, scalar2=None, op0=ALU.mult), reads=["t8"], writes=["nmx"])
                    A(lambda: nc.scalar.activation(out=ex[R, :], in_=lg[R, :], func=AF.Exp, bias=nmx[R, 0:1], scale=1.0), reads=["lg", "nmx"], writes=["ex"])
                    V(lambda: nc.vector.tensor_tensor(out=ex[R, :], in0=ex[R, :], in1=msk[R, :], op=ALU.mult), reads=["ex", "msk"], writes=["ex"])
                    V(lambda: nc.vector.reduce_sum(out=ssum[R, :], in_=ex[R, :], axis=AX.X), reads=["ex"], writes=["ssum"])
                    V(lambda: nc.vector.reciprocal(out=ssum[R, :], in_=ssum[R, :]), reads=["ssum"], writes=["ssum"])
                    V(lambda: nc.vector.tensor_scalar(out=gt[R, :], in0=ex[R, :], scalar1=ssum[R, 0:1], scalar2=None, op0=ALU.mult), reads=["ex", "ssum"], writes=["gt"])
                    stop_here(45)
                    ld(lambda: nc.sync.dma_start(out=gates_scr[I, R, :], in_=gt[R, :]), reads=["gt"], writes=[uniq("gts")])
                    bq, bqk = bank()
                    P(lambda: nc.tensor.transpose(out=bq[0:32, 0:rows], in_=gt[R, :], identity=ident[R, R]), reads=["gt", "ident"], writes=[bqk])
                    A(lambda: nc.scalar.copy(out=gtT[:, R], in_=bq[0:32, 0:rows]), reads=[bqk], writes=["gtT"])
                    for half in range(2):
                        hs = slice(half * 512, (half + 1) * 512)
                        bq, bqk = bank()
                        mm(bq[R, :], bqk, [(gtT[:, R], b2all[:, hs])], ["gtT", "b2all"])
                        V(lambda: nc.vector.scalar_tensor_tensor(out=ya[R, hs], in0=h1[R, hs], scalar=ALPHA, in1=bq[R, :], op0=ALU.mult, op1=ALU.add),
                          reads=[bqk, "h1"], writes=["ya%d" % half])
                    ld(lambda: nc.sync.dma_start(out=yacc_scr[I, R, :], in_=ya[R, :]), reads=["ya0", "ya1"], writes=[uniq("yas")])
                    stop_here(46)
                    if I == 15:
                        stop_here(47)
                S.barrier()
                if KSTOP == 4:
                    S.finish("sp")
                    raise _Stop()
        with ExitStack() as esB:
            sbB = lambda n, s, dt=F32: _sbuf(esB, n, s, dt)
            b1T = sbB("b1T", [128, 16, 32])
            with ExitStack() as est:
                b1rows = _sbuf(est, "b1rows", [32, 2048], F32)
                ld(lambda: nc.sync.dma_start(out=b1rows[:], in_=b1_d), writes=["b1rows"])
                bq, bqk = bank()
                for c_ in range(16):
                    P(lambda: nc.tensor.transpose(out=bq[:, c_ * 32:(c_ + 1) * 32], in_=b1rows[0:32, c_ * 128:(c_ + 1) * 128], identity=ident[0:32, 0:32]),
                      reads=["b1rows", "ident"], writes=[bqk], inc=(c_ == 15))
                V(lambda: nc.vector.tensor_copy(out=b1T[:], in_=bq[:, :].rearrange("p (c e) -> p c e", e=32)), reads=[bqk], writes=["b1T"])
                S.barrier()
            b1T1 = sbB("b1T1", [128, 8, 32])
            V(lambda: nc.vector.tensor_scalar(out=b1T1[:], in0=b1T[:, 8:16, :], scalar1=1.0, scalar2=None, op0=ALU.add), writes=["b1T1"])
            w1t = [sbB("w1t%d" % i, [128, 8, 2048], BF16) for i in range(2)]
            w2t = [sbB("w2t%d" % i, [128, 8, D], BF16) for i in range(2)]
            h1Th = sbB("h1Th", [128, 8, 9 * 128], BF16); yacc = sbB("yacc", [128, 9, D]); gts = sbB("gts", [128, 9, 32])
            g2b = sbB("g2b", [128, D]); b2b = sbB("b2b", [128, D])
            g32 = [sbB("g32_%d" % i, [128, 512]) for i in range(2)]; sg32 = [sbB("sg32_%d" % i, [128, 512]) for i in range(2)]
            u32 = [sbB("u32_%d" % i, [128, 512]) for i in range(2)]
            actT = [sbB("actT%d" % i, [128, 8, 512], BF16) for i in range(2)]
            bn = sbB("bn", [128, 2, 6]); mv = sbB("mv", [128, 2]); sd = sbB("sd", [128, 1]); rs1 = sbB("rs1", [128, 1]); yo = [sbB("yo%d" % i, [128, D]) for i in range(2)]
            ld(lambda: nc.sync.dma_start(out=g2b[:], in_=ln2g.partition_broadcast(128)), writes=["g2b"])
            ld(lambda: nc.sync.dma_start(out=b2b[:], in_=ln2b.partition_broadcast(128)), writes=["b2b"])
            def load_expert(e_, slot):
                w1e_, w1k_ = w1t[slot % 2], "w1t%d" % (slot % 2)
                w2e_, w2k_ = w2t[slot % 2], "w2t%d" % (slot % 2)
                for q in range(4):
                    ldc(lambda: nc.gpsimd.dma_start(out=w1e_[:, :, q * 512:(q + 1) * 512], in_=w1_d[e_].rearrange("(c p) f -> p c f", p=128)[:, :, q * 512:(q + 1) * 512]),
                        writes=[w1k_ + "_%d" % q])
                for q in range(2):
                    ldc(lambda: nc.gpsimd.dma_start(out=w2e_[:, :, q * 512:(q + 1) * 512], in_=w2_d[e_].rearrange("(c p) f -> p c f", p=128)[:, :, q * 512:(q + 1) * 512]),
                        writes=[w2k_ + "_%d" % q])

            for hf, tiles in enumerate(HALVES):
                nt = len(tiles)
                ncols = nt * 128
                t0 = tiles[0]
                ld(lambda: nc.sync.dma_start(out=h1Th[:, :, 0:ncols], in_=h1T_scr[:, :, t0 * 128:t0 * 128 + ncols]), writes=["h1Th"])
                ld(lambda: nc.sync.dma_start(out=yacc[:, 0:nt, :], in_=yacc_scr[t0:t0 + nt].rearrange("t p d -> p t d")), writes=["yacc%d" % i for i in range(nt)])
                ld(lambda: nc.sync.dma_start(out=gts[:, 0:nt, :], in_=gates_scr[t0:t0 + nt].rearrange("t p e -> p t e")), writes=["gts"])
                groups = [(g0, min(512, ncols - g0)) for g0 in range(0, ncols, 512)]
                items = [(e, gi) for e in range(32) for gi in range(len(groups))]

                def stage_a(it):
                    e, gi = items[it]
                    g0, n = groups[gi]
                    slot = (hf * 32 + e) % 2
                    w1e, w1k = w1t[slot], "w1t%d" % slot
                    at, atk = actT[it % 2], "actT%d" % (it % 2)
                    akeys = []
                    for fc in range(8):
                        bi = fc % 2
                        gg, ggk = g32[bi], "g32_%d" % bi
                        sgg, sgk = sg32[bi], "sg32_%d" % bi
                        uu, uuk = u32[bi], "u32_%d" % bi
                        bgq, bgk = bank()
                        mm(bgq[:, 0:n], bgk, [(w1e[:, kc, fc * 128:(fc + 1) * 128], h1Th[:, kc, g0:g0 + n]) for kc in range(8)], ["h1Th", w1k + "_%d" % (fc // 4)])
                        buq, buk = bank()
                        mm(buq[:, 0:n], buk, [(w1e[:, kc, 1024 + fc * 128:1024 + (fc + 1) * 128], h1Th[:, kc, g0:g0 + n]) for kc in range(8)],
                           ["h1Th", w1k + "_%d" % (2 + fc // 4)])
                        V(lambda: nc.vector.tensor_scalar(out=gg[:, 0:n], in0=bgq[:, 0:n], scalar1=b1T[:, fc, e:e + 1], scalar2=7.0, op0=ALU.add, op1=ALU.min),
                          reads=[bgk, "b1T"], writes=[ggk])
                        A(lambda: nc.scalar.activation(out=sgg[:, 0:n], in_=gg[:, 0:n], func=AF.Sigmoid, scale=1.702), reads=[ggk], writes=[sgk])
                        V(lambda: nc.vector.tensor_scalar(out=uu[:, 0:n], in0=buq[:, 0:n], scalar1=b1T1[:, fc, e:e + 1], scalar2=8.0, op0=ALU.add, op1=ALU.min),
                          reads=[buk, "b1T1"], writes=[uuk])
                        V(lambda: nc.vector.tensor_tensor(out=sgg[:, 0:n], in0=sgg[:, 0:n], in1=gg[:, 0:n], op=ALU.mult), reads=[sgk, ggk], writes=[sgk])
                        k = atk + "_%d" % fc
                        V(lambda: nc.vector.scalar_tensor_tensor(out=at[:, fc, 0:n], in0=uu[:, 0:n], scalar=-6.0, in1=sgg[:, 0:n], op0=ALU.max, op1=ALU.mult),
                          reads=[uuk, sgk], writes=[k])
                        akeys.append(k)
                    return akeys

                def stage_b(it, akeys):
                    e, gi = items[it]
                    g0, n = groups[gi]
                    slot = (hf * 32 + e) % 2
                    w2e, w2k = w2t[slot], "w2t%d" % slot
                    at = actT[it % 2]
                    for tt_ in range(n // 128):
                        ti = g0 // 128 + tt_
                        for half in range(2):
                            hs = slice(half * 512, (half + 1) * 512)
                            bq, bqk = bank()
                            mm(bq[:, :], bqk, [(at[:, fc, tt_ * 128:(tt_ + 1) * 128], w2e[:, fc, hs]) for fc in range(8)], akeys + [w2k + "_%d" % half])
                            V(lambda: nc.vector.scalar_tensor_tensor(out=yacc[:, ti, hs], in0=bq[:, :], scalar=gts[:, ti, e:e + 1], in1=yacc[:, ti, hs],
                                                                     op0=ALU.mult, op1=ALU.add), reads=[bqk, "gts", "yacc%d" % ti], writes=["yacc%d" % ti])

                load_expert(0, hf * 32 + 0)
                load_expert(1, hf * 32 + 1)
                pend = stage_a(0)
                for it in range(len(items)):
                    nxt = stage_a(it + 1) if it + 1 < len(items) else None
                    stage_b(it, pend)
                    pend = nxt
                    e, gi = items[it]
                    if gi == len(groups) - 1 and e + 2 < 32:
                        load_expert(e + 2, hf * 32 + e + 2)
                for ti, I in enumerate(tiles):
                    rows = 128 if I < NQ else ST
                    R = slice(0, rows)
                    yk = "yacc%d" % ti
                    for half in range(2):
                        V(lambda: nc.vector.bn_stats(out=bn[R, half, :], in_=yacc[R, ti, half * 512:(half + 1) * 512]), reads=[yk], writes=["bn%d" % half])
                    V(lambda: nc.vector.bn_aggr(out=mv[R, :], in_=bn[R, :, :]), reads=["bn0", "bn1"], writes=["mv"])
                    ln_rstd(mv[R, 1:2], rs1[R, :], sd[R, :], ["mv"], "rs1")
                    yoi, yok = yo[ti % 2], "yo%d" % (ti % 2)
                    V(lambda: nc.vector.tensor_scalar(out=yoi[R, :], in0=yacc[R, ti, :], scalar1=mv[R, 0:1], scalar2=rs1[R, 0:1], op0=ALU.subtract, op1=ALU.mult),
                      reads=[yk, "mv", "rs1"], writes=[yok])
                    G(lambda: nc.gpsimd.tensor_tensor(out=yoi[R, :], in0=yoi[R, :], in1=g2b[R, :], op=ALU.mult), reads=[yok, "g2b"], writes=[yok])
                    G(lambda: nc.gpsimd.tensor_tensor(out=yoi[R, :], in0=yoi[R, :], in1=b2b[R, :], op=ALU.add), reads=[yok, "b2b"], writes=[yok])
                    if I < NQ:
                        ld(lambda: nc.sync.dma_start(out=y_p[I * 128:(I + 1) * 128, :], in_=yoi[:, :]), reads=[yok], writes=[uniq("yout")])
                    else:
                        ld(lambda: nc.sync.dma_start(out=y_s[:, :], in_=yoi[R, :]), reads=[yok], writes=[uniq("yout")])
            S.finish("sp")


def _bucket_table():
    n = np.arange(0, 512)
    nf = np.maximum(n, 1).astype(np.float32)
    large = 16 + (np.log(nf / np.float32(16.0)) / np.float32(math.log(128 / 16)) * np.float32(16.0)).astype(np.int32)
    large = np.minimum(large, 31)
    return np.where(n < 16, n, large).astype(np.int64)


def _bias_tables(rel_bias):
    bt = _bucket_table()
    kk = np.arange(128)[:, None]
    cc = np.arange(128)[None, :]
    dist = np.zeros((5, 128, 128), np.int64)
    for r in range(3):
        d = np.where(cc < 64, 128 * (1 - r) + 64 + cc - kk, 128 * (2 - r) + cc - kk)
        dist[r] = d
    dist[3] = 128 + cc - kk
    dist[4] = cc - kk
    valid = dist >= 0
    valid[3][:, 4:] = True
    valid[4][4:, :] = True
    valid[4][:, 4:] = True
    dcl = np.clip(dist, 0, 511)
    braw = rel_bias[bt[dcl]]
    braw = np.where(valid[..., None], braw, 0.0).astype(np.float32)
    braw = np.ascontiguousarray(braw.transpose(1, 0, 3, 2)).reshape(128, 5 * 4 * 128)
    bmask = np.where(valid, 0.0, 8.0 * NEG).astype(np.float32)
    bmask = np.ascontiguousarray(np.broadcast_to(bmask[:, :, None, :], (5, 128, 4, 128)).transpose(1, 0, 2, 3)).reshape(128, 5 * 4 * 128)
    return braw, bmask


def kernel(x_prompt, x_sample, cache_k, cache_v, page_table, state_conv, w_in, b_in, lambda_q1, lambda_k1,
           lambda_q2, lambda_k2, subln_g, rel_bias, w_attn_proj, conv_w, conv_b, conv_ln_g, conv_ln_b,
           w_conv_proj, b_conv_proj, w_out, ln1_g, ln1_b, router_w, router_b, expert_w1, expert_b1,
           expert_w2, expert_b2, ln2_g, ln2_b):
    f = lambda a: np.ascontiguousarray(np.asarray(a, dtype=np.float32))
    x_prompt, x_sample, state_conv = f(x_prompt), f(x_sample), f(state_conv)
    rel_bias = f(rel_bias)
    braw, bmask = _bias_tables(rel_bias)
    shared = {
        "ck": f(cache_k).reshape(NPOOL * 128, 512), "cv": f(cache_v).reshape(NPOOL * 128, 512),
        "iot": np.arange(128, dtype=np.float32).reshape(128, 1), "braw": braw, "bmask": bmask,
        "w_in": f(w_in)[0], "b_in": f(b_in)[0],
        "lq1": f(lambda_q1)[0], "lk1": f(lambda_k1)[0], "lq2": f(lambda_q2)[0], "lk2": f(lambda_k2)[0],
        "subg": f(subln_g)[0], "rb31": np.ascontiguousarray(rel_bias[31]),
        "wap": f(w_attn_proj)[0], "convw": f(conv_w)[0], "convb": f(conv_b)[0], "clng": f(conv_ln_g)[0], "clnb": f(conv_ln_b)[0],
        "wcp": f(w_conv_proj)[0], "bcp": f(b_conv_proj)[0], "wout": f(w_out)[0], "ln1g": f(ln1_g)[0], "ln1b": f(ln1_b)[0],
        "rw": f(router_w)[0], "rbias": f(router_b)[0], "w1": f(expert_w1)[0], "b1": f(expert_b1)[0],
        "w2": f(expert_w2)[0], "b2": f(expert_b2)[0], "ln2g": f(ln2_g)[0], "ln2b": f(ln2_b)[0],
    }
    pt = np.ascontiguousarray(np.asarray(page_table, dtype=np.int32))
    nc = build_program()
    in_maps = []
    for c in range(NCORES):
        b, j = c // 2, c % 2
        if j == 1:
            xl = x_prompt[b]
        else:
            xl = np.concatenate([np.zeros((64, D), np.float32), x_prompt[b, :L - 64]], axis=0)
        one = np.ones((128, 1), np.float32)
        vm = one.copy()
        if j == 0:
            vm[:64] = 0.0
        m = dict(shared)
        m.update({
            "xloc": np.ascontiguousarray(xl),
            "xs": np.ascontiguousarray(x_sample[c * SS:(c + 1) * SS].reshape(ST, D)),
            "ptab": np.ascontiguousarray(pt[c * SS:(c + 1) * SS].reshape(-1)),
            "sconv": np.ascontiguousarray(state_conv[0, c * SS:(c + 1) * SS]),
            "vmk": vm, "hm": (one * float(j)).astype(np.float32),
        })
        in_maps.append(m)
    res = run_bass_kernel_spmd(nc, in_maps, core_ids=list(range(NCORES))).results
    B = x_prompt.shape[0]
    y_p = np.zeros((B, L, D), np.float32)
    nk_p = np.zeros((1, B, L, 4, 128), np.float32)
    nv_p = np.zeros((1, B, L, 4, 128), np.float32)
    nc_p = np.zeros((1, B, 30, 512), np.float32)
    y_s = np.zeros((128, 4, D), np.float32)
    nk_s = np.zeros((1, 128, 4, 4, 128), np.float32)
    nv_s = np.zeros((1, 128, 4, 4, 128), np.float32)
    nc_s = np.zeros((1, 128, 30, 512), np.float32)
    for c in range(NCORES):
        b, j = c // 2, c % 2
        r = res[c]
        rows = (np.arange(NBLK)[:, None] * 128 + 64 * j + np.arange(64)[None, :]).reshape(-1)
        y_p[b, rows] = r["y_p"]
        nk_p[0, b, rows] = r["nk_p"].reshape(TOK, 4, 128)
        nv_p[0, b, rows] = r["nv_p"].reshape(TOK, 4, 128)
        if j == 1:
            nc_p[0, b] = r["nc_p"]
        ss = slice(c * SS, (c + 1) * SS)
        y_s[ss] = r["y_s"].reshape(SS, 4, D)
        nk_s[0, ss] = r["nk_s"].reshape(SS, 4, 4, 128)
        nv_s[0, ss] = r["nv_s"].reshape(SS, 4, 4, 128)
        nc_s[0, ss] = r["nc_s"]
    return (y_p, y_s, nk_p, nv_p, nc_p, nk_s, nv_s, nc_s)
```
